# Optimizing a Trainium2 kernel written in Bass

```python
import math
import jax, jax.numpy as jnp
from jax import lax
import numpy as np

D_MODEL = 1024
BATCH = 8
SEQ = 2048
DEPTH = 2

HEAD_DIM = 64
A_WIDTH = D_MODEL // 2
A_HEADS = A_WIDTH // (2 * HEAD_DIM)
POOL_WINDOWS = (2, 4, 8, 16)
POOL_WIDTH = D_MODEL // 4
POOL_GROUPS = len(POOL_WINDOWS)
POOL_GROUP_DIM = POOL_WIDTH // POOL_GROUPS
C_WIDTH = D_MODEL // 4
C_HEADS = C_WIDTH // HEAD_DIM
DILATED_PAIRS = ((128, 1), (512, 4), (2048, 16))
IN_WIDTH = 3 * A_WIDTH + POOL_WIDTH + 3 * C_WIDTH
BLOCK = 128
ROPE_THETA = 10000.0
N_GROUPS = 4
EXPERTS_PER_GROUP = 8
TOP_K = 2
EXPERT_HIDDEN = D_MODEL // 4
N_MOD = 6
EPS = 1e-6

kernel_name = 'hymba_style_diffattn_pool_dilated_hmoe'


def rms_norm(x, g):
    xf = x.astype(jnp.float32)
    y = xf * lax.rsqrt(jnp.mean(xf * xf, axis=-1, keepdims=True) + EPS)
    return (y * g.astype(jnp.float32)).astype(x.dtype)


def rope_tables(seq, dim):
    inv = 1.0 / (ROPE_THETA ** (jnp.arange(0, dim, 2, dtype=jnp.float32) / dim))
    ang = jnp.arange(seq, dtype=jnp.float32)[:, None] * inv[None, :]
    ang = jnp.concatenate([ang, ang], axis=-1)
    return jnp.cos(ang), jnp.sin(ang)


def rope(t, cos, sin):
    half = t.shape[-1] // 2
    tf = t.astype(jnp.float32)
    rot = jnp.concatenate([-tf[..., half:], tf[..., :half]], axis=-1)
    return (tf * cos + rot * sin).astype(t.dtype)


def diff_attention(q, k, v, lam):
    B, H, _, S, dh = q.shape
    nb = S // BLOCK
    kpos = jnp.arange(S)
    scale = dh ** -0.5

    def block(i):
        qb = lax.dynamic_slice_in_dim(q, i * BLOCK, BLOCK, axis=3)
        s = jnp.einsum('bhmqd,bhmkd->bhmqk', qb, k, preferred_element_type=jnp.float32) * scale
        qpos = i * BLOCK + jnp.arange(BLOCK)
        s = jnp.where(kpos[None, :] <= qpos[:, None], s, -jnp.inf)
        p = jax.nn.softmax(s, axis=-1)
        a = p[:, :, 0] - lam * p[:, :, 1]
        return jnp.einsum('bhqk,bhkd->bhqd', a.astype(v.dtype), v)

    o = lax.map(block, jnp.arange(nb))
    return o.transpose(1, 2, 0, 3, 4).reshape(B, H, S, v.shape[-1])


def banded_causal_attention(q, k, v, w):
    lead = list(q.shape[:-2])
    L, dh = q.shape[-2], q.shape[-1]
    nb = -(-L // BLOCK)
    pad = nb * BLOCK - L
    padcfg = [(0, 0)] * len(lead) + [(0, pad), (0, 0)]

    def blocks(t):
        return jnp.pad(t, padcfg).reshape(*lead, nb, BLOCK, dh)

    def with_prev(t):
        prev = jnp.concatenate([jnp.zeros_like(t[..., :1, :, :]), t[..., :-1, :, :]], axis=-3)
        return jnp.concatenate([prev, t], axis=-2)

    qb = blocks(q)
    kb = with_prev(blocks(k))
    vb = with_prev(blocks(v))
    s = jnp.einsum('...nqd,...nkd->...nqk', qb, kb, preferred_element_type=jnp.float32) * (dh ** -0.5)
    a = jnp.arange(BLOCK)[:, None]
    b = jnp.arange(2 * BLOCK)[None, :]
    dist = a + BLOCK - b
    band = (dist >= 0) & (dist <= w)
    first = (jnp.arange(nb)[:, None, None] == 0) & (b[None] < BLOCK)
    mask = band[None] & jnp.logical_not(first)
    s = jnp.where(mask, s, -jnp.inf)
    lse = jax.nn.logsumexp(s, axis=-1)
    p = jnp.exp(s - lse[..., None])
    o = jnp.einsum('...nqk,...nkd->...nqd', p.astype(v.dtype), vb)
    o = o.reshape(*lead, nb * BLOCK, dh)[..., :L, :]
    lse = lse.reshape(*lead, nb * BLOCK)[..., :L]
    return o, lse


def dilated_branch(q, k, v, window, dil):
    B, H, S, dh = q.shape
    L = S // dil

    def sub(t):
        return t.reshape(B, H, L, dil, dh).transpose(0, 1, 3, 2, 4)

    o, lse = banded_causal_attention(sub(q), sub(k), sub(v), window // dil)
    o = o.transpose(0, 1, 3, 2, 4).reshape(B, H, S, dh)
    lse = lse.transpose(0, 1, 3, 2).reshape(B, H, S)
    return o, lse


def dilated_mixture(q, k, v):
    outs, lses = [], []
    for window, dil in DILATED_PAIRS:
        o, l = dilated_branch(q, k, v, window, dil)
        outs.append(o)
        lses.append(l)
    wts = jax.nn.softmax(jnp.stack(lses, axis=0), axis=0)
    out = jnp.sum(wts[..., None] * jnp.stack(outs, axis=0).astype(jnp.float32), axis=0)
    return out.astype(q.dtype)


def pool_mixer(u, w_pool, b_pool, scale):
    B, S, _ = u.shape
    uf = u.astype(jnp.float32).reshape(B, S, POOL_GROUPS, POOL_GROUP_DIM)
    cs = jnp.concatenate([jnp.zeros((B, 1, POOL_GROUPS, POOL_GROUP_DIM), jnp.float32),
                          jnp.cumsum(uf, axis=1)], axis=1)
    t = jnp.arange(S)
    win = jnp.array(POOL_WINDOWS, dtype=jnp.int32)
    lo = jnp.maximum(t[:, None] + 1 - win[None, :], 0)
    cnt = (t[:, None] + 1 - lo).astype(jnp.float32)
    gidx = jnp.arange(POOL_GROUPS)
    window_sum = cs[:, 1:] - cs[:, lo, gidx]
    d = window_sum / cnt[None, :, :, None] - uf
    y = jnp.einsum('bsgc,gce->bsge', d, w_pool.astype(jnp.float32)) + b_pool.astype(jnp.float32)
    return (y.reshape(B, S, POOL_WIDTH) * scale.astype(jnp.float32)).astype(u.dtype)


def hier_moe(h, w_rg, b_rg, w_re, b_re, w1, w3, w2):
    B, S, D = h.shape
    t = h.reshape(-1, D)
    gl = (t @ w_rg).astype(jnp.float32) + b_rg.astype(jnp.float32)
    gp = jax.nn.softmax(gl, axis=-1)
    g_idx = jnp.argmax(gl, axis=-1)
    g_w = jnp.take_along_axis(gp, g_idx[:, None], axis=1)[:, 0]
    el = jnp.einsum('td,gde->tge', t, w_re).astype(jnp.float32) + b_re.astype(jnp.float32)
    el_sel = jnp.take_along_axis(el, g_idx[:, None, None], axis=1)[:, 0]
    top_v, top_i = lax.top_k(el_sel, TOP_K)
    top_w = jax.nn.softmax(top_v, axis=-1) * g_w[:, None]
    e_w = jnp.sum(jax.nn.one_hot(top_i, EXPERTS_PER_GROUP, dtype=jnp.float32) * top_w[..., None], axis=1)
    combine = jax.nn.one_hot(g_idx, N_GROUPS, dtype=jnp.float32)[:, :, None] * e_w[:, None, :]
    y = jnp.zeros((t.shape[0], D), jnp.float32)
    for g in range(N_GROUPS):
        hid = jax.nn.silu(jnp.einsum('td,edf->tef', t, w1[g])) * jnp.einsum('td,edf->tef', t, w3[g])
        hid = hid * combine[:, g, :, None].astype(hid.dtype)
        y = y + jnp.einsum('tef,efd->td', hid, w2[g]).astype(jnp.float32)
    return y.reshape(B, S, D).astype(h.dtype)


def setup_inputs(seed: int = 0) -> dict:
    key = jax.random.key(seed)
    ks = jax.random.split(key, 24)
    L, D = DEPTH, D_MODEL
    G, E, F = N_GROUPS, EXPERTS_PER_GROUP, EXPERT_HIDDEN

    def nrm(k, shape, scale):
        return jax.random.normal(k, shape, jnp.float32) * scale

    return {
        'x': nrm(ks[0], (BATCH, SEQ, D), 1.0),
        'c': nrm(ks[1], (BATCH, D), 1.0),
        'w_mod': nrm(ks[2], (L, D, N_MOD * D), 0.5 * D ** -0.5),
        'b_mod': nrm(ks[3], (L, N_MOD * D), 0.01),
        'g_norm1': 1.0 + nrm(ks[4], (L, D), 0.1),
        'w_in': nrm(ks[5], (L, D, IN_WIDTH), D ** -0.5),
        'gq_a': 1.0 + nrm(ks[6], (L, HEAD_DIM), 0.1),
        'gk_a': 1.0 + nrm(ks[7], (L, HEAD_DIM), 0.1),
        'lam_a': nrm(ks[8], (L, 4, HEAD_DIM), 0.1),
        'g_sub_a': 1.0 + nrm(ks[9], (L, 2 * HEAD_DIM), 0.1),
        'w_pool': nrm(ks[10], (L, POOL_GROUPS, POOL_GROUP_DIM, POOL_GROUP_DIM), POOL_GROUP_DIM ** -0.5),
        'b_pool': nrm(ks[11], (L, POOL_GROUPS, POOL_GROUP_DIM), 0.01),
        'pool_scale': 1.0 + nrm(ks[12], (L, POOL_WIDTH), 0.1),
        'gq_c': 1.0 + nrm(ks[13], (L, HEAD_DIM), 0.1),
        'gk_c': 1.0 + nrm(ks[14], (L, HEAD_DIM), 0.1),
        'w_out': nrm(ks[15], (L, D, D), D ** -0.5),
        'g_norm2': 1.0 + nrm(ks[16], (L, D), 0.1),
        'w_rg': nrm(ks[17], (L, D, G), D ** -0.5),
        'b_rg': nrm(ks[18], (L, G), 0.01),
        'w_re': nrm(ks[19], (L, G, D, E), D ** -0.5),
        'b_re': nrm(ks[20], (L, G, E), 0.01),
        'w1': nrm(ks[21], (L, G, E, D, F), D ** -0.5),
        'w3': nrm(ks[22], (L, G, E, D, F), D ** -0.5),
        'w2': nrm(ks[23], (L, G, E, F, D), F ** -0.5),
    }


def reference(x, c, w_mod, b_mod, g_norm1, w_in, gq_a, gk_a, lam_a, g_sub_a, w_pool, b_pool,
              pool_scale, gq_c, gk_c, w_out, g_norm2, w_rg, b_rg, w_re, b_re, w1, w3, w2):
    B, S, D = x.shape
    cos, sin = rope_tables(S, HEAD_DIM)
    cond = jax.nn.silu(c)
    cuts = [A_WIDTH, 2 * A_WIDTH, 3 * A_WIDTH, 3 * A_WIDTH + POOL_WIDTH,
            3 * A_WIDTH + POOL_WIDTH + C_WIDTH, 3 * A_WIDTH + POOL_WIDTH + 2 * C_WIDTH]
    for l in range(DEPTH):
        mod = cond @ w_mod[l] + b_mod[l]
        sh1, sc1, ga1, sh2, sc2, ga2 = [m[:, None, :] for m in jnp.split(mod, N_MOD, axis=-1)]

        h = rms_norm(x, g_norm1[l]) * (1.0 + sc1) + sh1
        z = h @ w_in[l]
        qa, ka, va, ub, qc, kc, vc = jnp.split(z, cuts, axis=-1)

        qa = qa.reshape(B, S, A_HEADS, 2, HEAD_DIM).transpose(0, 2, 3, 1, 4)
        ka = ka.reshape(B, S, A_HEADS, 2, HEAD_DIM).transpose(0, 2, 3, 1, 4)
        va = va.reshape(B, S, A_HEADS, 2 * HEAD_DIM).transpose(0, 2, 1, 3)
        qa = rope(rms_norm(qa, gq_a[l]), cos, sin)
        ka = rope(rms_norm(ka, gk_a[l]), cos, sin)
        lam_init = 0.8 - 0.6 * math.exp(-0.3 * l)
        lp = lam_a[l].astype(jnp.float32)
        lam = jnp.exp(jnp.sum(lp[0] * lp[1])) - jnp.exp(jnp.sum(lp[2] * lp[3])) + lam_init
        ya = diff_attention(qa, ka, va, lam)
        ya = (rms_norm(ya, g_sub_a[l]) * (1.0 - lam_init)).transpose(0, 2, 1, 3).reshape(B, S, A_WIDTH)

        yb = pool_mixer(ub, w_pool[l], b_pool[l], pool_scale[l])

        qc = qc.reshape(B, S, C_HEADS, HEAD_DIM).transpose(0, 2, 1, 3)
        kc = kc.reshape(B, S, C_HEADS, HEAD_DIM).transpose(0, 2, 1, 3)
        vc = vc.reshape(B, S, C_HEADS, HEAD_DIM).transpose(0, 2, 1, 3)
        qc = rope(rms_norm(qc, gq_c[l]), cos, sin)
        kc = rope(rms_norm(kc, gk_c[l]), cos, sin)
        yc = dilated_mixture(qc, kc, vc).transpose(0, 2, 1, 3).reshape(B, S, C_WIDTH)

        y = jnp.concatenate([ya, yb, yc], axis=-1) @ w_out[l]
        x = x + ga1 * y

        h = rms_norm(x, g_norm2[l]) * (1.0 + sc2) + sh2
        x = x + ga2 * hier_moe(h, w_rg[l], b_rg[l], w_re[l], b_re[l], w1[l], w3[l], w2[l])
    return x
```

```python
import contextlib
import numpy as np
import ml_dtypes
import concourse.bass as bass
import concourse.mybir as mybir
from concourse.bass_utils import run_bass_kernel_spmd

F32 = mybir.dt.float32
BF16 = mybir.dt.bfloat16
AF = mybir.ActivationFunctionType
ALU = mybir.AluOpType
AX = mybir.AxisListType

ENGINES = ('sp', 'act', 'dve', 'pool', 'pe')
N_DMA_SEMS = 80
DT_SIZE = {F32: 4, BF16: 2}


class Buf:
    def __init__(self, name, kind, view, lo=0, hi=0):
        self.name = name
        self.kind = kind
        self.view = view
        self.lo, self.hi = lo, hi
        self.W = {}
        self.R = {}

    def ap(self):
        return self.view


class Op:
    __slots__ = ('eng', 'fn', 'deps', 'marked', 'done_key', 'done_val', 'is_dma', 'pre_wait', 'idx')

    def __init__(self, eng, fn, is_dma):
        self.eng = eng
        self.fn = fn
        self.deps = {}
        self.marked = False
        self.done_key = None
        self.done_val = None
        self.is_dma = is_dma
        self.pre_wait = None


class Prog:
    def __init__(self, nc, sbuf_bytes=200 * 1024):
        self.nc = nc
        self.ops = {e: [] for e in ENGINES}
        self.n_dma = 0
        self.dma_ops = []
        self.sbuf_bytes = sbuf_bytes
        self.sb_handle = nc.alloc_sbuf_tensor("sb_all", [128, sbuf_bytes // 4], F32)
        self.ps_handle = nc.alloc_psum_tensor("ps_all", [128, 4096], F32)
        self.sb_top = 0
        self.scopes = []
        self.live = {'sbuf': [], 'psum': []}
        self.retired = {'sbuf': [], 'psum': []}
        self.max_top = 0

    def _mk(self, name, kind, lo, shape, dtype):
        esz = DT_SIZE[dtype]
        n = int(np.prod(shape[1:]))
        nbytes = n * esz
        hi = lo + nbytes
        base = self.sb_handle if kind == 'sbuf' else self.ps_handle
        assert lo % 4 == 0
        v = base.ap()[0:shape[0], lo // 4:(lo + ((nbytes + 3) // 4) * 4) // 4]
        if dtype != F32:
            v = v.bitcast(dtype)
            v = v[:, 0:n]
        if len(shape) > 2:
            names = [f"d{i}" for i in range(len(shape) - 1)]
            kw = {nm: s for nm, s in zip(names[:-1], shape[1:-1])}
            v = v.rearrange("p (" + " ".join(names) + ") -> p " + " ".join(names), **kw)
        b = Buf(name, kind, v, lo, hi)
        for o in self.live[kind]:
            assert o.hi <= lo or o.lo >= hi, f"alias live {name} vs {o.name}"
        for o in self.retired[kind]:
            if not (o.hi <= lo or o.lo >= hi):
                for k, op in list(o.W.items()) + list(o.R.items()):
                    if k not in b.R or b.R[k].idx < op.idx:
                        b.R[k] = op
        self.live[kind].append(b)
        return b

    def sbuf(self, name, shape, dtype):
        lo = (self.sb_top + 63) // 64 * 64
        b = self._mk(name, 'sbuf', lo, shape, dtype)
        self.sb_top = b.hi
        self.max_top = max(self.max_top, self.sb_top)
        assert self.sb_top <= self.sbuf_bytes, f"SBUF overflow at {name}: {self.sb_top}"
        if self.scopes:
            self.scopes[-1][1].append(b)
        return b

    def psum(self, name, shape, dtype, byte_off):
        b = self._mk(name, 'psum', byte_off, shape, dtype)
        if self.scopes:
            self.scopes[-1][2].append(b)
        return b

    def dram_buf(self, name):
        return Buf(name, 'dram', None)

    def push(self):
        self.scopes.append((self.sb_top, [], []))

    def pop(self):
        top, sb, ps = self.scopes.pop()
        for b in sb:
            self.live['sbuf'].remove(b)
            self.retired['sbuf'].append(b)
        for b in ps:
            self.live['psum'].remove(b)
            self.retired['psum'].append(b)
        self.sb_top = top

    def free_psum(self, bufs):
        for b in bufs:
            self.live['psum'].remove(b)
            self.retired['psum'].append(b)
            for sc in self.scopes:
                if b in sc[2]:
                    sc[2].remove(b)

    def _track(self, op, reads, writes):
        op.idx = self._next_idx = getattr(self, '_next_idx', 0) + 1
        deps = op.deps

        def add(p):
            if p is op:
                return
            if p.eng == 'pe' and op.eng == 'pe' and not p.is_dma and not op.is_dma:
                return
            k = p.done_key
            if k not in deps or deps[k].idx < p.idx:
                deps[k] = p

        for b in reads:
            for p in b.W.values():
                add(p)
        for b in writes:
            for p in b.W.values():
                add(p)
            for p in b.R.values():
                add(p)
        for p in deps.values():
            p.marked = True
        for b in reads:
            b.R[op.done_key] = op
        for b in writes:
            if b.R:
                b.R = {}
                b.W = {}
            b.W[op.done_key] = op

    def op(self, eng, fn, reads, writes):
        o = Op(eng, fn, False)
        o.done_key = ('e', eng)
        self._track(o, reads, writes)
        self.ops[eng].append(o)
        return o

    def barrier(self):
        lasts = {}
        for e in ENGINES:
            for o in reversed(self.ops[e]):
                if o.fn is not None and not o.is_dma:
                    lasts[o.done_key] = o
                    break
        for o in self.dma_ops:
            k = o.done_key
            if k not in lasts or lasts[k].idx < o.idx:
                lasts[k] = o
        for p in lasts.values():
            p.marked = True
        for e in ENGINES:
            o = Op(e, None, False)
            o.done_key = ('e', e)
            o.idx = self._next_idx = getattr(self, '_next_idx', 0) + 1
            o.deps = {k: p for k, p in lasts.items() if not (k == ('e', e))}
            self.ops[e].append(o)

    def dma(self, eng, out, in_, reads, writes, **kw):
        o = Op(eng, lambda e: e.dma_start(out=out, in_=in_, **kw), True)
        i = self.n_dma
        self.n_dma += 1
        slot = i % N_DMA_SEMS
        o.done_key = ('d', slot)
        o.done_val = 16 * (i // N_DMA_SEMS + 1)
        if i >= N_DMA_SEMS:
            o.pre_wait = (('d', slot), 16 * (i // N_DMA_SEMS))
        self._track(o, reads, writes)
        self.ops[eng].append(o)
        self.dma_ops.append(o)
        return o

    def finish(self, final_bufs):
        nc = self.nc
        for e in ENGINES:
            c = 0
            for o in self.ops[e]:
                if o.is_dma or o.fn is None:
                    continue
                if o.marked:
                    c += 1
                    o.done_val = c
        final_waits = {}
        for b in final_bufs:
            for k, p in b.W.items():
                final_waits[k] = max(final_waits.get(k, 0), p.done_val)
        with contextlib.ExitStack() as st:
            sems = {}
            for e in ENGINES:
                sems[('e', e)] = st.enter_context(nc.semaphore(f"s_{e}"))
            for i in range(min(N_DMA_SEMS, max(1, self.n_dma))):
                sems[('d', i)] = st.enter_context(nc.semaphore(f"s_d{i}"))
            block = st.enter_context(nc.Block())

            def emit(ename, eng):
                waited = {}
                for o in self.ops[ename]:
                    need = {}
                    for k, p in o.deps.items():
                        assert p.done_val is not None, "dep not numbered"
                        need[k] = max(need.get(k, 0), p.done_val)
                    if o.pre_wait is not None:
                        k, v = o.pre_wait
                        need[k] = max(need.get(k, 0), v)
                    for k, v in need.items():
                        if waited.get(k, 0) >= v:
                            continue
                        eng.wait_ge(sems[k], v)
                        waited[k] = v
                    if o.fn is None:
                        continue
                    ins = o.fn(eng)
                    if o.is_dma:
                        ins.then_inc(sems[o.done_key], 16)
                    elif o.marked:
                        ins.then_inc(sems[o.done_key], 1)
                if ename == 'sp':
                    for k, v in final_waits.items():
                        if waited.get(k, 0) < v:
                            eng.wait_ge(sems[k], v)

            @block.sync
            def _(eng):
                emit('sp', eng)

            @block.scalar
            def _(eng):
                emit('act', eng)

            @block.vector
            def _(eng):
                emit('dve', eng)

            @block.gpsimd
            def _(eng):
                emit('pool', eng)

            @block.tensor
            def _(eng):
                emit('pe', eng)


S = 2048
D = 1024
NT = S // 128
DEPTH = 2
NEG = -30000.0
FENCE_M = False
BARRIERS = False
EPS = 1e-6
WINDOWS = (2, 4, 8, 16)


def host_consts():
    c = {}
    c['identf'] = np.eye(128, dtype=np.float32)
    c['identb'] = np.eye(128, dtype=np.float32).astype(ml_dtypes.bfloat16)
    inv = 1.0 / (10000.0 ** (np.arange(0, 64, 2, dtype=np.float32) / 64.0))
    ang = np.arange(S, dtype=np.float32)[:, None] * inv[None, :]
    cos = np.cos(ang).astype(np.float32)
    sin = np.sin(ang).astype(np.float32)
    def tm(a):
        return np.ascontiguousarray(a.reshape(NT, 128, 32).transpose(1, 0, 2))
    c['rope'] = np.ascontiguousarray(np.stack([tm(cos), tm(-sin), tm(sin)], axis=2))
    ki = np.arange(128)[:, None]
    cc = np.arange(896)[None, :]
    delta = cc - 384 - ki
    c['cstrip'] = np.where(delta >= 0, 0.0, NEG).astype(np.float32).astype(ml_dtypes.bfloat16)
    cc = np.arange(2432)[None, :]
    delta = cc - 384 - ki
    mult = ((delta >= 0) & (delta <= 128)).astype(np.int32) \
        + ((delta >= 0) & (delta <= 512) & (delta % 4 == 0)).astype(np.int32) \
        + ((delta >= 0) & (delta % 16 == 0)).astype(np.int32)
    c['dstrip'] = mult.astype(np.float32).astype(ml_dtypes.bfloat16)
    c['tri01'] = (np.arange(128)[None, :] >= np.arange(128)[:, None]).astype(np.float32).astype(ml_dtypes.bfloat16)
    c['invtab'] = np.tile((1.0 / (np.arange(16, dtype=np.float32) + 1.0))[None, :], (128, 1)).astype(np.float32)
    return c


def build_program(stop_at=None, layers=(0, 1)):
    nc = bass.Bass("TRN2", target_bir_lowering=False)
    L = DEPTH
    stop_layer = 0
    if stop_at is not None and '@' in stop_at:
        stop_at, sl_ = stop_at.split('@')
        stop_layer = int(sl_)

    def din(name, shape, dt=F32):
        return nc.dram_tensor(name, list(shape), dt, kind="ExternalInput").ap()

    x_d = din("x", [S, D])
    ccol_d = din("c_col", [128, 8])
    w_mod_d = din("w_mod", [L, D, 6 * D])
    b_mod_d = din("b_mod", [L, 6 * D])
    g1_d = din("g_norm1", [L, D])
    w_in_d = din("w_in", [L, D, 2560])
    gqa_d = din("gq_a", [L, 64])
    gka_d = din("gk_a", [L, 64])
    lam_d = din("lam_a", [L, 256])
    gsub_d = din("g_sub_a", [L, 128])
    wpool_d = din("w_pool", [L, 4, 64, 64])
    bpool_d = din("b_pool", [L, 256])
    pscale_d = din("pool_scale", [L, 256])
    gqc_d = din("gq_c", [L, 64])
    gkc_d = din("gk_c", [L, 64])
    w_out_d = din("w_out", [L, D, D])
    g2_d = din("g_norm2", [L, D])
    w_rg_d = din("w_rg", [L, D, 4])
    b_rg_d = din("b_rg", [L, 4])
    w_re_d = din("w_re", [L, 4, D, 8])
    b_re_d = din("b_re", [L, 32])
    if stop_at in ('n1', 'pool', 'attC', 'mix') and stop_layer == 0:
        w1_d = w3_d = w2_d = None
    else:
        w1_d = din("w1", [L, 32, D, 256])
        w3_d = din("w3", [L, 32, D, 256])
        w2_d = din("w2", [L, 32, 256, D])
    identf_d = din("identf", [128, 128])
    identb_d = din("identb", [128, 128], BF16)
    rope_d = din("rope", [128, NT, 3, 32])
    cstrip_d = din("cstrip", [128, 896], BF16)
    dstrip_d = din("dstrip", [128, 2432], BF16)
    invtab_d = din("invtab", [128, 16])
    tri01_d = din("tri01", [128, 128], BF16)
    out_d = nc.dram_tensor("out", [S, D], F32, kind="ExternalOutput").ap()
    dbg_d = nc.dram_tensor("dbg", [128, 2048], F32, kind="ExternalOutput").ap() if stop_at == 'pool' else None

    P = Prog(nc, sbuf_bytes=206 * 1024)
    out_buf = P.dram_buf("out")
    extra_final = []

    Xt = [P.sbuf(f"X{t}", [128, D], F32) for t in range(NT)]
    X = None
    identf = P.sbuf("identf", [128, 128], F32)
    identb = P.sbuf("identb", [128, 128], BF16)
    rope = P.sbuf("rope", [128, NT, 3, 32], F32)
    cstrip = P.sbuf("cstrip", [128, 896], BF16)
    dstrip = P.sbuf("dstrip", [128, 2432], BF16)
    invtab = P.sbuf("invtab", [128, 16], F32)
    tri01 = P.sbuf("tri01", [128, 128], BF16)
    condrep = P.sbuf("condrep", [128, 8, 128], BF16)
    Acol = [P.sbuf(f"Acol{i}", [128, 8], F32) for i in range(2)]
    Bcol = [P.sbuf(f"Bcol{i}", [128, 8], F32) for i in range(2)]
    gab = [P.sbuf(f"gab{i}", [128, D], F32) for i in range(2)]
    ones_col = P.sbuf("ones_col", [128, 1], F32)
    zerob = P.sbuf("zerob", [128, 128], BF16)

    xv = x_d.rearrange("(tt p) d -> p tt d", p=128)
    for t in range(NT):
        P.dma('sp', Xt[t].ap(), xv[:, t, :], reads=[], writes=[Xt[t]])
    P.dma('sp', identf.ap(), identf_d, [], [identf])
    P.dma('sp', identb.ap(), identb_d, [], [identb])
    P.dma('sp', rope.ap(), rope_d, [], [rope])
    P.dma('sp', cstrip.ap(), cstrip_d, [], [cstrip])
    P.dma('sp', dstrip.ap(), dstrip_d, [], [dstrip])
    P.dma('sp', invtab.ap(), invtab_d, [], [invtab])
    P.dma('sp', tri01.ap(), tri01_d, [], [tri01])
    P.op('pool', lambda e: e.memset(ones_col.ap(), 1.0), [], [ones_col])
    P.op('pool', lambda e: e.memset(zerob.ap(), 0.0), [], [zerob])

    P.push()
    ccol = P.sbuf("ccol", [128, 8], F32)
    ctmp = P.sbuf("ctmp", [128, 8], F32)
    P.dma('sp', ccol.ap(), ccol_d, [], [ccol])
    P.op('act', lambda e: e.activation(out=ctmp.ap(), in_=ccol.ap(), func=AF.Exp, scale=-1.0), [ccol], [ctmp])
    P.op('dve', lambda e: e.tensor_scalar(out=ctmp.ap(), in0=ctmp.ap(), scalar1=1.0, scalar2=None, op0=ALU.add), [ctmp], [ctmp])
    P.op('dve', lambda e: e.reciprocal(out=ctmp.ap(), in_=ctmp.ap()), [ctmp], [ctmp])
    P.op('dve', lambda e: e.tensor_tensor(out=ctmp.ap(), in0=ctmp.ap(), in1=ccol.ap(), op=ALU.mult), [ctmp, ccol], [ctmp])
    P.op('dve', lambda e: e.tensor_copy(out=condrep.ap(), in_=ctmp.ap().unsqueeze(2).to_broadcast([128, 8, 128])),
         [ctmp], [condrep])
    P.pop()

    BANK = 2048
    rr = {'evac': 0}

    def evac_engine():
        rr['evac'] += 1
        return 'act' if rr['evac'] % 2 == 0 else 'dve'

    def affine_evac(eng, out_ap, in_ap, sc_ap, bi_ap, reads, writes):
        if eng == 'act':
            P.op('act', lambda e: e.activation(out=out_ap, in_=in_ap, func=AF.Identity, bias=bi_ap, scale=sc_ap), reads, writes)
        else:
            P.op('dve', lambda e: e.tensor_scalar(out=out_ap, in0=in_ap, scalar1=sc_ap, scalar2=bi_ap, op0=ALU.mult, op1=ALU.add), reads, writes)

    def rstd_from_ss(ss_buf, n, tmp_buf=None):
        P.op('act', lambda e: e.activation(out=ss_buf.ap(), in_=ss_buf.ap(), func=AF.Ln, scale=1.0 / n, bias=EPS), [ss_buf], [ss_buf])
        P.op('act', lambda e: e.activation(out=ss_buf.ap(), in_=ss_buf.ap(), func=AF.Exp, scale=-0.5), [ss_buf], [ss_buf])

    for l in layers:
        if BARRIERS and l != layers[0]:
            P.barrier()
        P.push()
        wmb = [P.sbuf(f"wmb{i}", [128, 8, 512], BF16) for i in range(2)]
        bmb = [P.sbuf(f"bmb{i}", [128, 512], F32) for i in range(2)]
        gnb = [P.sbuf(f"gnb{i}", [128, 512], F32) for i in range(2)]
        modblk = [P.sbuf(f"modblk{i}", [128, 512], F32) for i in range(2)]
        dtmp = P.sbuf("dtmp", [128, 4, 128], F32)
        pm = [P.psum(f"pm{i}", [128, 512], F32, i * BANK) for i in range(2)]
        for j in range(12):
            v, half = j // 2, j % 2
            wb, bb, gb, mb, pp = wmb[j % 2], bmb[j % 2], gnb[j % 2], modblk[j % 2], pm[j % 2]
            P.dma('pool', wb.ap(), w_mod_d[l, :, j * 512:(j + 1) * 512].rearrange("(c p) n -> p c n", p=128), [X] if FENCE_M else [], [wb])
            P.dma('sp', bb.ap(), b_mod_d[l, j * 512:(j + 1) * 512].partition_broadcast(128), [X] if FENCE_M else [], [bb])
            for k in range(8):
                P.op('pe', lambda e, k=k, pp=pp, wb=wb: e.matmul(pp.ap(), lhsT=condrep.ap()[:, k, :], rhs=wb.ap()[:, k, :],
                                                               start=(k == 0), stop=(k == 7)), [condrep, wb], [pp])
            sub = 0 if v < 3 else 1
            vv = v % 3
            if vv == 2:
                dst = gab[sub].ap()[:, half * 512:(half + 1) * 512]
                P.op('dve', lambda e, dst=dst, pp=pp, bb=bb: e.tensor_tensor(out=dst, in0=pp.ap(), in1=bb.ap(), op=ALU.add),
                     [pp, bb], [gab[sub]])
                continue
            P.op('dve', lambda e, mb=mb, pp=pp, bb=bb: e.tensor_tensor(out=mb.ap(), in0=pp.ap(), in1=bb.ap(), op=ALU.add), [pp, bb], [mb])
            if vv == 1:
                gsrc = (g1_d if sub == 0 else g2_d)[l, half * 512:(half + 1) * 512].partition_broadcast(128)
                P.dma('sp', gb.ap(), gsrc, [], [gb])
                P.op('dve', lambda e, mb=mb, gb=gb: e.scalar_tensor_tensor(out=mb.ap(), in0=mb.ap(), scalar=1.0, in1=gb.ap(),
                                                                          op0=ALU.add, op1=ALU.mult), [mb, gb], [mb])
                dstb = Acol[sub]
            else:
                dstb = Bcol[sub]
            P.op('dve', lambda e, mb=mb: e.tensor_tensor(out=dtmp.ap(), in0=mb.ap().rearrange("p (c j) -> p c j", c=4),
                                                        in1=identf.ap().unsqueeze(1).to_broadcast([128, 4, 128]), op=ALU.mult),
                 [mb, identf], [dtmp])
            P.op('dve', lambda e, dstb=dstb, half=half: e.reduce_sum(out=dstb.ap()[:, half * 4:(half + 1) * 4], in_=dtmp.ap(), axis=AX.X),
                 [dtmp], [dstb])
        P.pop()

        if stop_at == 'M' and l == stop_layer:
            break
        if BARRIERS:
            P.barrier()

        def norm_to_hT(hT, sub, extra_scope_bufs):
            ssq, junk, diag, pn = extra_scope_bufs
            for tt in range(NT):
                P.op('act', lambda e, tt=tt: e.activation(out=junk.ap(), in_=Xt[tt].ap(), func=AF.Square,
                                                         accum_out=ssq.ap()[:, tt:tt + 1]), [Xt[tt]], [junk, ssq])
            rstd_from_ss(ssq, D)
            for tt in range(NT):
                P.op('dve', lambda e, tt=tt: e.tensor_scalar(out=diag.ap()[:, tt, :], in0=identf.ap(), scalar1=ssq.ap()[:, tt:tt + 1],
                                                            scalar2=None, op0=ALU.mult), [identf, ssq], [diag])
            i = 0
            for tq in range(4):
                for c in range(8):
                    pp = pn[i % 2]
                    i += 1
                    for t4 in range(4):
                        tt = tq * 4 + t4
                        P.op('pe', lambda e, pp=pp, tt=tt, c=c, t4=t4: e.matmul(pp.ap()[:, t4 * 128:(t4 + 1) * 128],
                                                                              lhsT=Xt[tt].ap()[:, c * 128:(c + 1) * 128],
                                                                              rhs=diag.ap()[:, tt, :], start=True, stop=True),
                             [Xt[tt], diag], [pp])
                    affine_evac(evac_engine(), hT.ap()[:, c, tq * 512:(tq + 1) * 512], pp.ap(),
                                Acol[sub].ap()[:, c:c + 1], Bcol[sub].ap()[:, c:c + 1], [pp, Acol[sub], Bcol[sub]], [hT])

        P.push()
        hT = P.sbuf("hT", [128, 8, S], BF16)
        P.push()
        ssq = P.sbuf("ssq", [128, NT], F32)
        junk = P.sbuf("junk", [128, D], BF16)
        diag = P.sbuf("diag", [128, NT, 128], F32)
        pn = [P.psum(f"pn{i}", [128, 512], F32, i * BANK) for i in range(2)]
        norm_to_hT(hT, 0, (ssq, junk, diag, pn))
        if stop_at == 'pool' and l == stop_layer:
            dbgb = P.dram_buf("dbg")
            scr = P.sbuf("scr", [128, 2048], F32)
            P.op('dve', lambda e: e.tensor_copy(out=scr.ap()[:, 0:1024], in_=X.ap()[:, 3, :]), [X], [scr])
            P.op('dve', lambda e: e.tensor_copy(out=scr.ap()[:, 1024:1032], in_=Acol[0].ap()), [Acol[0]], [scr])
            P.op('dve', lambda e: e.tensor_copy(out=scr.ap()[:, 1032:1040], in_=Bcol[0].ap()), [Bcol[0]], [scr])
            P.op('dve', lambda e: e.tensor_copy(out=scr.ap()[:, 1040:1056], in_=ssq.ap()), [ssq], [scr])
            P.op('dve', lambda e: e.tensor_copy(out=scr.ap()[:, 1536:2048], in_=hT.ap()[:, 0, 0:512]), [hT], [scr])
            P.dma('sp', dbg_d, scr.ap(), [scr], [dbgb])
            extra_final.append(dbgb)
        P.pop()

        if stop_at == 'n1' and l == stop_layer:
            break
        if BARRIERS:
            P.barrier()

        lam_init = 0.8 - 0.6 * float(np.exp(-0.3 * l))

        gq = {}
        for nm, src in (('qa', gqa_d), ('ka', gka_d), ('qc', gqc_d), ('kc', gkc_d)):
            gq[nm] = P.sbuf("g_" + nm, [128, 64], F32)
            P.dma('sp', gq[nm].ap(), src[l, :].partition_broadcast(128), [], [gq[nm]])
        gsub = P.sbuf("gsub", [128, 128], F32)
        P.dma('sp', gsub.ap(), gsub_d[l, :].partition_broadcast(128), [], [gsub])
        P.op('dve', lambda e, gsub=gsub, li=lam_init: e.tensor_scalar(out=gsub.ap(), in0=gsub.ap(), scalar1=1.0 - li, scalar2=None, op0=ALU.mult),
             [gsub], [gsub])
        lamt = P.sbuf("lamt", [128, 256], F32)
        lamv = P.sbuf("lamv", [128, 2], F32)
        neglam = P.sbuf("neglam", [128, 1], F32)
        P.dma('sp', lamt.ap(), lam_d[l, :].partition_broadcast(128), [], [lamt])
        lv = lamt.ap().rearrange("p (a d) -> p a d", a=4)
        P.op('dve', lambda e: e.tensor_tensor(out=lv[:, 0, :], in0=lv[:, 0, :], in1=lv[:, 1, :], op=ALU.mult), [lamt], [lamt])
        P.op('dve', lambda e: e.tensor_tensor(out=lv[:, 2, :], in0=lv[:, 2, :], in1=lv[:, 3, :], op=ALU.mult), [lamt], [lamt])
        P.op('dve', lambda e: e.reduce_sum(out=lamv.ap()[:, 0:1], in_=lv[:, 0, :], axis=AX.X), [lamt], [lamv])
        P.op('dve', lambda e: e.reduce_sum(out=lamv.ap()[:, 1:2], in_=lv[:, 2, :], axis=AX.X), [lamt], [lamv])
        P.op('act', lambda e: e.activation(out=lamv.ap(), in_=lamv.ap(), func=AF.Exp), [lamv], [lamv])
        P.op('dve', lambda e: e.tensor_tensor(out=neglam.ap(), in0=lamv.ap()[:, 1:2], in1=lamv.ap()[:, 0:1], op=ALU.subtract), [lamv], [neglam])
        P.op('dve', lambda e, neglam=neglam, li=lam_init: e.tensor_scalar(out=neglam.ap(), in0=neglam.ap(), scalar1=-li, scalar2=None, op0=ALU.add), [neglam], [neglam])

        def load_w_in_block(dst, col0, ncols):
            P.dma('pool', dst.ap(), w_in_d[l, :, col0:col0 + ncols].rearrange("(c p) n -> p c n", p=128), [], [dst])

        def load_wout_rows(dst, row0):
            P.dma('pool', dst.ap(), w_out_d[l, row0:row0 + 256, :].rearrange("(c p) n -> p c n", p=128), [], [dst])
            P.op('pool', lambda e: e.tensor_tensor(out=dst.ap(), in0=dst.ap(), in1=gab[0].ap().unsqueeze(1).to_broadcast([128, 2, D]),
                                                   op=ALU.mult), [dst, gab[0]], [dst])

        def wout_accumulate(lhs_fn, lhs_bufs, wo_sb, tt, pw):
            for half in range(2):
                pp = pw[half]
                for c in range(2):
                    P.op('pe', lambda e, pp=pp, c=c, half=half: e.matmul(pp.ap(), lhsT=lhs_fn(c), rhs=wo_sb.ap()[:, c, half * 512:(half + 1) * 512],
                                                                         start=(c == 0), stop=(c == 1)), lhs_bufs + [wo_sb], [pp])
                xs = Xt[tt].ap()[:, half * 512:(half + 1) * 512]
                P.op('dve', lambda e, xs=xs, pp=pp: e.tensor_tensor(out=xs, in0=xs, in1=pp.ap(), op=ALU.add), [Xt[tt], pp], [Xt[tt]])

        P.push()
        ubT = P.sbuf("ubT", [128, 2, S], F32)
        wblk = P.sbuf("wblk_p", [128, 8, 256], BF16)
        wo_sb = P.sbuf("wo_p", [128, 2, D], BF16)
        pA = P.sbuf("poolA", [128, S], F32)
        pB = P.sbuf("poolB", [128, S], F32)
        dT = P.sbuf("dT", [128, 2, S], BF16)
        ypT = P.sbuf("ypT", [128, 2, S], BF16)
        wpd = P.sbuf("wpd", [128, 2, 128], F32)
        wpdb = P.sbuf("wpdb", [128, 2, 128], BF16)
        psb = P.sbuf("psb", [128, 256], F32)
        bcol = P.sbuf("bcol", [128, 2], F32)
        scol = P.sbuf("scol", [128, 2], F32)
        pz = [P.psum(f"pz{i}", [128, 512], F32, i * BANK) for i in range(2)]
        pw = [P.psum(f"pw{i}", [128, 512], F32, (2 + i) * BANK) for i in range(2)]
        load_w_in_block(wblk, 1536, 256)
        load_wout_rows(wo_sb, 512)
        P.op('pool', lambda e: e.memset(wpd.ap(), 0.0), [], [wpd])
        for g in range(4):
            ch, hp = g // 2, (g % 2) * 64
            P.dma('sp', wpd.ap()[hp:hp + 64, ch, hp:hp + 64], wpool_d[l, g], [], [wpd])
        P.dma('sp', psb.ap(), pscale_d[l, :].partition_broadcast(128), [], [psb])
        P.dma('sp', bcol.ap(), bpool_d[l, :].rearrange("(c p) -> p c", p=128), [], [bcol], allow_slow_non_contiguous=True)
        P.dma('sp', scol.ap(), pscale_d[l, :].rearrange("(c p) -> p c", p=128), [], [scol], allow_slow_non_contiguous=True)
        P.op('dve', lambda e: e.tensor_tensor(out=wpdb.ap(), in0=wpd.ap(), in1=psb.ap().rearrange("p (c n) -> p c n", c=2), op=ALU.mult),
             [wpd, psb], [wpdb])
        P.op('dve', lambda e: e.tensor_tensor(out=bcol.ap(), in0=bcol.ap(), in1=scol.ap(), op=ALU.mult), [bcol, scol], [bcol])
        i = 0
        for ch in range(2):
            for tq in range(4):
                pp = pz[i % 2]
                i += 1
                for k in range(8):
                    P.op('pe', lambda e, pp=pp, k=k, ch=ch, tq=tq: e.matmul(pp.ap(), lhsT=wblk.ap()[:, k, ch * 128:(ch + 1) * 128],
                                                                          rhs=hT.ap()[:, k, tq * 512:(tq + 1) * 512],
                                                                          start=(k == 0), stop=(k == 7)), [wblk, hT], [pp])
                dst = ubT.ap()[:, ch, tq * 512:(tq + 1) * 512]
                if i % 2 == 0:
                    P.op('act', lambda e, dst=dst, pp=pp: e.copy(out=dst, in_=pp.ap()), [pp], [ubT])
                else:
                    P.op('dve', lambda e, dst=dst, pp=pp: e.tensor_copy(out=dst, in_=pp.ap()), [pp], [ubT])
        for ch in range(2):
            u = ubT.ap()[:, ch, :]
            seq = [(1, u, pA), (2, pA.ap(), pB)] + ([(4, pB.ap(), pA), (8, pA.ap(), pB)] if ch == 1 else [])
            srcbuf = ubT
            for sh, src, dstb in seq:
                P.op('pool', lambda e, sh=sh, src=src, dstb=dstb: e.tensor_copy(out=dstb.ap()[:, 0:sh], in_=src[:, 0:sh]), [srcbuf], [dstb])
                P.op('pool', lambda e, sh=sh, src=src, dstb=dstb: e.tensor_tensor(out=dstb.ap()[:, sh:S], in0=src[:, sh:S], in1=src[:, 0:S - sh],
                                                                                  op=ALU.add), [srcbuf], [dstb])
                srcbuf = dstb
            for hp, sb_, w in ((0, pA, WINDOWS[2 * ch]), (64, pB, WINDOWS[2 * ch + 1])):
                P.op('dve', lambda e, hp=hp, sb_=sb_, w=w, ch=ch: e.scalar_tensor_tensor(
                    out=dT.ap()[hp:hp + 64, ch, :], in0=sb_.ap()[hp:hp + 64, :], scalar=1.0 / w, in1=ubT.ap()[hp:hp + 64, ch, :],
                    op0=ALU.mult, op1=ALU.subtract), [sb_, ubT], [dT])
                P.op('dve', lambda e, hp=hp, sb_=sb_, w=w: e.tensor_tensor(out=sb_.ap()[hp:hp + 64, 0:w - 1], in0=sb_.ap()[hp:hp + 64, 0:w - 1],
                                                                           in1=invtab.ap()[hp:hp + 64, 0:w - 1], op=ALU.mult), [sb_, invtab], [sb_])
                P.op('dve', lambda e, hp=hp, sb_=sb_, w=w, ch=ch: e.tensor_tensor(out=dT.ap()[hp:hp + 64, ch, 0:w - 1], in0=sb_.ap()[hp:hp + 64, 0:w - 1],
                                                                                  in1=ubT.ap()[hp:hp + 64, ch, 0:w - 1], op=ALU.subtract),
                     [sb_, ubT], [dT])
            for tq in range(4):
                pp = pz[i % 2]
                i += 1
                P.op('pe', lambda e, pp=pp, ch=ch, tq=tq: e.matmul(pp.ap(), lhsT=wpdb.ap()[:, ch, :], rhs=dT.ap()[:, ch, tq * 512:(tq + 1) * 512],
                                                                  start=True, stop=True), [wpdb, dT], [pp])
                P.op('act', lambda e, pp=pp, ch=ch, tq=tq: e.activation(out=ypT.ap()[:, ch, tq * 512:(tq + 1) * 512], in_=pp.ap(), func=AF.Identity,
                                                                       bias=bcol.ap()[:, ch:ch + 1], scale=1.0), [pp, bcol], [ypT])
        for tt in range(NT):
            wout_accumulate(lambda c, tt=tt: ypT.ap()[:, c, tt * 128:(tt + 1) * 128], [ypT], wo_sb, tt, pw)
        P.pop()

        if stop_at == 'pool' and l == stop_layer:
            break

        def attention_group(kind, qcol, kcol, vcol, worow, gqb, gkb):
            P.push()
            nh = 2 if kind == 'A' else 4
            dv = 128 if kind == 'A' else 64
            qT = P.sbuf("qT", [128, 2, 2, S], BF16)
            kT = P.sbuf("kT", [128, 2, S], BF16)
            P.op('pool', lambda e: e.memset(qT.ap(), 0.0), [], [qT])
            vA = P.sbuf("vA", [128, NT, nh, dv + 1], BF16)
            wo_sb = P.sbuf("wo_a", [128, 2, D], BF16)
            load_wout_rows(wo_sb, worow)
            P.op('pool', lambda e: e.memset(vA.ap()[:, :, :, dv:dv + 1], 1.0), [], [vA])

            P.push()
            wq = P.sbuf("wq", [128, 8, 256], BF16)
            wk = P.sbuf("wk", [128, 8, 256], BF16)
            wv = P.sbuf("wv", [128, 8, 256], BF16)
            load_w_in_block(wq, qcol, 256)
            load_w_in_block(wk, kcol, 256)
            load_w_in_block(wv, vcol, 256)
            NB = 5
            sq = [P.sbuf(f"sq{i}", [128, 4, 64], F32) for i in range(NB)]
            ssr = [P.sbuf(f"ssr{i}", [128, 4], F32) for i in range(NB)]
            qg = [P.sbuf(f"qg{i}", [128, 4, 2, 32], F32) for i in range(NB)]
            t1 = [P.sbuf(f"t1{i}", [128, 4, 2, 32], F32) for i in range(NB)]
            t2 = [P.sbuf(f"t2{i}", [128, 4, 2, 32], F32) for i in range(NB)]
            qo = [P.sbuf(f"qo{i}", [128, 4, 64], BF16) for i in range(NB)]
            NPZ = 6
            pz = [P.psum(f"pza{i}", [128, 256], F32, i * BANK) for i in range(NPZ)]
            ptr = [P.psum(f"ptr{i}", [128, 2, 128], BF16, (6 + i) * BANK) for i in range(2)]
            cnt = 0
            pend_tr = []
            n_tr = [0]
            TR_LAG = 3
            for tt in range(NT):
                for which, wsb, gb_, dstT in (('q', wq, gqb, qT), ('k', wk, gkb, kT), ('v', wv, None, None)):
                    pp = pz[cnt % NPZ]
                    for k in range(8):
                        P.op('pe', lambda e, pp=pp, k=k, tt=tt, wsb=wsb: e.matmul(pp.ap(), lhsT=hT.ap()[:, k, tt * 128:(tt + 1) * 128],
                                                                                rhs=wsb.ap()[:, k, :], start=(k == 0), stop=(k == 7)),
                             [hT, wsb], [pp])
                    if which == 'v':
                        P.op('act', lambda e, pp=pp, tt=tt: e.copy(out=vA.ap()[:, tt, :, 0:dv], in_=pp.ap().rearrange("p (h d) -> p h d", h=nh)),
                             [pp], [vA])
                        cnt += 1
                        continue
                    b = (len(pend_tr) + n_tr[0]) % NB
                    s_, r_, g_, a_, b_, o_ = sq[b], ssr[b], qg[b], t1[b], t2[b], qo[b]
                    pv = pp.ap().rearrange("p (h d) -> p h d", h=4)
                    P.op('act', lambda e, s_=s_, pv=pv: e.activation(out=s_.ap(), in_=pv, func=AF.Square), [pp], [s_])
                    P.op('dve', lambda e, s_=s_, r_=r_: e.reduce_sum(out=r_.ap(), in_=s_.ap(), axis=AX.X), [s_], [r_])
                    rstd_from_ss(r_, 64)
                    P.op('dve', lambda e, g_=g_, pv=pv, gb_=gb_: e.tensor_tensor(out=g_.ap().rearrange("p h a d -> p h (a d)"), in0=pv,
                                                                             in1=gb_.ap().unsqueeze(1).to_broadcast([128, 4, 64]), op=ALU.mult),
                         [pp, gb_], [g_])
                    cosb = rope.ap()[:, tt, 0:1, :].unsqueeze(1).to_broadcast([128, 4, 2, 32])
                    P.op('dve', lambda e, a_=a_, g_=g_, cosb=cosb: e.tensor_tensor(out=a_.ap(), in0=g_.ap(), in1=cosb, op=ALU.mult), [g_, rope], [a_])
                    nsin = rope.ap()[:, tt, 1:2, :].to_broadcast([128, 4, 32])
                    psin = rope.ap()[:, tt, 2:3, :].to_broadcast([128, 4, 32])
                    P.op('pool', lambda e, b_=b_, g_=g_, nsin=nsin: e.tensor_tensor(out=b_.ap()[:, :, 0, :], in0=g_.ap()[:, :, 1, :], in1=nsin, op=ALU.mult),
                         [g_, rope], [b_])
                    P.op('pool', lambda e, b_=b_, g_=g_, psin=psin: e.tensor_tensor(out=b_.ap()[:, :, 1, :], in0=g_.ap()[:, :, 0, :], in1=psin, op=ALU.mult),
                         [g_, rope], [b_])
                    P.op('dve', lambda e, a_=a_, b_=b_: e.tensor_tensor(out=a_.ap(), in0=a_.ap(), in1=b_.ap(), op=ALU.add), [a_, b_], [a_])
                    P.op('dve', lambda e, a_=a_, r_=r_, o_=o_: e.tensor_tensor(out=o_.ap(), in0=a_.ap().rearrange("p h a d -> p h (a d)"),
                                                                             in1=r_.ap().unsqueeze(2).to_broadcast([128, 4, 64]), op=ALU.mult),
                         [a_, r_], [o_])
                    def emit_tr(o_=o_, dstT=dstT, tt=tt, ti=len(pend_tr) + n_tr[0]):
                        pt_ = ptr[ti % 2]
                        for pr in range(2):
                            P.op('pe', lambda e, pt_=pt_, pr=pr, o_=o_: e.transpose(pt_.ap()[:, pr, :], o_.ap()[:, 2 * pr:2 * pr + 2, :].rearrange("p h d -> p (h d)"),
                                                                                   identb.ap()), [o_, identb], [pt_])
                        if dstT is qT:
                            for hf in range(2):
                                P.op('act', lambda e, pt_=pt_, tt=tt, hf=hf: e.copy(out=qT.ap()[hf * 64:(hf + 1) * 64, :, hf, tt * 128:(tt + 1) * 128],
                                                                                 in_=pt_.ap()[hf * 64:(hf + 1) * 64, :, :]), [pt_], [qT])
                        else:
                            P.op('act', lambda e, pt_=pt_, dstT=dstT, tt=tt: e.copy(out=dstT.ap()[:, :, tt * 128:(tt + 1) * 128], in_=pt_.ap()), [pt_], [dstT])
                    pend_tr.append(emit_tr)
                    while len(pend_tr) > TR_LAG:
                        pend_tr.pop(0)()
                        n_tr[0] += 1
                    cnt += 1
            while pend_tr:
                pend_tr.pop(0)()
                n_tr[0] += 1
            P.pop()

            P.push()
            NPT = 3 if kind == 'A' else 6
            LAG = 1 if kind == 'A' else 3
            PT = [P.sbuf(f"PT{i}", [128, 512], BF16) for i in range(NPT)]
            pst = [P.psum(f"pst{i}", [128, 512], F32, bk * BANK) for i, bk in enumerate((0, 1) if kind == 'A' else (0, 1, 4, 5))]
            ptr2 = P.psum("ptr2", [128, 2, 128], BF16, 6 * BANK)
            pw = [P.psum("pwa0", [128, 512], F32, 7 * BANK), P.psum("pwa1", [128, 512], F32, 6 * BANK + 1024)] if False else None
            pwo = P.psum("pwo", [128, 512], F32, 7 * BANK)
            if kind == 'A':
                pO = [[P.psum(f"pO{m}{j}", [128, dv + 1], F32, (2 + 2 * m + j // 2) * BANK + (j % 2) * 1024) for j in range(4)] for m in range(2)]
            else:
                pO = [[P.psum(f"pO{m}{j}", [128, dv + 1], F32, (2 + m) * BANK + j * 512) for j in range(4)] for m in range(2)]
            strip = cstrip if kind == 'A' else dstrip
            ycat = P.sbuf("ycat", [128, 4, 256], BF16)
            ycT = P.sbuf("ycT", [128, 2, 128], BF16)
            if kind == 'A':
                Oc = [P.sbuf(f"Oc{m}", [128, 4, dv + 1], F32) for m in range(2)]
                rr_ = P.sbuf("rr_", [128, 2, 4], F32)
                av = P.sbuf("av", [128, 4, 128], F32)
                bv = P.sbuf("bv", [128, 4, 128], F32)
                ssa = P.sbuf("ssa", [128, 4], F32)
            else:
                Oc = [P.sbuf(f"Oc{m}", [128, 4, dv + 1], F32) for m in range(4)]
                rr_ = P.sbuf("rr_", [128, 4, 4], F32)
            state = {'st': 0, 'pt': 0}

            def run_streams(streams, qc):
                nk = 4 * qc + 4
                items = [(si, kt) for kt in range(nk) for si in range(len(streams))]
                staged = []

                def issue_qk(si, kt):
                    qsel, ksel, vsel, Oacc = streams[si]
                    ps = pst[state['st'] % len(pst)]
                    state['st'] += 1
                    masked = (kind == 'C') or (kt >= 4 * qc)
                    if kind == 'A' and masked:
                        c0 = (kt - 4 * qc) * 128
                        P.op('pe', lambda e, ps=ps, kt=kt, c0=c0: e.matmul(ps.ap()[:, c0:512], lhsT=ksel(kt), rhs=qsel(qc)[:, c0:512], start=True, stop=True),
                             [kT, qT], [ps])
                        pt = PT[state['pt'] % NPT]
                        state['pt'] += 1
                        P.op('act', lambda e, pt=pt, ps=ps, c0=c0: e.activation(out=pt.ap()[:, c0:512], in_=ps.ap()[:, c0:512], func=AF.Exp, scale=0.125), [ps], [pt])
                        P.op('pool', lambda e, pt=pt, c0=c0: e.tensor_tensor(out=pt.ap()[:, c0:c0 + 128], in0=pt.ap()[:, c0:c0 + 128], in1=tri01.ap(), op=ALU.mult),
                             [pt, tri01], [pt])
                        return pt
                    P.op('pe', lambda e, ps=ps, kt=kt: e.matmul(ps.ap(), lhsT=ksel(kt), rhs=qsel(qc), start=True, stop=True), [kT, qT], [ps])
                    pt = PT[state['pt'] % NPT]
                    state['pt'] += 1
                    P.op('act', lambda e, pt=pt, ps=ps: e.activation(out=pt.ap(), in_=ps.ap(), func=AF.Exp, scale=0.125), [ps], [pt])
                    if kind == 'C':
                        off = 512 * qc - 128 * kt + 384
                        P.op('pool', lambda e, pt=pt, off=off: e.tensor_tensor(out=pt.ap(), in0=pt.ap(), in1=strip.ap()[:, off:off + 512], op=ALU.mult),
                             [pt, strip], [pt])
                    return pt

                def issue_pv(si, kt, pt):
                    qsel, ksel, vsel, Oacc = streams[si]
                    for j in range(4):
                        qt = 4 * qc + j
                        if kt > qt:
                            continue
                        P.op('pe', lambda e, j=j, kt=kt, pt=pt, Oacc=Oacc, qt=qt: e.matmul(Oacc[j].ap(), lhsT=pt.ap()[:, j * 128:(j + 1) * 128], rhs=vsel(kt),
                                                                                        start=False, stop=(kt == qt), skip_group_check=True),
                             [pt, vA], [Oacc[j]])

                for (qsel, ksel, vsel, Oacc) in streams:
                    banks = sorted(set(o.lo // BANK for o in Oacc))
                    for bk in banks:
                        accs = [o for o in Oacc if o.lo // BANK == bk]
                        bank_ap = P.ps_handle.ap()[:, bk * 512:(bk + 1) * 512]
                        P.op('pe', lambda e, bank_ap=bank_ap: e.matmul(bank_ap, lhsT=zerob.ap(), rhs=dstrip.ap()[:, 0:512], start=True, stop=True),
                             [zerob, dstrip], accs)
                pend = []
                for (si, kt) in items:
                    pt = issue_qk(si, kt)
                    pend.append((si, kt, pt))
                    if len(pend) > LAG:
                        issue_pv(*pend.pop(0))
                while pend:
                    issue_pv(*pend.pop(0))

            pend_tail = []

            def flush_tail():
                while pend_tail:
                    pend_tail.pop(0)()

            for qc in range(4):
                if kind == 'A':
                    for h in range(2):
                        streams = []
                        for m in range(2):
                            lo = m * 64
                            streams.append((lambda qc_, h=h, m=m: qT.ap()[:, h, m, qc_ * 512:(qc_ + 1) * 512],
                                            lambda kt, h=h: kT.ap()[:, h, kt * 128:(kt + 1) * 128],
                                            lambda kt, h=h: vA.ap()[:, kt, h, :],
                                            pO[m]))
                        run_streams(streams, qc)
                        if h == 0:
                            flush_tail()
                        for m in range(2):
                            for jj in range(2):
                                src_bank = pO[m][2 * jj]
                                for j in (2 * jj, 2 * jj + 1):
                                    eng = 'act' if j % 2 == 0 else 'dve'
                                    if eng == 'act':
                                        P.op('act', lambda e, m=m, j=j: e.copy(out=Oc[m].ap()[:, j, :], in_=pO[m][j].ap()), [pO[m][j]], [Oc[m]])
                                    else:
                                        P.op('dve', lambda e, m=m, j=j: e.tensor_copy(out=Oc[m].ap()[:, j, :], in_=pO[m][j].ap()), [pO[m][j]], [Oc[m]])
                        for m in range(2):
                            P.op('dve', lambda e, m=m: e.reciprocal(out=rr_.ap()[:, m, :], in_=Oc[m].ap()[:, :, dv]), [Oc[m]], [rr_])
                        P.op('dve', lambda e: e.tensor_scalar(out=rr_.ap()[:, 1, :], in0=rr_.ap()[:, 1, :], scalar1=neglam.ap(), scalar2=None, op0=ALU.mult),
                             [rr_, neglam], [rr_])
                        P.op('pool', lambda e: e.tensor_tensor(out=av.ap(), in0=Oc[0].ap()[:, :, 0:dv], in1=rr_.ap()[:, 0, :].unsqueeze(2).to_broadcast([128, 4, dv]),
                                                               op=ALU.mult), [Oc[0], rr_], [av])
                        P.op('dve', lambda e: e.tensor_tensor(out=bv.ap(), in0=Oc[1].ap()[:, :, 0:dv], in1=rr_.ap()[:, 1, :].unsqueeze(2).to_broadcast([128, 4, dv]),
                                                              op=ALU.mult), [Oc[1], rr_], [bv])
                        P.op('dve', lambda e: e.tensor_tensor(out=av.ap(), in0=av.ap(), in1=bv.ap(), op=ALU.add), [av, bv], [av])
                        P.op('pool', lambda e: e.tensor_tensor(out=bv.ap(), in0=av.ap(), in1=av.ap(), op=ALU.mult), [av], [bv])
                        P.op('dve', lambda e: e.reduce_sum(out=ssa.ap(), in_=bv.ap(), axis=AX.X), [bv], [ssa])
                        rstd_from_ss(ssa, 128)
                        P.op('dve', lambda e: e.tensor_tensor(out=av.ap(), in0=av.ap(), in1=ssa.ap().unsqueeze(2).to_broadcast([128, 4, dv]), op=ALU.mult),
                             [av, ssa], [av])
                        P.op('pool', lambda e, h=h: e.tensor_tensor(out=ycat.ap()[:, :, h * 128:(h + 1) * 128], in0=av.ap(),
                                                                    in1=gsub.ap().unsqueeze(1).to_broadcast([128, 4, dv]), op=ALU.mult), [av, gsub], [ycat])
                else:
                    for hp in range(2):
                        streams = []
                        for hh in range(2):
                            hd = 2 * hp + hh
                            lo = (hd % 2) * 64
                            pr = hd // 2
                            streams.append((lambda qc_, pr=pr, hd=hd: qT.ap()[:, pr, hd % 2, qc_ * 512:(qc_ + 1) * 512],
                                            lambda kt, pr=pr: kT.ap()[:, pr, kt * 128:(kt + 1) * 128],
                                            lambda kt, hd=hd: vA.ap()[:, kt, hd, :],
                                            pO[hh]))
                        run_streams(streams, qc)
                        if hp == 0:
                            flush_tail()
                        for hh in range(2):
                            hd = 2 * hp + hh
                            for j in range(4):
                                if (hd + j) % 2 == 0:
                                    P.op('act', lambda e, hd=hd, hh=hh, j=j: e.copy(out=Oc[hd].ap()[:, j, :], in_=pO[hh][j].ap()), [pO[hh][j]], [Oc[hd]])
                                else:
                                    P.op('dve', lambda e, hd=hd, hh=hh, j=j: e.tensor_copy(out=Oc[hd].ap()[:, j, :], in_=pO[hh][j].ap()), [pO[hh][j]], [Oc[hd]])
                    for hd in range(4):
                        P.op('dve', lambda e, hd=hd: e.reciprocal(out=rr_.ap()[:, hd, :], in_=Oc[hd].ap()[:, :, dv]), [Oc[hd]], [rr_])
                        eng = 'pool' if hd % 2 == 0 else 'dve'
                        P.op(eng, lambda e, hd=hd: e.tensor_tensor(out=ycat.ap()[:, :, hd * 64:(hd + 1) * 64], in0=Oc[hd].ap()[:, :, 0:dv],
                                                                   in1=rr_.ap()[:, hd, :].unsqueeze(2).to_broadcast([128, 4, dv]), op=ALU.mult),
                             [Oc[hd], rr_], [ycat])
                def tail(qc=qc):
                    for j in range(4):
                      tt = 4 * qc + j
                      for c in range(2):
                          P.op('pe', lambda e, j=j, c=c: e.transpose(ptr2.ap()[:, c, :], ycat.ap()[:, j, c * 128:(c + 1) * 128], identb.ap()),
                               [ycat, identb], [ptr2])
                      P.op('act', lambda e: e.copy(out=ycT.ap(), in_=ptr2.ap()), [ptr2], [ycT])
                      for half in range(2):
                          for c in range(2):
                              P.op('pe', lambda e, c=c, half=half: e.matmul(pwo.ap(), lhsT=ycT.ap()[:, c, :], rhs=wo_sb.ap()[:, c, half * 512:(half + 1) * 512],
                                                                            start=(c == 0), stop=(c == 1)), [ycT, wo_sb], [pwo])
                          xs = Xt[tt].ap()[:, half * 512:(half + 1) * 512]
                          P.op('dve', lambda e, xs=xs: e.tensor_tensor(out=xs, in0=xs, in1=pwo.ap(), op=ALU.add), [Xt[tt], pwo], [Xt[tt]])
                pend_tail.append(tail)
            flush_tail()
            P.pop()
            P.pop()

        attention_group('C', 1792, 2048, 2304, 768, gq['qc'], gq['kc'])
        if stop_at == 'attC' and l == stop_layer:
            break
        attention_group('A', 0, 512, 1024, 0, gq['qa'], gq['ka'])
        attention_group('A', 256, 768, 1280, 256, gq['qa'], gq['ka'])
        P.pop()
        if stop_at == 'mix' and l == stop_layer:
            break

        P.push()
        hT = P.sbuf("h2T", [128, 8, S], BF16)
        P.push()
        ssq = P.sbuf("ssq", [128, NT], F32)
        junk = P.sbuf("junk", [128, D], BF16)
        diag = P.sbuf("diag", [128, NT, 128], F32)
        pn = [P.psum(f"pn{i}", [128, 512], F32, i * BANK) for i in range(2)]
        norm_to_hT(hT, 1, (ssq, junk, diag, pn))
        P.pop()

        cw = P.sbuf("cw", [128, NT, 32], F32)
        P.push()
        wrf = P.sbuf("wrf", [128, 8, 36], F32)
        wrb = P.sbuf("wrb", [128, 8, 36], BF16)
        brb = P.sbuf("brb", [128, 36], F32)
        lg = P.sbuf("lg", [128, NT, 36], F32)
        P.dma('sp', wrf.ap()[:, :, 0:4], w_rg_d[l].rearrange("(c p) n -> p c n", p=128), [], [wrf], allow_slow_non_contiguous=True)
        for g in range(4):
            P.dma('sp', wrf.ap()[:, :, 4 + 8 * g:12 + 8 * g], w_re_d[l, g].rearrange("(c p) n -> p c n", p=128), [], [wrf], allow_slow_non_contiguous=True)
        P.dma('sp', brb.ap()[:, 0:4], b_rg_d[l, :].partition_broadcast(128), [], [brb])
        P.dma('sp', brb.ap()[:, 4:36], b_re_d[l, :].partition_broadcast(128), [], [brb])
        P.op('dve', lambda e: e.tensor_copy(out=wrb.ap(), in_=wrf.ap()), [wrf], [wrb])
        pl = [P.psum(f"pl{i}", [128, 8, 36], F32, (2 + i) * BANK) for i in range(2)]
        for tt in range(NT):
            pp = pl[tt // 8]
            for k in range(8):
                P.op('pe', lambda e, pp=pp, tt=tt, k=k: e.matmul(pp.ap()[:, tt % 8, :], lhsT=hT.ap()[:, k, tt * 128:(tt + 1) * 128], rhs=wrb.ap()[:, k, :],
                                                                start=(k == 0), stop=(k == 7)), [hT, wrb], [pp])
        for i2 in range(2):
            P.op('dve', lambda e, i2=i2: e.tensor_tensor(out=lg.ap()[:, 8 * i2:8 * i2 + 8, :], in0=pl[i2].ap(),
                                                         in1=brb.ap().unsqueeze(1).to_broadcast([128, 8, 36]), op=ALU.add), [pl[i2], brb], [lg])
        mg = P.sbuf("mg", [128, NT], F32)
        ohg = P.sbuf("ohg", [128, NT, 4], F32)
        eg = P.sbuf("eg", [128, NT, 4], F32)
        gw = P.sbuf("gw", [128, NT], F32)
        elm = P.sbuf("elm", [128, NT, 32], F32)
        elm2 = P.sbuf("elm2", [128, NT, 32], F32)
        oh1 = P.sbuf("oh1", [128, NT, 32], F32)
        oh2 = P.sbuf("oh2", [128, NT, 32], F32)
        tp1 = P.sbuf("tp1", [128, NT], F32)
        tp2 = P.sbuf("tp2", [128, NT], F32)
        w1g = P.sbuf("w1g", [128, NT], F32)
        w2g = P.sbuf("w2g", [128, NT], F32)
        gl = lg.ap()[:, :, 0:4]
        el = lg.ap()[:, :, 4:36]
        bc4 = lambda b_: b_.ap().unsqueeze(2).to_broadcast([128, NT, 4])
        bc32 = lambda b_: b_.ap().unsqueeze(2).to_broadcast([128, NT, 32])
        P.op('dve', lambda e: e.reduce_max(out=mg.ap(), in_=gl, axis=AX.X), [lg], [mg])
        P.op('dve', lambda e: e.tensor_tensor(out=ohg.ap(), in0=gl, in1=bc4(mg), op=ALU.is_ge), [lg, mg], [ohg])
        P.op('dve', lambda e: e.tensor_tensor(out=eg.ap(), in0=gl, in1=bc4(mg), op=ALU.subtract), [lg, mg], [eg])
        P.op('act', lambda e: e.activation(out=eg.ap(), in_=eg.ap(), func=AF.Exp), [eg], [eg])
        P.op('dve', lambda e: e.reduce_sum(out=gw.ap(), in_=eg.ap(), axis=AX.X), [eg], [gw])
        P.op('dve', lambda e: e.reciprocal(out=gw.ap(), in_=gw.ap()), [gw], [gw])
        P.op('dve', lambda e: e.tensor_scalar(out=ohg.ap(), in0=ohg.ap(), scalar1=1.0, scalar2=1e30, op0=ALU.subtract, op1=ALU.mult), [ohg], [ohg])
        P.op('dve', lambda e: e.tensor_tensor(out=elm.ap().rearrange("p t (g x) -> p t g x", g=4), in0=el.rearrange("p t (g x) -> p t g x", g=4),
                                              in1=ohg.ap().unsqueeze(3).to_broadcast([128, NT, 4, 8]), op=ALU.add), [lg, ohg], [elm])
        P.op('dve', lambda e: e.reduce_max(out=tp1.ap(), in_=elm.ap(), axis=AX.X), [elm], [tp1])
        P.op('dve', lambda e: e.tensor_tensor(out=oh1.ap(), in0=elm.ap(), in1=bc32(tp1), op=ALU.is_ge), [elm, tp1], [oh1])
        P.op('dve', lambda e: e.scalar_tensor_tensor(out=elm2.ap(), in0=oh1.ap(), scalar=-1e30, in1=elm.ap(), op0=ALU.mult, op1=ALU.add), [oh1, elm], [elm2])
        P.op('dve', lambda e: e.reduce_max(out=tp2.ap(), in_=elm2.ap(), axis=AX.X), [elm2], [tp2])
        P.op('dve', lambda e: e.tensor_tensor(out=oh2.ap(), in0=elm2.ap(), in1=bc32(tp2), op=ALU.is_ge), [elm2, tp2], [oh2])
        P.op('dve', lambda e: e.tensor_tensor(out=tp2.ap(), in0=tp2.ap(), in1=tp1.ap(), op=ALU.subtract), [tp2, tp1], [tp2])
        P.op('act', lambda e: e.activation(out=tp2.ap(), in_=tp2.ap(), func=AF.Exp), [tp2], [tp2])
        P.op('dve', lambda e: e.tensor_scalar(out=tp1.ap(), in0=tp2.ap(), scalar1=1.0, scalar2=None, op0=ALU.add), [tp2], [tp1])
        P.op('dve', lambda e: e.reciprocal(out=tp1.ap(), in_=tp1.ap()), [tp1], [tp1])
        P.op('dve', lambda e: e.tensor_tensor(out=w1g.ap(), in0=tp1.ap(), in1=gw.ap(), op=ALU.mult), [tp1, gw], [w1g])
        P.op('dve', lambda e: e.tensor_tensor(out=w2g.ap(), in0=w1g.ap(), in1=tp2.ap(), op=ALU.mult), [w1g, tp2], [w2g])
        P.op('dve', lambda e: e.tensor_tensor(out=oh1.ap(), in0=oh1.ap(), in1=bc32(w1g), op=ALU.mult), [oh1, w1g], [oh1])
        P.op('dve', lambda e: e.tensor_tensor(out=oh2.ap(), in0=oh2.ap(), in1=bc32(w2g), op=ALU.mult), [oh2, w2g], [oh2])
        P.op('dve', lambda e: e.tensor_tensor(out=cw.ap(), in0=oh1.ap(), in1=oh2.ap(), op=ALU.add), [oh1, oh2], [cw])
        P.pop()

        NW = 2
        W1 = [P.sbuf(f"W1_{i}", [128, 8, 256], BF16) for i in range(NW)]
        W3 = [P.sbuf(f"W3_{i}", [128, 8, 256], BF16) for i in range(NW)]
        W2 = [P.sbuf(f"W2_{i}", [128, 2, D], BF16) for i in range(NW)]
        gT = [[P.sbuf(f"gT_{i}_{q}", [128, 2, 512], BF16) for q in range(4)] for i in range(2)]
        sl = [P.sbuf(f"sl_{i}", [128, 512], BF16) for i in range(2)]
        ph1 = [P.psum(f"ph1_{i}", [128, 512], F32, i * BANK) for i in range(2)]
        ph3 = [P.psum(f"ph3_{i}", [128, 512], F32, (2 + i) * BANK) for i in range(2)]
        py = [P.psum(f"py_{i}", [128, D], F32, (4 + 2 * i) * BANK) for i in range(2)]
        n_exp = 32 if stop_at != 'moe2' else 2
        ci = 0
        yi = [0]

        def emit_y(ex, wi, tq_, tiles):
            g_ = gT[ex % 2][tq_]
            for t4 in tiles:
                tt = tq_ * 4 + t4
                pp = py[yi[0] % 2]
                yi[0] += 1
                for half in range(2):
                    for fc in range(2):
                        P.op('pe', lambda e, pp=pp, half=half, fc=fc, t4=t4, g_=g_, wi=wi: e.matmul(
                            pp.ap()[:, half * 512:(half + 1) * 512], lhsT=g_.ap()[:, fc, t4 * 128:(t4 + 1) * 128],
                            rhs=W2[wi].ap()[:, fc, half * 512:(half + 1) * 512], start=(fc == 0), stop=(fc == 1)), [g_, W2[wi]], [pp])
                P.op('dve', lambda e, pp=pp, tt=tt, ex=ex: e.scalar_tensor_tensor(out=Xt[tt].ap(), in0=pp.ap(), scalar=cw.ap()[:, tt, ex:ex + 1],
                                                                               in1=Xt[tt].ap(), op0=ALU.mult, op1=ALU.add), [pp, cw, Xt[tt]], [Xt[tt]])

        for ex in range(n_exp):
            wi = ex % NW
            P.dma('pool', W1[wi].ap(), w1_d[l, ex].rearrange("(c p) n -> p c n", p=128), [], [W1[wi]])
            P.dma('pool', W3[wi].ap(), w3_d[l, ex].rearrange("(c p) n -> p c n", p=128), [], [W3[wi]])
            P.dma('pool', W2[wi].ap(), w2_d[l, ex].rearrange("(c p) n -> p c n", p=128), [], [W2[wi]])
            P.op('pool', lambda e, wi=wi: e.tensor_tensor(out=W2[wi].ap(), in0=W2[wi].ap(), in1=gab[1].ap().unsqueeze(1).to_broadcast([128, 2, D]),
                                                          op=ALU.mult), [W2[wi], gab[1]], [W2[wi]])
            for tq in range(4):
                g_ = gT[ex % 2][tq]
                for fc in range(2):
                    p1, p3, s_ = ph1[ci % 2], ph3[ci % 2], sl[ci % 2]
                    ci += 1
                    for k in range(8):
                        P.op('pe', lambda e, p1=p1, k=k, fc=fc, tq=tq, wi=wi: e.matmul(p1.ap(), lhsT=W1[wi].ap()[:, k, fc * 128:(fc + 1) * 128],
                                                                                     rhs=hT.ap()[:, k, tq * 512:(tq + 1) * 512], start=(k == 0), stop=(k == 7)),
                             [W1[wi], hT], [p1])
                    for k in range(8):
                        P.op('pe', lambda e, p3=p3, k=k, fc=fc, tq=tq, wi=wi: e.matmul(p3.ap(), lhsT=W3[wi].ap()[:, k, fc * 128:(fc + 1) * 128],
                                                                                     rhs=hT.ap()[:, k, tq * 512:(tq + 1) * 512], start=(k == 0), stop=(k == 7)),
                             [W3[wi], hT], [p3])
                    if tq > 0:
                        emit_y(ex, wi, tq - 1, (2 * fc, 2 * fc + 1))
                    elif ex > 0:
                        emit_y(ex - 1, (ex - 1) % NW, 3, (2 * fc, 2 * fc + 1))
                    P.op('act', lambda e, p1=p1, s_=s_: e.activation(out=s_.ap(), in_=p1.ap(), func=AF.Silu), [p1], [s_])
                    P.op('dve', lambda e, p3=p3, s_=s_, g_=g_, fc=fc: e.tensor_tensor(out=g_.ap()[:, fc, :], in0=s_.ap(), in1=p3.ap(),
                                                                                   op=ALU.mult), [s_, p3], [g_])
            if ex == n_exp - 1:
                emit_y(ex, wi, 3, (0, 1, 2, 3))
        P.pop()
        if stop_at in ('l0', 'moe2') and l == stop_layer:
            break

    ov = out_d.rearrange("(tt p) d -> p tt d", p=128)
    if stop_at == 'n1':
        for c in range(8):
            P.op('dve', lambda e, c=c: e.tensor_copy(out=X.ap()[:, c, :].rearrange("p (a b) -> p a b", a=1)[:, 0, :], in_=hT.ap()[:, c, 0:1024]), [hT], [X])
            P.op('dve', lambda e, c=c: e.tensor_copy(out=X.ap()[:, 8 + c, :], in_=hT.ap()[:, c, 1024:2048]), [hT], [X])
    for t in range(NT):
        P.dma('sp', ov[:, t, :], Xt[t].ap(), [Xt[t]], [out_buf])
    P.finish([out_buf] + extra_final)
    print("max sbuf top", P.max_top, "n_dma", P.n_dma, {e: len(P.ops[e]) for e in ENGINES})
    return nc


_CACHE = {}


def make_in_maps(inputs):
    consts = host_consts()
    f = lambda a: np.ascontiguousarray(np.asarray(a, dtype=np.float32))
    L = DEPTH
    shared = {
        'w_mod': f(inputs['w_mod']), 'b_mod': f(inputs['b_mod']), 'g_norm1': f(inputs['g_norm1']),
        'w_in': f(inputs['w_in']), 'gq_a': f(inputs['gq_a']), 'gk_a': f(inputs['gk_a']),
        'lam_a': f(inputs['lam_a']).reshape(L, 256), 'g_sub_a': f(inputs['g_sub_a']),
        'w_pool': f(inputs['w_pool']), 'b_pool': f(inputs['b_pool']).reshape(L, 256),
        'pool_scale': f(inputs['pool_scale']), 'gq_c': f(inputs['gq_c']), 'gk_c': f(inputs['gk_c']),
        'w_out': f(inputs['w_out']), 'g_norm2': f(inputs['g_norm2']), 'w_rg': f(inputs['w_rg']),
        'b_rg': f(inputs['b_rg']), 'w_re': f(inputs['w_re']), 'b_re': f(inputs['b_re']).reshape(L, 32),
        'w1': f(inputs['w1']).reshape(L, 32, D, 256), 'w3': f(inputs['w3']).reshape(L, 32, D, 256),
        'w2': f(inputs['w2']).reshape(L, 32, 256, D),
    }
    shared.update(consts)
    x = f(inputs['x'])
    c = f(inputs['c'])
    maps = []
    for b in range(8):
        m = dict(shared)
        m['x'] = np.ascontiguousarray(x[b])
        m['c_col'] = np.ascontiguousarray(c[b].reshape(8, 128).T)
        maps.append(m)
    return maps


FUSED = True


def kernel(**inputs):
    in_maps = make_in_maps(inputs)
    if FUSED:
        if 'nc' not in _CACHE:
            _CACHE['nc'] = build_program()
        res = run_bass_kernel_spmd(_CACHE['nc'], in_maps, core_ids=list(range(8)))
        return np.stack([np.asarray(r["out"], dtype=np.float32) for r in res.results], axis=0)
    for l in range(DEPTH):
        if ('nc', l) not in _CACHE:
            _CACHE[('nc', l)] = build_program(layers=(l,))
        res = run_bass_kernel_spmd(_CACHE[('nc', l)], in_maps, core_ids=list(range(8)))
        outs = [np.asarray(r["out"], dtype=np.float32) for r in res.results]
        for b in range(8):
            in_maps[b]['x'] = outs[b]
    return np.stack(outs, axis=0)
```

```python
import contextlib
import numpy as np
import ml_dtypes
import concourse.bass as bass
import concourse.mybir as mybir
from concourse.bass_utils import run_bass_kernel_spmd

F32 = mybir.dt.float32
BF16 = mybir.dt.bfloat16
AF = mybir.ActivationFunctionType
ALU = mybir.AluOpType
AX = mybir.AxisListType

ENGINES = ('sp', 'act', 'dve', 'pool', 'pe')
N_DMA_SEMS = 80
DT_SIZE = {F32: 4, BF16: 2}


class Buf:
    def __init__(self, name, kind, view, lo=0, hi=0):
        self.name = name
        self.kind = kind
        self.view = view
        self.lo, self.hi = lo, hi
        self.W = {}
        self.R = {}

    def ap(self):
        return self.view


class Op:
    __slots__ = ('eng', 'fn', 'deps', 'marked', 'done_key', 'done_val', 'is_dma', 'pre_wait', 'idx')

    def __init__(self, eng, fn, is_dma):
        self.eng = eng
        self.fn = fn
        self.deps = {}
        self.marked = False
        self.done_key = None
        self.done_val = None
        self.is_dma = is_dma
        self.pre_wait = None


class Prog:
    def __init__(self, nc, sbuf_bytes=200 * 1024):
        self.nc = nc
        self.ops = {e: [] for e in ENGINES}
        self.n_dma = 0
        self.dma_ops = []
        self.sbuf_bytes = sbuf_bytes
        self.sb_handle = nc.alloc_sbuf_tensor("sb_all", [128, sbuf_bytes // 4], F32)
        self.ps_handle = nc.alloc_psum_tensor("ps_all", [128, 4096], F32)
        self.sb_top = 0
        self.scopes = []
        self.live = {'sbuf': [], 'psum': []}
        self.retired = {'sbuf': [], 'psum': []}
        self.max_top = 0

    def _mk(self, name, kind, lo, shape, dtype):
        esz = DT_SIZE[dtype]
        n = int(np.prod(shape[1:]))
        nbytes = n * esz
        hi = lo + nbytes
        base = self.sb_handle if kind == 'sbuf' else self.ps_handle
        assert lo % 4 == 0
        v = base.ap()[0:shape[0], lo // 4:(lo + ((nbytes + 3) // 4) * 4) // 4]
        if dtype != F32:
            v = v.bitcast(dtype)
            v = v[:, 0:n]
        if len(shape) > 2:
            names = [f"d{i}" for i in range(len(shape) - 1)]
            kw = {nm: s for nm, s in zip(names[:-1], shape[1:-1])}
            v = v.rearrange("p (" + " ".join(names) + ") -> p " + " ".join(names), **kw)
        b = Buf(name, kind, v, lo, hi)
        for o in self.live[kind]:
            assert o.hi <= lo or o.lo >= hi, f"alias live {name} vs {o.name}"
        for o in self.retired[kind]:
            if not (o.hi <= lo or o.lo >= hi):
                for k, op in list(o.W.items()) + list(o.R.items()):
                    if k not in b.R or b.R[k].idx < op.idx:
                        b.R[k] = op
        self.live[kind].append(b)
        return b

    def sbuf(self, name, shape, dtype):
        lo = (self.sb_top + 63) // 64 * 64
        b = self._mk(name, 'sbuf', lo, shape, dtype)
        self.sb_top = b.hi
        self.max_top = max(self.max_top, self.sb_top)
        assert self.sb_top <= self.sbuf_bytes, f"SBUF overflow at {name}: {self.sb_top}"
        if self.scopes:
            self.scopes[-1][1].append(b)
        return b

    def psum(self, name, shape, dtype, byte_off):
        b = self._mk(name, 'psum', byte_off, shape, dtype)
        if self.scopes:
            self.scopes[-1][2].append(b)
        return b

    def dram_buf(self, name):
        return Buf(name, 'dram', None)

    def push(self):
        self.scopes.append((self.sb_top, [], []))

    def pop(self):
        top, sb, ps = self.scopes.pop()
        for b in sb:
            self.live['sbuf'].remove(b)
            self.retired['sbuf'].append(b)
        for b in ps:
            self.live['psum'].remove(b)
            self.retired['psum'].append(b)
        self.sb_top = top

    def free_psum(self, bufs):
        for b in bufs:
            self.live['psum'].remove(b)
            self.retired['psum'].append(b)
            for sc in self.scopes:
                if b in sc[2]:
                    sc[2].remove(b)

    def _track(self, op, reads, writes):
        op.idx = self._next_idx = getattr(self, '_next_idx', 0) + 1
        deps = op.deps

        def add(p):
            if p is op:
                return
            if p.eng == 'pe' and op.eng == 'pe' and not p.is_dma and not op.is_dma:
                return
            k = p.done_key
            if k not in deps or deps[k].idx < p.idx:
                deps[k] = p

        for b in reads:
            for p in b.W.values():
                add(p)
        for b in writes:
            for p in b.W.values():
                add(p)
            for p in b.R.values():
                add(p)
        for p in deps.values():
            p.marked = True
        for b in reads:
            b.R[op.done_key] = op
        for b in writes:
            if b.R:
                b.R = {}
                b.W = {}
            b.W[op.done_key] = op

    def op(self, eng, fn, reads, writes):
        o = Op(eng, fn, False)
        o.done_key = ('e', eng)
        self._track(o, reads, writes)
        self.ops[eng].append(o)
        return o

    def barrier(self):
        lasts = {}
        for e in ENGINES:
            for o in reversed(self.ops[e]):
                if o.fn is not None and not o.is_dma:
                    lasts[o.done_key] = o
                    break
        for o in self.dma_ops:
            k = o.done_key
            if k not in lasts or lasts[k].idx < o.idx:
                lasts[k] = o
        for p in lasts.values():
            p.marked = True
        for e in ENGINES:
            o = Op(e, None, False)
            o.done_key = ('e', e)
            o.idx = self._next_idx = getattr(self, '_next_idx', 0) + 1
            o.deps = {k: p for k, p in lasts.items() if not (k == ('e', e))}
            self.ops[e].append(o)

    def dma(self, eng, out, in_, reads, writes, **kw):
        o = Op(eng, lambda e: e.dma_start(out=out, in_=in_, **kw), True)
        i = self.n_dma
        self.n_dma += 1
        slot = i % N_DMA_SEMS
        o.done_key = ('d', slot)
        o.done_val = 16 * (i // N_DMA_SEMS + 1)
        if i >= N_DMA_SEMS:
            o.pre_wait = (('d', slot), 16 * (i // N_DMA_SEMS))
        self._track(o, reads, writes)
        self.ops[eng].append(o)
        self.dma_ops.append(o)
        return o

    def finish(self, final_bufs):
        nc = self.nc
        for e in ENGINES:
            c = 0
            for o in self.ops[e]:
                if o.is_dma or o.fn is None:
                    continue
                if o.marked:
                    c += 1
                    o.done_val = c
        final_waits = {}
        for b in final_bufs:
            for k, p in b.W.items():
                final_waits[k] = max(final_waits.get(k, 0), p.done_val)
        with contextlib.ExitStack() as st:
            sems = {}
            for e in ENGINES:
                sems[('e', e)] = st.enter_context(nc.semaphore(f"s_{e}"))
            for i in range(min(N_DMA_SEMS, max(1, self.n_dma))):
                sems[('d', i)] = st.enter_context(nc.semaphore(f"s_d{i}"))
            block = st.enter_context(nc.Block())

            def emit(ename, eng):
                waited = {}
                for o in self.ops[ename]:
                    need = {}
                    for k, p in o.deps.items():
                        assert p.done_val is not None, "dep not numbered"
                        need[k] = max(need.get(k, 0), p.done_val)
                    if o.pre_wait is not None:
                        k, v = o.pre_wait
                        need[k] = max(need.get(k, 0), v)
                    for k, v in need.items():
                        if waited.get(k, 0) >= v:
                            continue
                        eng.wait_ge(sems[k], v)
                        waited[k] = v
                    if o.fn is None:
                        continue
                    ins = o.fn(eng)
                    if o.is_dma:
                        ins.then_inc(sems[o.done_key], 16)
                    elif o.marked:
                        ins.then_inc(sems[o.done_key], 1)
                if ename == 'sp':
                    for k, v in final_waits.items():
                        if waited.get(k, 0) < v:
                            eng.wait_ge(sems[k], v)

            @block.sync
            def _(eng):
                emit('sp', eng)

            @block.scalar
            def _(eng):
                emit('act', eng)

            @block.vector
            def _(eng):
                emit('dve', eng)

            @block.gpsimd
            def _(eng):
                emit('pool', eng)

            @block.tensor
            def _(eng):
                emit('pe', eng)


S = 2048
D = 1024
NT = S // 128
DEPTH = 2
NEG = -30000.0
FENCE_M = False
BARRIERS = False
EPS = 1e-6
WINDOWS = (2, 4, 8, 16)


def host_consts():
    c = {}
    c['identf'] = np.eye(128, dtype=np.float32)
    c['identb'] = np.eye(128, dtype=np.float32).astype(ml_dtypes.bfloat16)
    inv = 1.0 / (10000.0 ** (np.arange(0, 64, 2, dtype=np.float32) / 64.0))
    ang = np.arange(S, dtype=np.float32)[:, None] * inv[None, :]
    cos = np.cos(ang).astype(np.float32)
    sin = np.sin(ang).astype(np.float32)
    def tm(a):
        return np.ascontiguousarray(a.reshape(NT, 128, 32).transpose(1, 0, 2))
    c['rope'] = np.ascontiguousarray(np.stack([tm(cos), tm(-sin), tm(sin)], axis=2))
    ki = np.arange(128)[:, None]
    cc = np.arange(896)[None, :]
    delta = cc - 384 - ki
    c['cstrip'] = np.where(delta >= 0, 0.0, NEG).astype(np.float32).astype(ml_dtypes.bfloat16)
    cc = np.arange(2432)[None, :]
    delta = cc - 384 - ki
    mult = ((delta >= 0) & (delta <= 128)).astype(np.int32) \
        + ((delta >= 0) & (delta <= 512) & (delta % 4 == 0)).astype(np.int32) \
        + ((delta >= 0) & (delta % 16 == 0)).astype(np.int32)
    c['dstrip'] = mult.astype(np.float32).astype(ml_dtypes.bfloat16)
    c['tri01'] = (np.arange(128)[None, :] >= np.arange(128)[:, None]).astype(np.float32).astype(ml_dtypes.bfloat16)
    c['invtab'] = np.tile((1.0 / (np.arange(16, dtype=np.float32) + 1.0))[None, :], (128, 1)).astype(np.float32)
    return c


def build_program(stop_at=None, layers=(0, 1)):
    nc = bass.Bass("TRN2", target_bir_lowering=False)
    L = DEPTH
    stop_layer = 0
    if stop_at is not None and '@' in stop_at:
        stop_at, sl_ = stop_at.split('@')
        stop_layer = int(sl_)

    def din(name, shape, dt=F32):
        return nc.dram_tensor(name, list(shape), dt, kind="ExternalInput").ap()

    x_d = din("x", [S, D])
    ccol_d = din("c_col", [128, 8])
    w_mod_d = din("w_mod", [L, D, 6 * D])
    b_mod_d = din("b_mod", [L, 6 * D])
    g1_d = din("g_norm1", [L, D])
    w_in_d = din("w_in", [L, D, 2560])
    gqa_d = din("gq_a", [L, 64])
    gka_d = din("gk_a", [L, 64])
    lam_d = din("lam_a", [L, 256])
    gsub_d = din("g_sub_a", [L, 128])
    wpool_d = din("w_pool", [L, 4, 64, 64])
    bpool_d = din("b_pool", [L, 256])
    pscale_d = din("pool_scale", [L, 256])
    gqc_d = din("gq_c", [L, 64])
    gkc_d = din("gk_c", [L, 64])
    w_out_d = din("w_out", [L, D, D])
    g2_d = din("g_norm2", [L, D])
    w_rg_d = din("w_rg", [L, D, 4])
    b_rg_d = din("b_rg", [L, 4])
    w_re_d = din("w_re", [L, 4, D, 8])
    b_re_d = din("b_re", [L, 32])
    if stop_at in ('n1', 'pool', 'attC', 'mix') and stop_layer == 0:
        w1_d = w3_d = w2_d = None
    else:
        w1_d = din("w1", [L, 32, D, 256])
        w3_d = din("w3", [L, 32, D, 256])
        w2_d = din("w2", [L, 32, 256, D])
    identf_d = din("identf", [128, 128])
    identb_d = din("identb", [128, 128], BF16)
    rope_d = din("rope", [128, NT, 3, 32])
    cstrip_d = din("cstrip", [128, 896], BF16)
    dstrip_d = din("dstrip", [128, 2432], BF16)
    invtab_d = din("invtab", [128, 16])
    tri01_d = din("tri01", [128, 128], BF16)
    out_d = nc.dram_tensor("out", [S, D], F32, kind="ExternalOutput").ap()
    dbg_d = nc.dram_tensor("dbg", [128, 2048], F32, kind="ExternalOutput").ap() if stop_at == 'pool' else None

    P = Prog(nc, sbuf_bytes=206 * 1024)
    out_buf = P.dram_buf("out")
    extra_final = []

    Xt = [P.sbuf(f"X{t}", [128, D], F32) for t in range(NT)]
    X = None
    identf = P.sbuf("identf", [128, 128], F32)
    identb = P.sbuf("identb", [128, 128], BF16)
    rope = P.sbuf("rope", [128, NT, 3, 32], F32)
    cstrip = P.sbuf("cstrip", [128, 896], BF16)
    dstrip = P.sbuf("dstrip", [128, 2432], BF16)
    invtab = P.sbuf("invtab", [128, 16], F32)
    tri01 = P.sbuf("tri01", [128, 128], BF16)
    condrep = P.sbuf("condrep", [128, 8, 128], BF16)
    Acol = [P.sbuf(f"Acol{i}", [128, 8], F32) for i in range(2)]
    Bcol = [P.sbuf(f"Bcol{i}", [128, 8], F32) for i in range(2)]
    gab = [P.sbuf(f"gab{i}", [128, D], F32) for i in range(2)]
    ones_col = P.sbuf("ones_col", [128, 1], F32)
    zerob = P.sbuf("zerob", [128, 128], BF16)

    xv = x_d.rearrange("(tt p) d -> p tt d", p=128)
    for t in range(NT):
        P.dma('sp', Xt[t].ap(), xv[:, t, :], reads=[], writes=[Xt[t]])
    P.dma('sp', identf.ap(), identf_d, [], [identf])
    P.dma('sp', identb.ap(), identb_d, [], [identb])
    P.dma('sp', rope.ap(), rope_d, [], [rope])
    P.dma('sp', cstrip.ap(), cstrip_d, [], [cstrip])
    P.dma('sp', dstrip.ap(), dstrip_d, [], [dstrip])
    P.dma('sp', invtab.ap(), invtab_d, [], [invtab])
    P.dma('sp', tri01.ap(), tri01_d, [], [tri01])
    P.op('pool', lambda e: e.memset(ones_col.ap(), 1.0), [], [ones_col])
    P.op('pool', lambda e: e.memset(zerob.ap(), 0.0), [], [zerob])

    P.push()
    ccol = P.sbuf("ccol", [128, 8], F32)
    ctmp = P.sbuf("ctmp", [128, 8], F32)
    P.dma('sp', ccol.ap(), ccol_d, [], [ccol])
    P.op('act', lambda e: e.activation(out=ctmp.ap(), in_=ccol.ap(), func=AF.Exp, scale=-1.0), [ccol], [ctmp])
    P.op('dve', lambda e: e.tensor_scalar(out=ctmp.ap(), in0=ctmp.ap(), scalar1=1.0, scalar2=None, op0=ALU.add), [ctmp], [ctmp])
    P.op('dve', lambda e: e.reciprocal(out=ctmp.ap(), in_=ctmp.ap()), [ctmp], [ctmp])
    P.op('dve', lambda e: e.tensor_tensor(out=ctmp.ap(), in0=ctmp.ap(), in1=ccol.ap(), op=ALU.mult), [ctmp, ccol], [ctmp])
    P.op('dve', lambda e: e.tensor_copy(out=condrep.ap(), in_=ctmp.ap().unsqueeze(2).to_broadcast([128, 8, 128])),
         [ctmp], [condrep])
    P.pop()

    BANK = 2048
    rr = {'evac': 0}

    def evac_engine():
        rr['evac'] += 1
        return 'act' if rr['evac'] % 2 == 0 else 'dve'

    def affine_evac(eng, out_ap, in_ap, sc_ap, bi_ap, reads, writes):
        if eng == 'act':
            P.op('act', lambda e: e.activation(out=out_ap, in_=in_ap, func=AF.Identity, bias=bi_ap, scale=sc_ap), reads, writes)
        else:
            P.op('dve', lambda e: e.tensor_scalar(out=out_ap, in0=in_ap, scalar1=sc_ap, scalar2=bi_ap, op0=ALU.mult, op1=ALU.add), reads, writes)

    def rstd_from_ss(ss_buf, n, tmp_buf=None):
        P.op('act', lambda e: e.activation(out=ss_buf.ap(), in_=ss_buf.ap(), func=AF.Ln, scale=1.0 / n, bias=EPS), [ss_buf], [ss_buf])
        P.op('act', lambda e: e.activation(out=ss_buf.ap(), in_=ss_buf.ap(), func=AF.Exp, scale=-0.5), [ss_buf], [ss_buf])

    for l in layers:
        if BARRIERS and l != layers[0]:
            P.barrier()
        P.push()
        wmb = [P.sbuf(f"wmb{i}", [128, 8, 512], BF16) for i in range(2)]
        bmb = [P.sbuf(f"bmb{i}", [128, 512], F32) for i in range(2)]
        gnb = [P.sbuf(f"gnb{i}", [128, 512], F32) for i in range(2)]
        modblk = [P.sbuf(f"modblk{i}", [128, 512], F32) for i in range(2)]
        dtmp = P.sbuf("dtmp", [128, 4, 128], F32)
        pm = [P.psum(f"pm{i}", [128, 512], F32, i * BANK) for i in range(2)]
        for j in range(12):
            v, half = j // 2, j % 2
            wb, bb, gb, mb, pp = wmb[j % 2], bmb[j % 2], gnb[j % 2], modblk[j % 2], pm[j % 2]
            P.dma('pool', wb.ap(), w_mod_d[l, :, j * 512:(j + 1) * 512].rearrange("(c p) n -> p c n", p=128), [X] if FENCE_M else [], [wb])
            P.dma('sp', bb.ap(), b_mod_d[l, j * 512:(j + 1) * 512].partition_broadcast(128), [X] if FENCE_M else [], [bb])
            for k in range(8):
                P.op('pe', lambda e, k=k, pp=pp, wb=wb: e.matmul(pp.ap(), lhsT=condrep.ap()[:, k, :], rhs=wb.ap()[:, k, :],
                                                               start=(k == 0), stop=(k == 7)), [condrep, wb], [pp])
            sub = 0 if v < 3 else 1
            vv = v % 3
            if vv == 2:
                dst = gab[sub].ap()[:, half * 512:(half + 1) * 512]
                P.op('dve', lambda e, dst=dst, pp=pp, bb=bb: e.tensor_tensor(out=dst, in0=pp.ap(), in1=bb.ap(), op=ALU.add),
                     [pp, bb], [gab[sub]])
                continue
            P.op('dve', lambda e, mb=mb, pp=pp, bb=bb: e.tensor_tensor(out=mb.ap(), in0=pp.ap(), in1=bb.ap(), op=ALU.add), [pp, bb], [mb])
            if vv == 1:
                gsrc = (g1_d if sub == 0 else g2_d)[l, half * 512:(half + 1) * 512].partition_broadcast(128)
                P.dma('sp', gb.ap(), gsrc, [], [gb])
                P.op('dve', lambda e, mb=mb, gb=gb: e.scalar_tensor_tensor(out=mb.ap(), in0=mb.ap(), scalar=1.0, in1=gb.ap(),
                                                                          op0=ALU.add, op1=ALU.mult), [mb, gb], [mb])
                dstb = Acol[sub]
            else:
                dstb = Bcol[sub]
            P.op('dve', lambda e, mb=mb: e.tensor_tensor(out=dtmp.ap(), in0=mb.ap().rearrange("p (c j) -> p c j", c=4),
                                                        in1=identf.ap().unsqueeze(1).to_broadcast([128, 4, 128]), op=ALU.mult),
                 [mb, identf], [dtmp])
            P.op('dve', lambda e, dstb=dstb, half=half: e.reduce_sum(out=dstb.ap()[:, half * 4:(half + 1) * 4], in_=dtmp.ap(), axis=AX.X),
                 [dtmp], [dstb])
        P.pop()

        if stop_at == 'M' and l == stop_layer:
            break
        if BARRIERS:
            P.barrier()

        def norm_to_hT(hT, sub, extra_scope_bufs):
            ssq, junk, diag, pn = extra_scope_bufs
            for tt in range(NT):
                P.op('act', lambda e, tt=tt: e.activation(out=junk.ap(), in_=Xt[tt].ap(), func=AF.Square,
                                                         accum_out=ssq.ap()[:, tt:tt + 1]), [Xt[tt]], [junk, ssq])
            rstd_from_ss(ssq, D)
            for tt in range(NT):
                P.op('dve', lambda e, tt=tt: e.tensor_scalar(out=diag.ap()[:, tt, :], in0=identf.ap(), scalar1=ssq.ap()[:, tt:tt + 1],
                                                            scalar2=None, op0=ALU.mult), [identf, ssq], [diag])
            i = 0
            for tq in range(4):
                for c in range(8):
                    pp = pn[i % 2]
                    i += 1
                    for t4 in range(4):
                        tt = tq * 4 + t4
                        P.op('pe', lambda e, pp=pp, tt=tt, c=c, t4=t4: e.matmul(pp.ap()[:, t4 * 128:(t4 + 1) * 128],
                                                                              lhsT=Xt[tt].ap()[:, c * 128:(c + 1) * 128],
                                                                              rhs=diag.ap()[:, tt, :], start=True, stop=True),
                             [Xt[tt], diag], [pp])
                    affine_evac(evac_engine(), hT.ap()[:, c, tq * 512:(tq + 1) * 512], pp.ap(),
                                Acol[sub].ap()[:, c:c + 1], Bcol[sub].ap()[:, c:c + 1], [pp, Acol[sub], Bcol[sub]], [hT])

        P.push()
        hT = P.sbuf("hT", [128, 8, S], BF16)
        P.push()
        ssq = P.sbuf("ssq", [128, NT], F32)
        junk = P.sbuf("junk", [128, D], BF16)
        diag = P.sbuf("diag", [128, NT, 128], F32)
        pn = [P.psum(f"pn{i}", [128, 512], F32, i * BANK) for i in range(2)]
        norm_to_hT(hT, 0, (ssq, junk, diag, pn))
        if stop_at == 'pool' and l == stop_layer:
            dbgb = P.dram_buf("dbg")
            scr = P.sbuf("scr", [128, 2048], F32)
            P.op('dve', lambda e: e.tensor_copy(out=scr.ap()[:, 0:1024], in_=X.ap()[:, 3, :]), [X], [scr])
            P.op('dve', lambda e: e.tensor_copy(out=scr.ap()[:, 1024:1032], in_=Acol[0].ap()), [Acol[0]], [scr])
            P.op('dve', lambda e: e.tensor_copy(out=scr.ap()[:, 1032:1040], in_=Bcol[0].ap()), [Bcol[0]], [scr])
            P.op('dve', lambda e: e.tensor_copy(out=scr.ap()[:, 1040:1056], in_=ssq.ap()), [ssq], [scr])
            P.op('dve', lambda e: e.tensor_copy(out=scr.ap()[:, 1536:2048], in_=hT.ap()[:, 0, 0:512]), [hT], [scr])
            P.dma('sp', dbg_d, scr.ap(), [scr], [dbgb])
            extra_final.append(dbgb)
        P.pop()

        if stop_at == 'n1' and l == stop_layer:
            break
        if BARRIERS:
            P.barrier()

        lam_init = 0.8 - 0.6 * float(np.exp(-0.3 * l))

        gq = {}
        for nm, src in (('qa', gqa_d), ('ka', gka_d), ('qc', gqc_d), ('kc', gkc_d)):
            gq[nm] = P.sbuf("g_" + nm, [128, 64], F32)
            P.dma('sp', gq[nm].ap(), src[l, :].partition_broadcast(128), [], [gq[nm]])
        gsub = P.sbuf("gsub", [128, 128], F32)
        P.dma('sp', gsub.ap(), gsub_d[l, :].partition_broadcast(128), [], [gsub])
        P.op('dve', lambda e, gsub=gsub, li=lam_init: e.tensor_scalar(out=gsub.ap(), in0=gsub.ap(), scalar1=1.0 - li, scalar2=None, op0=ALU.mult),
             [gsub], [gsub])
        lamt = P.sbuf("lamt", [128, 256], F32)
        lamv = P.sbuf("lamv", [128, 2], F32)
        neglam = P.sbuf("neglam", [128, 1], F32)
        P.dma('sp', lamt.ap(), lam_d[l, :].partition_broadcast(128), [], [lamt])
        lv = lamt.ap().rearrange("p (a d) -> p a d", a=4)
        P.op('dve', lambda e: e.tensor_tensor(out=lv[:, 0, :], in0=lv[:, 0, :], in1=lv[:, 1, :], op=ALU.mult), [lamt], [lamt])
        P.op('dve', lambda e: e.tensor_tensor(out=lv[:, 2, :], in0=lv[:, 2, :], in1=lv[:, 3, :], op=ALU.mult), [lamt], [lamt])
        P.op('dve', lambda e: e.reduce_sum(out=lamv.ap()[:, 0:1], in_=lv[:, 0, :], axis=AX.X), [lamt], [lamv])
        P.op('dve', lambda e: e.reduce_sum(out=lamv.ap()[:, 1:2], in_=lv[:, 2, :], axis=AX.X), [lamt], [lamv])
        P.op('act', lambda e: e.activation(out=lamv.ap(), in_=lamv.ap(), func=AF.Exp), [lamv], [lamv])
        P.op('dve', lambda e: e.tensor_tensor(out=neglam.ap(), in0=lamv.ap()[:, 1:2], in1=lamv.ap()[:, 0:1], op=ALU.subtract), [lamv], [neglam])
        P.op('dve', lambda e, neglam=neglam, li=lam_init: e.tensor_scalar(out=neglam.ap(), in0=neglam.ap(), scalar1=-li, scalar2=None, op0=ALU.add), [neglam], [neglam])

        def load_w_in_block(dst, col0, ncols):
            P.dma('pool', dst.ap(), w_in_d[l, :, col0:col0 + ncols].rearrange("(c p) n -> p c n", p=128), [], [dst])

        def load_wout_rows(dst, row0):
            P.dma('pool', dst.ap(), w_out_d[l, row0:row0 + 256, :].rearrange("(c p) n -> p c n", p=128), [], [dst])
            P.op('pool', lambda e: e.tensor_tensor(out=dst.ap(), in0=dst.ap(), in1=gab[0].ap().unsqueeze(1).to_broadcast([128, 2, D]),
                                                   op=ALU.mult), [dst, gab[0]], [dst])

        def wout_accumulate(lhs_fn, lhs_bufs, wo_sb, tt, pw):
            for half in range(2):
                pp = pw[half]
                for c in range(2):
                    P.op('pe', lambda e, pp=pp, c=c, half=half: e.matmul(pp.ap(), lhsT=lhs_fn(c), rhs=wo_sb.ap()[:, c, half * 512:(half + 1) * 512],
                                                                         start=(c == 0), stop=(c == 1)), lhs_bufs + [wo_sb], [pp])
                xs = Xt[tt].ap()[:, half * 512:(half + 1) * 512]
                P.op('dve', lambda e, xs=xs, pp=pp: e.tensor_tensor(out=xs, in0=xs, in1=pp.ap(), op=ALU.add), [Xt[tt], pp], [Xt[tt]])

        P.push()
        ubT = P.sbuf("ubT", [128, 2, S], F32)
        wblk = P.sbuf("wblk_p", [128, 8, 256], BF16)
        wo_sb = P.sbuf("wo_p", [128, 2, D], BF16)
        pA = P.sbuf("poolA", [128, S], F32)
        pB = P.sbuf("poolB", [128, S], F32)
        dT = P.sbuf("dT", [128, 2, S], BF16)
        ypT = P.sbuf("ypT", [128, 2, S], BF16)
        wpd = P.sbuf("wpd", [128, 2, 128], F32)
        wpdb = P.sbuf("wpdb", [128, 2, 128], BF16)
        psb = P.sbuf("psb", [128, 256], F32)
        bcol = P.sbuf("bcol", [128, 2], F32)
        scol = P.sbuf("scol", [128, 2], F32)
        pz = [P.psum(f"pz{i}", [128, 512], F32, i * BANK) for i in range(2)]
        pw = [P.psum(f"pw{i}", [128, 512], F32, (2 + i) * BANK) for i in range(2)]
        load_w_in_block(wblk, 1536, 256)
        load_wout_rows(wo_sb, 512)
        P.op('pool', lambda e: e.memset(wpd.ap(), 0.0), [], [wpd])
        for g in range(4):
            ch, hp = g // 2, (g % 2) * 64
            P.dma('sp', wpd.ap()[hp:hp + 64, ch, hp:hp + 64], wpool_d[l, g], [], [wpd])
        P.dma('sp', psb.ap(), pscale_d[l, :].partition_broadcast(128), [], [psb])
        P.dma('sp', bcol.ap(), bpool_d[l, :].rearrange("(c p) -> p c", p=128), [], [bcol], allow_slow_non_contiguous=True)
        P.dma('sp', scol.ap(), pscale_d[l, :].rearrange("(c p) -> p c", p=128), [], [scol], allow_slow_non_contiguous=True)
        P.op('dve', lambda e: e.tensor_tensor(out=wpdb.ap(), in0=wpd.ap(), in1=psb.ap().rearrange("p (c n) -> p c n", c=2), op=ALU.mult),
             [wpd, psb], [wpdb])
        P.op('dve', lambda e: e.tensor_tensor(out=bcol.ap(), in0=bcol.ap(), in1=scol.ap(), op=ALU.mult), [bcol, scol], [bcol])
        i = 0
        for ch in range(2):
            for tq in range(4):
                pp = pz[i % 2]
                i += 1
                for k in range(8):
                    P.op('pe', lambda e, pp=pp, k=k, ch=ch, tq=tq: e.matmul(pp.ap(), lhsT=wblk.ap()[:, k, ch * 128:(ch + 1) * 128],
                                                                          rhs=hT.ap()[:, k, tq * 512:(tq + 1) * 512],
                                                                          start=(k == 0), stop=(k == 7)), [wblk, hT], [pp])
                dst = ubT.ap()[:, ch, tq * 512:(tq + 1) * 512]
                if i % 2 == 0:
                    P.op('act', lambda e, dst=dst, pp=pp: e.copy(out=dst, in_=pp.ap()), [pp], [ubT])
                else:
                    P.op('dve', lambda e, dst=dst, pp=pp: e.tensor_copy(out=dst, in_=pp.ap()), [pp], [ubT])
        for ch in range(2):
            u = ubT.ap()[:, ch, :]
            seq = [(1, u, pA), (2, pA.ap(), pB)] + ([(4, pB.ap(), pA), (8, pA.ap(), pB)] if ch == 1 else [])
            srcbuf = ubT
            for sh, src, dstb in seq:
                P.op('pool', lambda e, sh=sh, src=src, dstb=dstb: e.tensor_copy(out=dstb.ap()[:, 0:sh], in_=src[:, 0:sh]), [srcbuf], [dstb])
                P.op('pool', lambda e, sh=sh, src=src, dstb=dstb: e.tensor_tensor(out=dstb.ap()[:, sh:S], in0=src[:, sh:S], in1=src[:, 0:S - sh],
                                                                                  op=ALU.add), [srcbuf], [dstb])
                srcbuf = dstb
            for hp, sb_, w in ((0, pA, WINDOWS[2 * ch]), (64, pB, WINDOWS[2 * ch + 1])):
                P.op('dve', lambda e, hp=hp, sb_=sb_, w=w, ch=ch: e.scalar_tensor_tensor(
                    out=dT.ap()[hp:hp + 64, ch, :], in0=sb_.ap()[hp:hp + 64, :], scalar=1.0 / w, in1=ubT.ap()[hp:hp + 64, ch, :],
                    op0=ALU.mult, op1=ALU.subtract), [sb_, ubT], [dT])
                P.op('dve', lambda e, hp=hp, sb_=sb_, w=w: e.tensor_tensor(out=sb_.ap()[hp:hp + 64, 0:w - 1], in0=sb_.ap()[hp:hp + 64, 0:w - 1],
                                                                           in1=invtab.ap()[hp:hp + 64, 0:w - 1], op=ALU.mult), [sb_, invtab], [sb_])
                P.op('dve', lambda e, hp=hp, sb_=sb_, w=w, ch=ch: e.tensor_tensor(out=dT.ap()[hp:hp + 64, ch, 0:w - 1], in0=sb_.ap()[hp:hp + 64, 0:w - 1],
                                                                                  in1=ubT.ap()[hp:hp + 64, ch, 0:w - 1], op=ALU.subtract),
                     [sb_, ubT], [dT])
            for tq in range(4):
                pp = pz[i % 2]
                i += 1
                P.op('pe', lambda e, pp=pp, ch=ch, tq=tq: e.matmul(pp.ap(), lhsT=wpdb.ap()[:, ch, :], rhs=dT.ap()[:, ch, tq * 512:(tq + 1) * 512],
                                                                  start=True, stop=True), [wpdb, dT], [pp])
                P.op('act', lambda e, pp=pp, ch=ch, tq=tq: e.activation(out=ypT.ap()[:, ch, tq * 512:(tq + 1) * 512], in_=pp.ap(), func=AF.Identity,
                                                                       bias=bcol.ap()[:, ch:ch + 1], scale=1.0), [pp, bcol], [ypT])
        for tt in range(NT):
            wout_accumulate(lambda c, tt=tt: ypT.ap()[:, c, tt * 128:(tt + 1) * 128], [ypT], wo_sb, tt, pw)
        P.pop()

        if stop_at == 'pool' and l == stop_layer:
            break

        def attention_group(kind, qcol, kcol, vcol, worow, gqb, gkb):
            P.push()
            nh = 2 if kind == 'A' else 4
            dv = 128 if kind == 'A' else 64
            qT = P.sbuf("qT", [128, 2, 2, S], BF16)
            kT = P.sbuf("kT", [128, 2, S], BF16)
            P.op('pool', lambda e: e.memset(qT.ap(), 0.0), [], [qT])
            vA = P.sbuf("vA", [128, NT, nh, dv + 1], BF16)
            wo_sb = P.sbuf("wo_a", [128, 2, D], BF16)
            load_wout_rows(wo_sb, worow)
            P.op('pool', lambda e: e.memset(vA.ap()[:, :, :, dv:dv + 1], 1.0), [], [vA])

            P.push()
            wq = P.sbuf("wq", [128, 8, 256], BF16)
            wk = P.sbuf("wk", [128, 8, 256], BF16)
            wv = P.sbuf("wv", [128, 8, 256], BF16)
            load_w_in_block(wq, qcol, 256)
            load_w_in_block(wk, kcol, 256)
            load_w_in_block(wv, vcol, 256)
            NB = 4
            sq = [P.sbuf(f"sq{i}", [128, 4, 64], F32) for i in range(NB)]
            ssr = [P.sbuf(f"ssr{i}", [128, 4], F32) for i in range(NB)]
            qg = [P.sbuf(f"qg{i}", [128, 4, 2, 32], F32) for i in range(NB)]
            t1 = [P.sbuf(f"t1{i}", [128, 4, 2, 32], F32) for i in range(NB)]
            t2 = [P.sbuf(f"t2{i}", [128, 4, 2, 32], F32) for i in range(NB)]
            qo = [P.sbuf(f"qo{i}", [128, 4, 64], BF16) for i in range(NB)]
            NPZ = 6
            pz = [P.psum(f"pza{i}", [128, 256], F32, i * BANK) for i in range(NPZ)]
            ptr = [P.psum(f"ptr{i}", [128, 2, 128], BF16, (6 + i) * BANK) for i in range(2)]
            cnt = 0
            pend_tr = []
            n_tr = [0]
            TR_LAG = 2
            for tt in range(NT):
                for which, wsb, gb_, dstT in (('q', wq, gqb, qT), ('k', wk, gkb, kT), ('v', wv, None, None)):
                    pp = pz[cnt % NPZ]
                    for k in range(8):
                        P.op('pe', lambda e, pp=pp, k=k, tt=tt, wsb=wsb: e.matmul(pp.ap(), lhsT=hT.ap()[:, k, tt * 128:(tt + 1) * 128],
                                                                                rhs=wsb.ap()[:, k, :], start=(k == 0), stop=(k == 7)),
                             [hT, wsb], [pp])
                    if which == 'v':
                        P.op('act', lambda e, pp=pp, tt=tt: e.copy(out=vA.ap()[:, tt, :, 0:dv], in_=pp.ap().rearrange("p (h d) -> p h d", h=nh)),
                             [pp], [vA])
                        cnt += 1
                        continue
                    b = cnt % NB
                    s_, r_, g_, a_, b_, o_ = sq[b], ssr[b], qg[b], t1[b], t2[b], qo[b]
                    pv = pp.ap().rearrange("p (h d) -> p h d", h=4)
                    P.op('act', lambda e, s_=s_, pv=pv: e.activation(out=s_.ap(), in_=pv, func=AF.Square), [pp], [s_])
                    P.op('dve', lambda e, s_=s_, r_=r_: e.reduce_sum(out=r_.ap(), in_=s_.ap(), axis=AX.X), [s_], [r_])
                    rstd_from_ss(r_, 64)
                    P.op('dve', lambda e, g_=g_, pv=pv, gb_=gb_: e.tensor_tensor(out=g_.ap().rearrange("p h a d -> p h (a d)"), in0=pv,
                                                                             in1=gb_.ap().unsqueeze(1).to_broadcast([128, 4, 64]), op=ALU.mult),
                         [pp, gb_], [g_])
                    cosb = rope.ap()[:, tt, 0:1, :].unsqueeze(1).to_broadcast([128, 4, 2, 32])
                    P.op('dve', lambda e, a_=a_, g_=g_, cosb=cosb: e.tensor_tensor(out=a_.ap(), in0=g_.ap(), in1=cosb, op=ALU.mult), [g_, rope], [a_])
                    nsin = rope.ap()[:, tt, 1:2, :].to_broadcast([128, 4, 32])
                    psin = rope.ap()[:, tt, 2:3, :].to_broadcast([128, 4, 32])
                    P.op('pool', lambda e, b_=b_, g_=g_, nsin=nsin: e.tensor_tensor(out=b_.ap()[:, :, 0, :], in0=g_.ap()[:, :, 1, :], in1=nsin, op=ALU.mult),
                         [g_, rope], [b_])
                    P.op('pool', lambda e, b_=b_, g_=g_, psin=psin: e.tensor_tensor(out=b_.ap()[:, :, 1, :], in0=g_.ap()[:, :, 0, :], in1=psin, op=ALU.mult),
                         [g_, rope], [b_])
                    P.op('dve', lambda e, a_=a_, b_=b_: e.tensor_tensor(out=a_.ap(), in0=a_.ap(), in1=b_.ap(), op=ALU.add), [a_, b_], [a_])
                    P.op('dve', lambda e, a_=a_, r_=r_, o_=o_: e.tensor_tensor(out=o_.ap(), in0=a_.ap().rearrange("p h a d -> p h (a d)"),
                                                                             in1=r_.ap().unsqueeze(2).to_broadcast([128, 4, 64]), op=ALU.mult),
                         [a_, r_], [o_])
                    def emit_tr(o_=o_, dstT=dstT, tt=tt, ti=len(pend_tr) + n_tr[0]):
                        pt_ = ptr[ti % 2]
                        for pr in range(2):
                            P.op('pe', lambda e, pt_=pt_, pr=pr, o_=o_: e.transpose(pt_.ap()[:, pr, :], o_.ap()[:, 2 * pr:2 * pr + 2, :].rearrange("p h d -> p (h d)"),
                                                                                   identb.ap()), [o_, identb], [pt_])
                        if dstT is qT:
                            for hf in range(2):
                                P.op('act', lambda e, pt_=pt_, tt=tt, hf=hf: e.copy(out=qT.ap()[hf * 64:(hf + 1) * 64, :, hf, tt * 128:(tt + 1) * 128],
                                                                                 in_=pt_.ap()[hf * 64:(hf + 1) * 64, :, :]), [pt_], [qT])
                        else:
                            P.op('act', lambda e, pt_=pt_, dstT=dstT, tt=tt: e.copy(out=dstT.ap()[:, :, tt * 128:(tt + 1) * 128], in_=pt_.ap()), [pt_], [dstT])
                    pend_tr.append(emit_tr)
                    while len(pend_tr) > TR_LAG:
                        pend_tr.pop(0)()
                        n_tr[0] += 1
                    cnt += 1
            while pend_tr:
                pend_tr.pop(0)()
                n_tr[0] += 1
            P.pop()

            P.push()
            NPT = 4 if kind == 'A' else 6
            LAG = 1 if kind == 'A' else 3
            PT = [P.sbuf(f"PT{i}", [128, 512], BF16) for i in range(NPT)]
            pst = [P.psum(f"pst{i}", [128, 512], F32, bk * BANK) for i, bk in enumerate((0, 1) if kind == 'A' else (0, 1, 4, 5))]
            ptr2 = P.psum("ptr2", [128, 2, 128], BF16, 6 * BANK)
            pw = [P.psum("pwa0", [128, 512], F32, 7 * BANK), P.psum("pwa1", [128, 512], F32, 6 * BANK + 1024)] if False else None
            pwo = P.psum("pwo", [128, 512], F32, 7 * BANK)
            if kind == 'A':
                pO = [[P.psum(f"pO{m}{j}", [128, dv + 1], F32, (2 + 2 * m + j // 2) * BANK + (j % 2) * 1024) for j in range(4)] for m in range(2)]
            else:
                pO = [[P.psum(f"pO{m}{j}", [128, dv + 1], F32, (2 + m) * BANK + j * 512) for j in range(4)] for m in range(2)]
            strip = cstrip if kind == 'A' else dstrip
            ycat = P.sbuf("ycat", [128, 4, 256], BF16)
            ycT = P.sbuf("ycT", [128, 2, 128], BF16)
            if kind == 'A':
                Oc = [P.sbuf(f"Oc{m}", [128, 4, dv + 1], F32) for m in range(2)]
                rr_ = P.sbuf("rr_", [128, 2, 4], F32)
                av = P.sbuf("av", [128, 4, 128], F32)
                bv = P.sbuf("bv", [128, 4, 128], F32)
                ssa = P.sbuf("ssa", [128, 4], F32)
            else:
                Oc = [P.sbuf(f"Oc{m}", [128, 4, dv + 1], F32) for m in range(4)]
                rr_ = P.sbuf("rr_", [128, 4, 4], F32)
            state = {'st': 0, 'pt': 0}

            def run_streams(streams, qc):
                nk = 4 * qc + 4
                items = [(si, kt) for kt in range(nk) for si in range(len(streams))]
                staged = []

                def issue_qk(si, kt):
                    qsel, ksel, vsel, Oacc = streams[si]
                    ps = pst[state['st'] % len(pst)]
                    state['st'] += 1
                    masked = (kind == 'C') or (kt >= 4 * qc)
                    if kind == 'A' and masked:
                        c0 = (kt - 4 * qc) * 128
                        P.op('pe', lambda e, ps=ps, kt=kt, c0=c0: e.matmul(ps.ap()[:, c0:512], lhsT=ksel(kt), rhs=qsel(qc)[:, c0:512], start=True, stop=True),
                             [kT, qT], [ps])
                        pt = PT[state['pt'] % NPT]
                        state['pt'] += 1
                        P.op('act', lambda e, pt=pt, ps=ps, c0=c0: e.activation(out=pt.ap()[:, c0:512], in_=ps.ap()[:, c0:512], func=AF.Exp, scale=0.125), [ps], [pt])
                        P.op('pool', lambda e, pt=pt, c0=c0: e.tensor_tensor(out=pt.ap()[:, c0:c0 + 128], in0=pt.ap()[:, c0:c0 + 128], in1=tri01.ap(), op=ALU.mult),
                             [pt, tri01], [pt])
                        return pt
                    P.op('pe', lambda e, ps=ps, kt=kt: e.matmul(ps.ap(), lhsT=ksel(kt), rhs=qsel(qc), start=True, stop=True), [kT, qT], [ps])
                    pt = PT[state['pt'] % NPT]
                    state['pt'] += 1
                    P.op('act', lambda e, pt=pt, ps=ps: e.activation(out=pt.ap(), in_=ps.ap(), func=AF.Exp, scale=0.125), [ps], [pt])
                    if kind == 'C':
                        off = 512 * qc - 128 * kt + 384
                        P.op('pool', lambda e, pt=pt, off=off: e.tensor_tensor(out=pt.ap(), in0=pt.ap(), in1=strip.ap()[:, off:off + 512], op=ALU.mult),
                             [pt, strip], [pt])
                    return pt

                def issue_pv(si, kt, pt):
                    qsel, ksel, vsel, Oacc = streams[si]
                    for j in range(4):
                        qt = 4 * qc + j
                        if kt > qt:
                            continue
                        P.op('pe', lambda e, j=j, kt=kt, pt=pt, Oacc=Oacc, qt=qt: e.matmul(Oacc[j].ap(), lhsT=pt.ap()[:, j * 128:(j + 1) * 128], rhs=vsel(kt),
                                                                                        start=False, stop=(kt == qt), skip_group_check=True),
                             [pt, vA], [Oacc[j]])

                for (qsel, ksel, vsel, Oacc) in streams:
                    banks = sorted(set(o.lo // BANK for o in Oacc))
                    for bk in banks:
                        accs = [o for o in Oacc if o.lo // BANK == bk]
                        bank_ap = P.ps_handle.ap()[:, bk * 512:(bk + 1) * 512]
                        P.op('pe', lambda e, bank_ap=bank_ap: e.matmul(bank_ap, lhsT=zerob.ap(), rhs=dstrip.ap()[:, 0:512], start=True, stop=True),
                             [zerob, dstrip], accs)
                pend = []
                for (si, kt) in items:
                    pt = issue_qk(si, kt)
                    pend.append((si, kt, pt))
                    if len(pend) > LAG:
                        issue_pv(*pend.pop(0))
                while pend:
                    issue_pv(*pend.pop(0))

            pend_tail = []

            def flush_tail():
                while pend_tail:
                    pend_tail.pop(0)()

            for qc in range(4):
                if kind == 'A':
                    for h in range(2):
                        streams = []
                        for m in range(2):
                            lo = m * 64
                            streams.append((lambda qc_, h=h, m=m: qT.ap()[:, h, m, qc_ * 512:(qc_ + 1) * 512],
                                            lambda kt, h=h: kT.ap()[:, h, kt * 128:(kt + 1) * 128],
                                            lambda kt, h=h: vA.ap()[:, kt, h, :],
                                            pO[m]))
                        run_streams(streams, qc)
                        if h == 0:
                            flush_tail()
                        for m in range(2):
                            for jj in range(2):
                                src_bank = pO[m][2 * jj]
                                for j in (2 * jj, 2 * jj + 1):
                                    eng = 'act' if j % 2 == 0 else 'dve'
                                    if eng == 'act':
                                        P.op('act', lambda e, m=m, j=j: e.copy(out=Oc[m].ap()[:, j, :], in_=pO[m][j].ap()), [pO[m][j]], [Oc[m]])
                                    else:
                                        P.op('dve', lambda e, m=m, j=j: e.tensor_copy(out=Oc[m].ap()[:, j, :], in_=pO[m][j].ap()), [pO[m][j]], [Oc[m]])
                        for m in range(2):
                            P.op('dve', lambda e, m=m: e.reciprocal(out=rr_.ap()[:, m, :], in_=Oc[m].ap()[:, :, dv]), [Oc[m]], [rr_])
                        P.op('dve', lambda e: e.tensor_scalar(out=rr_.ap()[:, 1, :], in0=rr_.ap()[:, 1, :], scalar1=neglam.ap(), scalar2=None, op0=ALU.mult),
                             [rr_, neglam], [rr_])
                        P.op('pool', lambda e: e.tensor_tensor(out=av.ap(), in0=Oc[0].ap()[:, :, 0:dv], in1=rr_.ap()[:, 0, :].unsqueeze(2).to_broadcast([128, 4, dv]),
                                                               op=ALU.mult), [Oc[0], rr_], [av])
                        P.op('dve', lambda e: e.tensor_tensor(out=bv.ap(), in0=Oc[1].ap()[:, :, 0:dv], in1=rr_.ap()[:, 1, :].unsqueeze(2).to_broadcast([128, 4, dv]),
                                                              op=ALU.mult), [Oc[1], rr_], [bv])
                        P.op('dve', lambda e: e.tensor_tensor(out=av.ap(), in0=av.ap(), in1=bv.ap(), op=ALU.add), [av, bv], [av])
                        P.op('pool', lambda e: e.tensor_tensor(out=bv.ap(), in0=av.ap(), in1=av.ap(), op=ALU.mult), [av], [bv])
                        P.op('dve', lambda e: e.reduce_sum(out=ssa.ap(), in_=bv.ap(), axis=AX.X), [bv], [ssa])
                        rstd_from_ss(ssa, 128)
                        P.op('dve', lambda e: e.tensor_tensor(out=av.ap(), in0=av.ap(), in1=ssa.ap().unsqueeze(2).to_broadcast([128, 4, dv]), op=ALU.mult),
                             [av, ssa], [av])
                        P.op('pool', lambda e, h=h: e.tensor_tensor(out=ycat.ap()[:, :, h * 128:(h + 1) * 128], in0=av.ap(),
                                                                    in1=gsub.ap().unsqueeze(1).to_broadcast([128, 4, dv]), op=ALU.mult), [av, gsub], [ycat])
                else:
                    for hp in range(2):
                        streams = []
                        for hh in range(2):
                            hd = 2 * hp + hh
                            lo = (hd % 2) * 64
                            pr = hd // 2
                            streams.append((lambda qc_, pr=pr, hd=hd: qT.ap()[:, pr, hd % 2, qc_ * 512:(qc_ + 1) * 512],
                                            lambda kt, pr=pr: kT.ap()[:, pr, kt * 128:(kt + 1) * 128],
                                            lambda kt, hd=hd: vA.ap()[:, kt, hd, :],
                                            pO[hh]))
                        run_streams(streams, qc)
                        if hp == 0:
                            flush_tail()
                        for hh in range(2):
                            hd = 2 * hp + hh
                            for j in range(4):
                                if (hd + j) % 2 == 0:
                                    P.op('act', lambda e, hd=hd, hh=hh, j=j: e.copy(out=Oc[hd].ap()[:, j, :], in_=pO[hh][j].ap()), [pO[hh][j]], [Oc[hd]])
                                else:
                                    P.op('dve', lambda e, hd=hd, hh=hh, j=j: e.tensor_copy(out=Oc[hd].ap()[:, j, :], in_=pO[hh][j].ap()), [pO[hh][j]], [Oc[hd]])
                    for hd in range(4):
                        P.op('dve', lambda e, hd=hd: e.reciprocal(out=rr_.ap()[:, hd, :], in_=Oc[hd].ap()[:, :, dv]), [Oc[hd]], [rr_])
                        eng = 'pool' if hd % 2 == 0 else 'dve'
                        P.op(eng, lambda e, hd=hd: e.tensor_tensor(out=ycat.ap()[:, :, hd * 64:(hd + 1) * 64], in0=Oc[hd].ap()[:, :, 0:dv],
                                                                   in1=rr_.ap()[:, hd, :].unsqueeze(2).to_broadcast([128, 4, dv]), op=ALU.mult),
                             [Oc[hd], rr_], [ycat])
                def tail(qc=qc):
                    for j in range(4):
                      tt = 4 * qc + j
                      for c in range(2):
                          P.op('pe', lambda e, j=j, c=c: e.transpose(ptr2.ap()[:, c, :], ycat.ap()[:, j, c * 128:(c + 1) * 128], identb.ap()),
                               [ycat, identb], [ptr2])
                      P.op('act', lambda e: e.copy(out=ycT.ap(), in_=ptr2.ap()), [ptr2], [ycT])
                      for half in range(2):
                          for c in range(2):
                              P.op('pe', lambda e, c=c, half=half: e.matmul(pwo.ap(), lhsT=ycT.ap()[:, c, :], rhs=wo_sb.ap()[:, c, half * 512:(half + 1) * 512],
                                                                            start=(c == 0), stop=(c == 1)), [ycT, wo_sb], [pwo])
                          xs = Xt[tt].ap()[:, half * 512:(half + 1) * 512]
                          P.op('dve', lambda e, xs=xs: e.tensor_tensor(out=xs, in0=xs, in1=pwo.ap(), op=ALU.add), [Xt[tt], pwo], [Xt[tt]])
                pend_tail.append(tail)
            flush_tail()
            P.pop()
            P.pop()

        attention_group('C', 1792, 2048, 2304, 768, gq['qc'], gq['kc'])
        if stop_at == 'attC' and l == stop_layer:
            break
        attention_group('A', 0, 512, 1024, 0, gq['qa'], gq['ka'])
        attention_group('A', 256, 768, 1280, 256, gq['qa'], gq['ka'])
        P.pop()
        if stop_at == 'mix' and l == stop_layer:
            break

        P.push()
        hT = P.sbuf("h2T", [128, 8, S], BF16)
        P.push()
        ssq = P.sbuf("ssq", [128, NT], F32)
        junk = P.sbuf("junk", [128, D], BF16)
        diag = P.sbuf("diag", [128, NT, 128], F32)
        pn = [P.psum(f"pn{i}", [128, 512], F32, i * BANK) for i in range(2)]
        norm_to_hT(hT, 1, (ssq, junk, diag, pn))
        P.pop()

        cw = P.sbuf("cw", [128, NT, 32], F32)
        P.push()
        wrf = P.sbuf("wrf", [128, 8, 36], F32)
        wrb = P.sbuf("wrb", [128, 8, 36], BF16)
        brb = P.sbuf("brb", [128, 36], F32)
        lg = P.sbuf("lg", [128, NT, 36], F32)
        P.dma('sp', wrf.ap()[:, :, 0:4], w_rg_d[l].rearrange("(c p) n -> p c n", p=128), [], [wrf], allow_slow_non_contiguous=True)
        for g in range(4):
            P.dma('sp', wrf.ap()[:, :, 4 + 8 * g:12 + 8 * g], w_re_d[l, g].rearrange("(c p) n -> p c n", p=128), [], [wrf], allow_slow_non_contiguous=True)
        P.dma('sp', brb.ap()[:, 0:4], b_rg_d[l, :].partition_broadcast(128), [], [brb])
        P.dma('sp', brb.ap()[:, 4:36], b_re_d[l, :].partition_broadcast(128), [], [brb])
        P.op('dve', lambda e: e.tensor_copy(out=wrb.ap(), in_=wrf.ap()), [wrf], [wrb])
        pl = [P.psum(f"pl{i}", [128, 8, 36], F32, (2 + i) * BANK) for i in range(2)]
        for tt in range(NT):
            pp = pl[tt // 8]
            for k in range(8):
                P.op('pe', lambda e, pp=pp, tt=tt, k=k: e.matmul(pp.ap()[:, tt % 8, :], lhsT=hT.ap()[:, k, tt * 128:(tt + 1) * 128], rhs=wrb.ap()[:, k, :],
                                                                start=(k == 0), stop=(k == 7)), [hT, wrb], [pp])
        for i2 in range(2):
            P.op('dve', lambda e, i2=i2: e.tensor_tensor(out=lg.ap()[:, 8 * i2:8 * i2 + 8, :], in0=pl[i2].ap(),
                                                         in1=brb.ap().unsqueeze(1).to_broadcast([128, 8, 36]), op=ALU.add), [pl[i2], brb], [lg])
        mg = P.sbuf("mg", [128, NT], F32)
        ohg = P.sbuf("ohg", [128, NT, 4], F32)
        eg = P.sbuf("eg", [128, NT, 4], F32)
        gw = P.sbuf("gw", [128, NT], F32)
        elm = P.sbuf("elm", [128, NT, 32], F32)
        elm2 = P.sbuf("elm2", [128, NT, 32], F32)
        oh1 = P.sbuf("oh1", [128, NT, 32], F32)
        oh2 = P.sbuf("oh2", [128, NT, 32], F32)
        tp1 = P.sbuf("tp1", [128, NT], F32)
        tp2 = P.sbuf("tp2", [128, NT], F32)
        w1g = P.sbuf("w1g", [128, NT], F32)
        w2g = P.sbuf("w2g", [128, NT], F32)
        gl = lg.ap()[:, :, 0:4]
        el = lg.ap()[:, :, 4:36]
        bc4 = lambda b_: b_.ap().unsqueeze(2).to_broadcast([128, NT, 4])
        bc32 = lambda b_: b_.ap().unsqueeze(2).to_broadcast([128, NT, 32])
        P.op('dve', lambda e: e.reduce_max(out=mg.ap(), in_=gl, axis=AX.X), [lg], [mg])
        P.op('dve', lambda e: e.tensor_tensor(out=ohg.ap(), in0=gl, in1=bc4(mg), op=ALU.is_ge), [lg, mg], [ohg])
        P.op('dve', lambda e: e.tensor_tensor(out=eg.ap(), in0=gl, in1=bc4(mg), op=ALU.subtract), [lg, mg], [eg])
        P.op('act', lambda e: e.activation(out=eg.ap(), in_=eg.ap(), func=AF.Exp), [eg], [eg])
        P.op('dve', lambda e: e.reduce_sum(out=gw.ap(), in_=eg.ap(), axis=AX.X), [eg], [gw])
        P.op('dve', lambda e: e.reciprocal(out=gw.ap(), in_=gw.ap()), [gw], [gw])
        P.op('dve', lambda e: e.tensor_scalar(out=ohg.ap(), in0=ohg.ap(), scalar1=1.0, scalar2=1e30, op0=ALU.subtract, op1=ALU.mult), [ohg], [ohg])
        P.op('dve', lambda e: e.tensor_tensor(out=elm.ap().rearrange("p t (g x) -> p t g x", g=4), in0=el.rearrange("p t (g x) -> p t g x", g=4),
                                              in1=ohg.ap().unsqueeze(3).to_broadcast([128, NT, 4, 8]), op=ALU.add), [lg, ohg], [elm])
        P.op('dve', lambda e: e.reduce_max(out=tp1.ap(), in_=elm.ap(), axis=AX.X), [elm], [tp1])
        P.op('dve', lambda e: e.tensor_tensor(out=oh1.ap(), in0=elm.ap(), in1=bc32(tp1), op=ALU.is_ge), [elm, tp1], [oh1])
        P.op('dve', lambda e: e.scalar_tensor_tensor(out=elm2.ap(), in0=oh1.ap(), scalar=-1e30, in1=elm.ap(), op0=ALU.mult, op1=ALU.add), [oh1, elm], [elm2])
        P.op('dve', lambda e: e.reduce_max(out=tp2.ap(), in_=elm2.ap(), axis=AX.X), [elm2], [tp2])
        P.op('dve', lambda e: e.tensor_tensor(out=oh2.ap(), in0=elm2.ap(), in1=bc32(tp2), op=ALU.is_ge), [elm2, tp2], [oh2])
        P.op('dve', lambda e: e.tensor_tensor(out=tp2.ap(), in0=tp2.ap(), in1=tp1.ap(), op=ALU.subtract), [tp2, tp1], [tp2])
        P.op('act', lambda e: e.activation(out=tp2.ap(), in_=tp2.ap(), func=AF.Exp), [tp2], [tp2])
        P.op('dve', lambda e: e.tensor_scalar(out=tp1.ap(), in0=tp2.ap(), scalar1=1.0, scalar2=None, op0=ALU.add), [tp2], [tp1])
        P.op('dve', lambda e: e.reciprocal(out=tp1.ap(), in_=tp1.ap()), [tp1], [tp1])
        P.op('dve', lambda e: e.tensor_tensor(out=w1g.ap(), in0=tp1.ap(), in1=gw.ap(), op=ALU.mult), [tp1, gw], [w1g])
        P.op('dve', lambda e: e.tensor_tensor(out=w2g.ap(), in0=w1g.ap(), in1=tp2.ap(), op=ALU.mult), [w1g, tp2], [w2g])
        P.op('dve', lambda e: e.tensor_tensor(out=oh1.ap(), in0=oh1.ap(), in1=bc32(w1g), op=ALU.mult), [oh1, w1g], [oh1])
        P.op('dve', lambda e: e.tensor_tensor(out=oh2.ap(), in0=oh2.ap(), in1=bc32(w2g), op=ALU.mult), [oh2, w2g], [oh2])
        P.op('dve', lambda e: e.tensor_tensor(out=cw.ap(), in0=oh1.ap(), in1=oh2.ap(), op=ALU.add), [oh1, oh2], [cw])
        P.pop()

        NW = 2
        W1 = [P.sbuf(f"W1_{i}", [128, 8, 256], BF16) for i in range(NW)]
        W3 = [P.sbuf(f"W3_{i}", [128, 8, 256], BF16) for i in range(NW)]
        W2 = [P.sbuf(f"W2_{i}", [128, 2, D], BF16) for i in range(NW)]
        gT = [[P.sbuf(f"gT_{i}_{q}", [128, 2, 512], BF16) for q in range(4)] for i in range(2)]
        sl = [P.sbuf(f"sl_{i}", [128, 512], BF16) for i in range(2)]
        ph1 = [P.psum(f"ph1_{i}", [128, 512], F32, i * BANK) for i in range(2)]
        ph3 = [P.psum(f"ph3_{i}", [128, 512], F32, (2 + i) * BANK) for i in range(2)]
        py = [P.psum(f"py_{i}", [128, D], F32, (4 + 2 * i) * BANK) for i in range(2)]
        n_exp = 32 if stop_at != 'moe2' else 2
        ci = 0
        yi = [0]

        def emit_y(ex, wi, tq_, tiles):
            g_ = gT[ex % 2][tq_]
            for t4 in tiles:
                tt = tq_ * 4 + t4
                pp = py[yi[0] % 2]
                yi[0] += 1
                for half in range(2):
                    for fc in range(2):
                        P.op('pe', lambda e, pp=pp, half=half, fc=fc, t4=t4, g_=g_, wi=wi: e.matmul(
                            pp.ap()[:, half * 512:(half + 1) * 512], lhsT=g_.ap()[:, fc, t4 * 128:(t4 + 1) * 128],
                            rhs=W2[wi].ap()[:, fc, half * 512:(half + 1) * 512], start=(fc == 0), stop=(fc == 1)), [g_, W2[wi]], [pp])
                P.op('dve', lambda e, pp=pp, tt=tt, ex=ex: e.scalar_tensor_tensor(out=Xt[tt].ap(), in0=pp.ap(), scalar=cw.ap()[:, tt, ex:ex + 1],
                                                                               in1=Xt[tt].ap(), op0=ALU.mult, op1=ALU.add), [pp, cw, Xt[tt]], [Xt[tt]])

        for ex in range(n_exp):
            wi = ex % NW
            P.dma('pool', W1[wi].ap(), w1_d[l, ex].rearrange("(c p) n -> p c n", p=128), [], [W1[wi]])
            P.dma('pool', W3[wi].ap(), w3_d[l, ex].rearrange("(c p) n -> p c n", p=128), [], [W3[wi]])
            P.dma('pool', W2[wi].ap(), w2_d[l, ex].rearrange("(c p) n -> p c n", p=128), [], [W2[wi]])
            P.op('pool', lambda e, wi=wi: e.tensor_tensor(out=W2[wi].ap(), in0=W2[wi].ap(), in1=gab[1].ap().unsqueeze(1).to_broadcast([128, 2, D]),
                                                          op=ALU.mult), [W2[wi], gab[1]], [W2[wi]])
            for tq in range(4):
                g_ = gT[ex % 2][tq]
                for fc in range(2):
                    p1, p3, s_ = ph1[ci % 2], ph3[ci % 2], sl[ci % 2]
                    ci += 1
                    for k in range(8):
                        P.op('pe', lambda e, p1=p1, k=k, fc=fc, tq=tq, wi=wi: e.matmul(p1.ap(), lhsT=W1[wi].ap()[:, k, fc * 128:(fc + 1) * 128],
                                                                                     rhs=hT.ap()[:, k, tq * 512:(tq + 1) * 512], start=(k == 0), stop=(k == 7)),
                             [W1[wi], hT], [p1])
                    for k in range(8):
                        P.op('pe', lambda e, p3=p3, k=k, fc=fc, tq=tq, wi=wi: e.matmul(p3.ap(), lhsT=W3[wi].ap()[:, k, fc * 128:(fc + 1) * 128],
                                                                                     rhs=hT.ap()[:, k, tq * 512:(tq + 1) * 512], start=(k == 0), stop=(k == 7)),
                             [W3[wi], hT], [p3])
                    if tq > 0:
                        emit_y(ex, wi, tq - 1, (2 * fc, 2 * fc + 1))
                    elif ex > 0:
                        emit_y(ex - 1, (ex - 1) % NW, 3, (2 * fc, 2 * fc + 1))
                    P.op('act', lambda e, p1=p1, s_=s_: e.activation(out=s_.ap(), in_=p1.ap(), func=AF.Silu), [p1], [s_])
                    P.op('dve', lambda e, p3=p3, s_=s_, g_=g_, fc=fc: e.tensor_tensor(out=g_.ap()[:, fc, :], in0=s_.ap(), in1=p3.ap(),
                                                                                   op=ALU.mult), [s_, p3], [g_])
            if ex == n_exp - 1:
                emit_y(ex, wi, 3, (0, 1, 2, 3))
        P.pop()
        if stop_at in ('l0', 'moe2') and l == stop_layer:
            break

    ov = out_d.rearrange("(tt p) d -> p tt d", p=128)
    if stop_at == 'n1':
        for c in range(8):
            P.op('dve', lambda e, c=c: e.tensor_copy(out=X.ap()[:, c, :].rearrange("p (a b) -> p a b", a=1)[:, 0, :], in_=hT.ap()[:, c, 0:1024]), [hT], [X])
            P.op('dve', lambda e, c=c: e.tensor_copy(out=X.ap()[:, 8 + c, :], in_=hT.ap()[:, c, 1024:2048]), [hT], [X])
    for t in range(NT):
        P.dma('sp', ov[:, t, :], Xt[t].ap(), [Xt[t]], [out_buf])
    P.finish([out_buf] + extra_final)
    print("max sbuf top", P.max_top, "n_dma", P.n_dma, {e: len(P.ops[e]) for e in ENGINES})
    return nc


_CACHE = {}


def make_in_maps(inputs):
    consts = host_consts()
    f = lambda a: np.ascontiguousarray(np.asarray(a, dtype=np.float32))
    L = DEPTH
    shared = {
        'w_mod': f(inputs['w_mod']), 'b_mod': f(inputs['b_mod']), 'g_norm1': f(inputs['g_norm1']),
        'w_in': f(inputs['w_in']), 'gq_a': f(inputs['gq_a']), 'gk_a': f(inputs['gk_a']),
        'lam_a': f(inputs['lam_a']).reshape(L, 256), 'g_sub_a': f(inputs['g_sub_a']),
        'w_pool': f(inputs['w_pool']), 'b_pool': f(inputs['b_pool']).reshape(L, 256),
        'pool_scale': f(inputs['pool_scale']), 'gq_c': f(inputs['gq_c']), 'gk_c': f(inputs['gk_c']),
        'w_out': f(inputs['w_out']), 'g_norm2': f(inputs['g_norm2']), 'w_rg': f(inputs['w_rg']),
        'b_rg': f(inputs['b_rg']), 'w_re': f(inputs['w_re']), 'b_re': f(inputs['b_re']).reshape(L, 32),
        'w1': f(inputs['w1']).reshape(L, 32, D, 256), 'w3': f(inputs['w3']).reshape(L, 32, D, 256),
        'w2': f(inputs['w2']).reshape(L, 32, 256, D),
    }
    shared.update(consts)
    x = f(inputs['x'])
    c = f(inputs['c'])
    maps = []
    for b in range(8):
        m = dict(shared)
        m['x'] = np.ascontiguousarray(x[b])
        m['c_col'] = np.ascontiguousarray(c[b].reshape(8, 128).T)
        maps.append(m)
    return maps


FUSED = True


def kernel(**inputs):
    in_maps = make_in_maps(inputs)
    if FUSED:
        if 'nc' not in _CACHE:
            _CACHE['nc'] = build_program()
        res = run_bass_kernel_spmd(_CACHE['nc'], in_maps, core_ids=list(range(8)))
        return np.stack([np.asarray(r["out"], dtype=np.float32) for r in res.results], axis=0)
    for l in range(DEPTH):
        if ('nc', l) not in _CACHE:
            _CACHE[('nc', l)] = build_program(layers=(l,))
        res = run_bass_kernel_spmd(_CACHE[('nc', l)], in_maps, core_ids=list(range(8)))
        outs = [np.asarray(r["out"], dtype=np.float32) for r in res.results]
        for b in range(8):
            in_maps[b]['x'] = outs[b]
    return np.stack(outs, axis=0)
```

```python
import contextlib
import numpy as np
import ml_dtypes
import concourse.bass as bass
import concourse.mybir as mybir
from concourse.bass_utils import run_bass_kernel_spmd

F32 = mybir.dt.float32
BF16 = mybir.dt.bfloat16
AF = mybir.ActivationFunctionType
ALU = mybir.AluOpType
AX = mybir.AxisListType

ENGINES = ('sp', 'act', 'dve', 'pool', 'pe')
N_DMA_SEMS = 80
DT_SIZE = {F32: 4, BF16: 2}


class Buf:
    def __init__(self, name, kind, view, lo=0, hi=0):
        self.name = name
        self.kind = kind
        self.view = view
        self.lo, self.hi = lo, hi
        self.W = {}
        self.R = {}

    def ap(self):
        return self.view


class Op:
    __slots__ = ('eng', 'fn', 'deps', 'marked', 'done_key', 'done_val', 'is_dma', 'pre_wait', 'idx')

    def __init__(self, eng, fn, is_dma):
        self.eng = eng
        self.fn = fn
        self.deps = {}
        self.marked = False
        self.done_key = None
        self.done_val = None
        self.is_dma = is_dma
        self.pre_wait = None


class Prog:
    def __init__(self, nc, sbuf_bytes=200 * 1024):
        self.nc = nc
        self.ops = {e: [] for e in ENGINES}
        self.n_dma = 0
        self.dma_ops = []
        self.sbuf_bytes = sbuf_bytes
        self.sb_handle = nc.alloc_sbuf_tensor("sb_all", [128, sbuf_bytes // 4], F32)
        self.ps_handle = nc.alloc_psum_tensor("ps_all", [128, 4096], F32)
        self.sb_top = 0
        self.scopes = []
        self.live = {'sbuf': [], 'psum': []}
        self.retired = {'sbuf': [], 'psum': []}
        self.max_top = 0

    def _mk(self, name, kind, lo, shape, dtype):
        esz = DT_SIZE[dtype]
        n = int(np.prod(shape[1:]))
        nbytes = n * esz
        hi = lo + nbytes
        base = self.sb_handle if kind == 'sbuf' else self.ps_handle
        assert lo % 4 == 0
        v = base.ap()[0:shape[0], lo // 4:(lo + ((nbytes + 3) // 4) * 4) // 4]
        if dtype != F32:
            v = v.bitcast(dtype)
            v = v[:, 0:n]
        if len(shape) > 2:
            names = [f"d{i}" for i in range(len(shape) - 1)]
            kw = {nm: s for nm, s in zip(names[:-1], shape[1:-1])}
            v = v.rearrange("p (" + " ".join(names) + ") -> p " + " ".join(names), **kw)
        b = Buf(name, kind, v, lo, hi)
        for o in self.live[kind]:
            assert o.hi <= lo or o.lo >= hi, f"alias live {name} vs {o.name}"
        for o in self.retired[kind]:
            if not (o.hi <= lo or o.lo >= hi):
                for k, op in list(o.W.items()) + list(o.R.items()):
                    if k not in b.R or b.R[k].idx < op.idx:
                        b.R[k] = op
        self.live[kind].append(b)
        return b

    def sbuf(self, name, shape, dtype):
        lo = (self.sb_top + 63) // 64 * 64
        b = self._mk(name, 'sbuf', lo, shape, dtype)
        self.sb_top = b.hi
        self.max_top = max(self.max_top, self.sb_top)
        assert self.sb_top <= self.sbuf_bytes, f"SBUF overflow at {name}: {self.sb_top}"
        if self.scopes:
            self.scopes[-1][1].append(b)
        return b

    def psum(self, name, shape, dtype, byte_off):
        b = self._mk(name, 'psum', byte_off, shape, dtype)
        if self.scopes:
            self.scopes[-1][2].append(b)
        return b

    def dram_buf(self, name):
        return Buf(name, 'dram', None)

    def push(self):
        self.scopes.append((self.sb_top, [], []))

    def pop(self):
        top, sb, ps = self.scopes.pop()
        for b in sb:
            self.live['sbuf'].remove(b)
            self.retired['sbuf'].append(b)
        for b in ps:
            self.live['psum'].remove(b)
            self.retired['psum'].append(b)
        self.sb_top = top

    def free_psum(self, bufs):
        for b in bufs:
            self.live['psum'].remove(b)
            self.retired['psum'].append(b)
            for sc in self.scopes:
                if b in sc[2]:
                    sc[2].remove(b)

    def _track(self, op, reads, writes):
        op.idx = self._next_idx = getattr(self, '_next_idx', 0) + 1
        deps = op.deps

        def add(p):
            if p is op:
                return
            if p.eng == 'pe' and op.eng == 'pe' and not p.is_dma and not op.is_dma:
                return
            k = p.done_key
            if k not in deps or deps[k].idx < p.idx:
                deps[k] = p

        for b in reads:
            for p in b.W.values():
                add(p)
        for b in writes:
            for p in b.W.values():
                add(p)
            for p in b.R.values():
                add(p)
        for p in deps.values():
            p.marked = True
        for b in reads:
            b.R[op.done_key] = op
        for b in writes:
            if b.R:
                b.R = {}
                b.W = {}
            b.W[op.done_key] = op

    def op(self, eng, fn, reads, writes):
        o = Op(eng, fn, False)
        o.done_key = ('e', eng)
        self._track(o, reads, writes)
        self.ops[eng].append(o)
        return o

    def barrier(self):
        lasts = {}
        for e in ENGINES:
            for o in reversed(self.ops[e]):
                if o.fn is not None and not o.is_dma:
                    lasts[o.done_key] = o
                    break
        for o in self.dma_ops:
            k = o.done_key
            if k not in lasts or lasts[k].idx < o.idx:
                lasts[k] = o
        for p in lasts.values():
            p.marked = True
        for e in ENGINES:
            o = Op(e, None, False)
            o.done_key = ('e', e)
            o.idx = self._next_idx = getattr(self, '_next_idx', 0) + 1
            o.deps = {k: p for k, p in lasts.items() if not (k == ('e', e))}
            self.ops[e].append(o)

    def dma(self, eng, out, in_, reads, writes, **kw):
        o = Op(eng, lambda e: e.dma_start(out=out, in_=in_, **kw), True)
        i = self.n_dma
        self.n_dma += 1
        slot = i % N_DMA_SEMS
        o.done_key = ('d', slot)
        o.done_val = 16 * (i // N_DMA_SEMS + 1)
        if i >= N_DMA_SEMS:
            o.pre_wait = (('d', slot), 16 * (i // N_DMA_SEMS))
        self._track(o, reads, writes)
        self.ops[eng].append(o)
        self.dma_ops.append(o)
        return o

    def finish(self, final_bufs):
        nc = self.nc
        for e in ENGINES:
            c = 0
            for o in self.ops[e]:
                if o.is_dma or o.fn is None:
                    continue
                if o.marked:
                    c += 1
                    o.done_val = c
        final_waits = {}
        for b in final_bufs:
            for k, p in b.W.items():
                final_waits[k] = max(final_waits.get(k, 0), p.done_val)
        with contextlib.ExitStack() as st:
            sems = {}
            for e in ENGINES:
                sems[('e', e)] = st.enter_context(nc.semaphore(f"s_{e}"))
            for i in range(min(N_DMA_SEMS, max(1, self.n_dma))):
                sems[('d', i)] = st.enter_context(nc.semaphore(f"s_d{i}"))
            block = st.enter_context(nc.Block())

            def emit(ename, eng):
                waited = {}
                for o in self.ops[ename]:
                    need = {}
                    for k, p in o.deps.items():
                        assert p.done_val is not None, "dep not numbered"
                        need[k] = max(need.get(k, 0), p.done_val)
                    if o.pre_wait is not None:
                        k, v = o.pre_wait
                        need[k] = max(need.get(k, 0), v)
                    for k, v in need.items():
                        if waited.get(k, 0) >= v:
                            continue
                        eng.wait_ge(sems[k], v)
                        waited[k] = v
                    if o.fn is None:
                        continue
                    ins = o.fn(eng)
                    if o.is_dma:
                        ins.then_inc(sems[o.done_key], 16)
                    elif o.marked:
                        ins.then_inc(sems[o.done_key], 1)
                if ename == 'sp':
                    for k, v in final_waits.items():
                        if waited.get(k, 0) < v:
                            eng.wait_ge(sems[k], v)

            @block.sync
            def _(eng):
                emit('sp', eng)

            @block.scalar
            def _(eng):
                emit('act', eng)

            @block.vector
            def _(eng):
                emit('dve', eng)

            @block.gpsimd
            def _(eng):
                emit('pool', eng)

            @block.tensor
            def _(eng):
                emit('pe', eng)


S = 2048
D = 1024
NT = S // 128
DEPTH = 2
NEG = -30000.0
FENCE_M = False
BARRIERS = False
EPS = 1e-6
WINDOWS = (2, 4, 8, 16)


def host_consts():
    c = {}
    c['identf'] = np.eye(128, dtype=np.float32)
    c['identb'] = np.eye(128, dtype=np.float32).astype(ml_dtypes.bfloat16)
    inv = 1.0 / (10000.0 ** (np.arange(0, 64, 2, dtype=np.float32) / 64.0))
    ang = np.arange(S, dtype=np.float32)[:, None] * inv[None, :]
    cos = np.cos(ang).astype(np.float32)
    sin = np.sin(ang).astype(np.float32)
    def tm(a):
        return np.ascontiguousarray(a.reshape(NT, 128, 32).transpose(1, 0, 2))
    c['rope'] = np.ascontiguousarray(np.stack([tm(cos), tm(-sin), tm(sin)], axis=2))
    ki = np.arange(128)[:, None]
    cc = np.arange(896)[None, :]
    delta = cc - 384 - ki
    c['cstrip'] = np.where(delta >= 0, 0.0, NEG).astype(np.float32).astype(ml_dtypes.bfloat16)
    cc = np.arange(2432)[None, :]
    delta = cc - 384 - ki
    mult = ((delta >= 0) & (delta <= 128)).astype(np.int32) \
        + ((delta >= 0) & (delta <= 512) & (delta % 4 == 0)).astype(np.int32) \
        + ((delta >= 0) & (delta % 16 == 0)).astype(np.int32)
    c['dstrip'] = mult.astype(np.float32).astype(ml_dtypes.bfloat16)
    c['tri01'] = (np.arange(128)[None, :] >= np.arange(128)[:, None]).astype(np.float32).astype(ml_dtypes.bfloat16)
    c['invtab'] = np.tile((1.0 / (np.arange(16, dtype=np.float32) + 1.0))[None, :], (128, 1)).astype(np.float32)
    return c


def build_program(stop_at=None, layers=(0, 1)):
    nc = bass.Bass("TRN2", target_bir_lowering=False)
    L = DEPTH
    stop_layer = 0
    if stop_at is not None and '@' in stop_at:
        stop_at, sl_ = stop_at.split('@')
        stop_layer = int(sl_)

    def din(name, shape, dt=F32):
        return nc.dram_tensor(name, list(shape), dt, kind="ExternalInput").ap()

    x_d = din("x", [S, D])
    ccol_d = din("c_col", [128, 8])
    w_mod_d = din("w_mod", [L, D, 6 * D])
    b_mod_d = din("b_mod", [L, 6 * D])
    g1_d = din("g_norm1", [L, D])
    w_in_d = din("w_in", [L, D, 2560])
    gqa_d = din("gq_a", [L, 64])
    gka_d = din("gk_a", [L, 64])
    lam_d = din("lam_a", [L, 256])
    gsub_d = din("g_sub_a", [L, 128])
    wpool_d = din("w_pool", [L, 4, 64, 64])
    bpool_d = din("b_pool", [L, 256])
    pscale_d = din("pool_scale", [L, 256])
    gqc_d = din("gq_c", [L, 64])
    gkc_d = din("gk_c", [L, 64])
    w_out_d = din("w_out", [L, D, D])
    g2_d = din("g_norm2", [L, D])
    w_rg_d = din("w_rg", [L, D, 4])
    b_rg_d = din("b_rg", [L, 4])
    w_re_d = din("w_re", [L, 4, D, 8])
    b_re_d = din("b_re", [L, 32])
    if stop_at in ('n1', 'pool', 'attC', 'mix') and stop_layer == 0:
        w1_d = w3_d = w2_d = None
    else:
        w1_d = din("w1", [L, 32, D, 256])
        w3_d = din("w3", [L, 32, D, 256])
        w2_d = din("w2", [L, 32, 256, D])
    identf_d = din("identf", [128, 128])
    identb_d = din("identb", [128, 128], BF16)
    rope_d = din("rope", [128, NT, 3, 32])
    cstrip_d = din("cstrip", [128, 896], BF16)
    dstrip_d = din("dstrip", [128, 2432], BF16)
    invtab_d = din("invtab", [128, 16])
    tri01_d = din("tri01", [128, 128], BF16)
    out_d = nc.dram_tensor("out", [S, D], F32, kind="ExternalOutput").ap()
    dbg_d = nc.dram_tensor("dbg", [128, 2048], F32, kind="ExternalOutput").ap() if stop_at == 'pool' else None

    P = Prog(nc, sbuf_bytes=206 * 1024)
    out_buf = P.dram_buf("out")
    extra_final = []

    Xt = [P.sbuf(f"X{t}", [128, D], F32) for t in range(NT)]
    X = None
    identf = P.sbuf("identf", [128, 128], F32)
    identb = P.sbuf("identb", [128, 128], BF16)
    rope = P.sbuf("rope", [128, NT, 3, 32], F32)
    cstrip = P.sbuf("cstrip", [128, 896], BF16)
    dstrip = P.sbuf("dstrip", [128, 2432], BF16)
    invtab = P.sbuf("invtab", [128, 16], F32)
    tri01 = P.sbuf("tri01", [128, 128], BF16)
    condrep = P.sbuf("condrep", [128, 8, 128], BF16)
    Acol = [P.sbuf(f"Acol{i}", [128, 8], F32) for i in range(2)]
    Bcol = [P.sbuf(f"Bcol{i}", [128, 8], F32) for i in range(2)]
    gab = [P.sbuf(f"gab{i}", [128, D], F32) for i in range(2)]
    ones_col = P.sbuf("ones_col", [128, 1], F32)
    zerob = P.sbuf("zerob", [128, 128], BF16)

    xv = x_d.rearrange("(tt p) d -> p tt d", p=128)
    for t in range(NT):
        P.dma('sp', Xt[t].ap(), xv[:, t, :], reads=[], writes=[Xt[t]])
    P.dma('sp', identf.ap(), identf_d, [], [identf])
    P.dma('sp', identb.ap(), identb_d, [], [identb])
    P.dma('sp', rope.ap(), rope_d, [], [rope])
    P.dma('sp', cstrip.ap(), cstrip_d, [], [cstrip])
    P.dma('sp', dstrip.ap(), dstrip_d, [], [dstrip])
    P.dma('sp', invtab.ap(), invtab_d, [], [invtab])
    P.dma('sp', tri01.ap(), tri01_d, [], [tri01])
    P.op('pool', lambda e: e.memset(ones_col.ap(), 1.0), [], [ones_col])
    P.op('pool', lambda e: e.memset(zerob.ap(), 0.0), [], [zerob])

    P.push()
    ccol = P.sbuf("ccol", [128, 8], F32)
    ctmp = P.sbuf("ctmp", [128, 8], F32)
    P.dma('sp', ccol.ap(), ccol_d, [], [ccol])
    P.op('act', lambda e: e.activation(out=ctmp.ap(), in_=ccol.ap(), func=AF.Exp, scale=-1.0), [ccol], [ctmp])
    P.op('dve', lambda e: e.tensor_scalar(out=ctmp.ap(), in0=ctmp.ap(), scalar1=1.0, scalar2=None, op0=ALU.add), [ctmp], [ctmp])
    P.op('dve', lambda e: e.reciprocal(out=ctmp.ap(), in_=ctmp.ap()), [ctmp], [ctmp])
    P.op('dve', lambda e: e.tensor_tensor(out=ctmp.ap(), in0=ctmp.ap(), in1=ccol.ap(), op=ALU.mult), [ctmp, ccol], [ctmp])
    P.op('dve', lambda e: e.tensor_copy(out=condrep.ap(), in_=ctmp.ap().unsqueeze(2).to_broadcast([128, 8, 128])),
         [ctmp], [condrep])
    P.pop()

    BANK = 2048
    rr = {'evac': 0}

    def evac_engine():
        rr['evac'] += 1
        return 'act' if rr['evac'] % 2 == 0 else 'dve'

    def affine_evac(eng, out_ap, in_ap, sc_ap, bi_ap, reads, writes):
        if eng == 'act':
            P.op('act', lambda e: e.activation(out=out_ap, in_=in_ap, func=AF.Identity, bias=bi_ap, scale=sc_ap), reads, writes)
        else:
            P.op('dve', lambda e: e.tensor_scalar(out=out_ap, in0=in_ap, scalar1=sc_ap, scalar2=bi_ap, op0=ALU.mult, op1=ALU.add), reads, writes)

    def rstd_from_ss(ss_buf, n, tmp_buf=None):
        P.op('act', lambda e: e.activation(out=ss_buf.ap(), in_=ss_buf.ap(), func=AF.Ln, scale=1.0 / n, bias=EPS), [ss_buf], [ss_buf])
        P.op('act', lambda e: e.activation(out=ss_buf.ap(), in_=ss_buf.ap(), func=AF.Exp, scale=-0.5), [ss_buf], [ss_buf])

    for l in layers:
        if BARRIERS and l != layers[0]:
            P.barrier()
        P.push()
        wmb = [P.sbuf(f"wmb{i}", [128, 8, 512], BF16) for i in range(2)]
        bmb = [P.sbuf(f"bmb{i}", [128, 512], F32) for i in range(2)]
        gnb = [P.sbuf(f"gnb{i}", [128, 512], F32) for i in range(2)]
        modblk = [P.sbuf(f"modblk{i}", [128, 512], F32) for i in range(2)]
        dtmp = P.sbuf("dtmp", [128, 4, 128], F32)
        pm = [P.psum(f"pm{i}", [128, 512], F32, i * BANK) for i in range(2)]
        for j in range(12):
            v, half = j // 2, j % 2
            wb, bb, gb, mb, pp = wmb[j % 2], bmb[j % 2], gnb[j % 2], modblk[j % 2], pm[j % 2]
            P.dma('pool', wb.ap(), w_mod_d[l, :, j * 512:(j + 1) * 512].rearrange("(c p) n -> p c n", p=128), [X] if FENCE_M else [], [wb])
            P.dma('sp', bb.ap(), b_mod_d[l, j * 512:(j + 1) * 512].partition_broadcast(128), [X] if FENCE_M else [], [bb])
            for k in range(8):
                P.op('pe', lambda e, k=k, pp=pp, wb=wb: e.matmul(pp.ap(), lhsT=condrep.ap()[:, k, :], rhs=wb.ap()[:, k, :],
                                                               start=(k == 0), stop=(k == 7)), [condrep, wb], [pp])
            sub = 0 if v < 3 else 1
            vv = v % 3
            if vv == 2:
                dst = gab[sub].ap()[:, half * 512:(half + 1) * 512]
                P.op('dve', lambda e, dst=dst, pp=pp, bb=bb: e.tensor_tensor(out=dst, in0=pp.ap(), in1=bb.ap(), op=ALU.add),
                     [pp, bb], [gab[sub]])
                continue
            P.op('dve', lambda e, mb=mb, pp=pp, bb=bb: e.tensor_tensor(out=mb.ap(), in0=pp.ap(), in1=bb.ap(), op=ALU.add), [pp, bb], [mb])
            if vv == 1:
                gsrc = (g1_d if sub == 0 else g2_d)[l, half * 512:(half + 1) * 512].partition_broadcast(128)
                P.dma('sp', gb.ap(), gsrc, [], [gb])
                P.op('dve', lambda e, mb=mb, gb=gb: e.scalar_tensor_tensor(out=mb.ap(), in0=mb.ap(), scalar=1.0, in1=gb.ap(),
                                                                          op0=ALU.add, op1=ALU.mult), [mb, gb], [mb])
                dstb = Acol[sub]
            else:
                dstb = Bcol[sub]
            P.op('dve', lambda e, mb=mb: e.tensor_tensor(out=dtmp.ap(), in0=mb.ap().rearrange("p (c j) -> p c j", c=4),
                                                        in1=identf.ap().unsqueeze(1).to_broadcast([128, 4, 128]), op=ALU.mult),
                 [mb, identf], [dtmp])
            P.op('dve', lambda e, dstb=dstb, half=half: e.reduce_sum(out=dstb.ap()[:, half * 4:(half + 1) * 4], in_=dtmp.ap(), axis=AX.X),
                 [dtmp], [dstb])
        P.pop()

        if stop_at == 'M' and l == stop_layer:
            break
        if BARRIERS:
            P.barrier()

        def norm_to_hT(hT, sub, extra_scope_bufs):
            ssq, junk, diag, pn = extra_scope_bufs
            for tt in range(NT):
                P.op('act', lambda e, tt=tt: e.activation(out=junk.ap(), in_=Xt[tt].ap(), func=AF.Square,
                                                         accum_out=ssq.ap()[:, tt:tt + 1]), [Xt[tt]], [junk, ssq])
            rstd_from_ss(ssq, D)
            for tt in range(NT):
                P.op('dve', lambda e, tt=tt: e.tensor_scalar(out=diag.ap()[:, tt, :], in0=identf.ap(), scalar1=ssq.ap()[:, tt:tt + 1],
                                                            scalar2=None, op0=ALU.mult), [identf, ssq], [diag])
            i = 0
            for tq in range(4):
                for c in range(8):
                    pp = pn[i % 2]
                    i += 1
                    for t4 in range(4):
                        tt = tq * 4 + t4
                        P.op('pe', lambda e, pp=pp, tt=tt, c=c, t4=t4: e.matmul(pp.ap()[:, t4 * 128:(t4 + 1) * 128],
                                                                              lhsT=Xt[tt].ap()[:, c * 128:(c + 1) * 128],
                                                                              rhs=diag.ap()[:, tt, :], start=True, stop=True),
                             [Xt[tt], diag], [pp])
                    affine_evac(evac_engine(), hT.ap()[:, c, tq * 512:(tq + 1) * 512], pp.ap(),
                                Acol[sub].ap()[:, c:c + 1], Bcol[sub].ap()[:, c:c + 1], [pp, Acol[sub], Bcol[sub]], [hT])

        P.push()
        hT = P.sbuf("hT", [128, 8, S], BF16)
        P.push()
        ssq = P.sbuf("ssq", [128, NT], F32)
        junk = P.sbuf("junk", [128, D], BF16)
        diag = P.sbuf("diag", [128, NT, 128], F32)
        pn = [P.psum(f"pn{i}", [128, 512], F32, i * BANK) for i in range(2)]
        norm_to_hT(hT, 0, (ssq, junk, diag, pn))
        if stop_at == 'pool' and l == stop_layer:
            dbgb = P.dram_buf("dbg")
            scr = P.sbuf("scr", [128, 2048], F32)
            P.op('dve', lambda e: e.tensor_copy(out=scr.ap()[:, 0:1024], in_=X.ap()[:, 3, :]), [X], [scr])
            P.op('dve', lambda e: e.tensor_copy(out=scr.ap()[:, 1024:1032], in_=Acol[0].ap()), [Acol[0]], [scr])
            P.op('dve', lambda e: e.tensor_copy(out=scr.ap()[:, 1032:1040], in_=Bcol[0].ap()), [Bcol[0]], [scr])
            P.op('dve', lambda e: e.tensor_copy(out=scr.ap()[:, 1040:1056], in_=ssq.ap()), [ssq], [scr])
            P.op('dve', lambda e: e.tensor_copy(out=scr.ap()[:, 1536:2048], in_=hT.ap()[:, 0, 0:512]), [hT], [scr])
            P.dma('sp', dbg_d, scr.ap(), [scr], [dbgb])
            extra_final.append(dbgb)
        P.pop()

        if stop_at == 'n1' and l == stop_layer:
            break
        if BARRIERS:
            P.barrier()

        lam_init = 0.8 - 0.6 * float(np.exp(-0.3 * l))

        gq = {}
        for nm, src in (('qa', gqa_d), ('ka', gka_d), ('qc', gqc_d), ('kc', gkc_d)):
            gq[nm] = P.sbuf("g_" + nm, [128, 64], F32)
            P.dma('sp', gq[nm].ap(), src[l, :].partition_broadcast(128), [], [gq[nm]])
        gsub = P.sbuf("gsub", [128, 128], F32)
        P.dma('sp', gsub.ap(), gsub_d[l, :].partition_broadcast(128), [], [gsub])
        P.op('dve', lambda e, gsub=gsub, li=lam_init: e.tensor_scalar(out=gsub.ap(), in0=gsub.ap(), scalar1=1.0 - li, scalar2=None, op0=ALU.mult),
             [gsub], [gsub])
        lamt = P.sbuf("lamt", [128, 256], F32)
        lamv = P.sbuf("lamv", [128, 2], F32)
        neglam = P.sbuf("neglam", [128, 1], F32)
        P.dma('sp', lamt.ap(), lam_d[l, :].partition_broadcast(128), [], [lamt])
        lv = lamt.ap().rearrange("p (a d) -> p a d", a=4)
        P.op('dve', lambda e: e.tensor_tensor(out=lv[:, 0, :], in0=lv[:, 0, :], in1=lv[:, 1, :], op=ALU.mult), [lamt], [lamt])
        P.op('dve', lambda e: e.tensor_tensor(out=lv[:, 2, :], in0=lv[:, 2, :], in1=lv[:, 3, :], op=ALU.mult), [lamt], [lamt])
        P.op('dve', lambda e: e.reduce_sum(out=lamv.ap()[:, 0:1], in_=lv[:, 0, :], axis=AX.X), [lamt], [lamv])
        P.op('dve', lambda e: e.reduce_sum(out=lamv.ap()[:, 1:2], in_=lv[:, 2, :], axis=AX.X), [lamt], [lamv])
        P.op('act', lambda e: e.activation(out=lamv.ap(), in_=lamv.ap(), func=AF.Exp), [lamv], [lamv])
        P.op('dve', lambda e: e.tensor_tensor(out=neglam.ap(), in0=lamv.ap()[:, 1:2], in1=lamv.ap()[:, 0:1], op=ALU.subtract), [lamv], [neglam])
        P.op('dve', lambda e, neglam=neglam, li=lam_init: e.tensor_scalar(out=neglam.ap(), in0=neglam.ap(), scalar1=-li, scalar2=None, op0=ALU.add), [neglam], [neglam])

        def load_w_in_block(dst, col0, ncols):
            P.dma('pool', dst.ap(), w_in_d[l, :, col0:col0 + ncols].rearrange("(c p) n -> p c n", p=128), [], [dst])

        def load_wout_rows(dst, row0):
            P.dma('pool', dst.ap(), w_out_d[l, row0:row0 + 256, :].rearrange("(c p) n -> p c n", p=128), [], [dst])
            P.op('pool', lambda e: e.tensor_tensor(out=dst.ap(), in0=dst.ap(), in1=gab[0].ap().unsqueeze(1).to_broadcast([128, 2, D]),
                                                   op=ALU.mult), [dst, gab[0]], [dst])

        def wout_accumulate(lhs_fn, lhs_bufs, wo_sb, tt, pw):
            for half in range(2):
                pp = pw[half]
                for c in range(2):
                    P.op('pe', lambda e, pp=pp, c=c, half=half: e.matmul(pp.ap(), lhsT=lhs_fn(c), rhs=wo_sb.ap()[:, c, half * 512:(half + 1) * 512],
                                                                         start=(c == 0), stop=(c == 1)), lhs_bufs + [wo_sb], [pp])
                xs = Xt[tt].ap()[:, half * 512:(half + 1) * 512]
                P.op('dve', lambda e, xs=xs, pp=pp: e.tensor_tensor(out=xs, in0=xs, in1=pp.ap(), op=ALU.add), [Xt[tt], pp], [Xt[tt]])

        P.push()
        ubT = P.sbuf("ubT", [128, 2, S], F32)
        wblk = P.sbuf("wblk_p", [128, 8, 256], BF16)
        wo_sb = P.sbuf("wo_p", [128, 2, D], BF16)
        pA = P.sbuf("poolA", [128, S], F32)
        pB = P.sbuf("poolB", [128, S], F32)
        dT = P.sbuf("dT", [128, 2, S], BF16)
        ypT = P.sbuf("ypT", [128, 2, S], BF16)
        wpd = P.sbuf("wpd", [128, 2, 128], F32)
        wpdb = P.sbuf("wpdb", [128, 2, 128], BF16)
        psb = P.sbuf("psb", [128, 256], F32)
        bcol = P.sbuf("bcol", [128, 2], F32)
        scol = P.sbuf("scol", [128, 2], F32)
        pz = [P.psum(f"pz{i}", [128, 512], F32, i * BANK) for i in range(2)]
        pw = [P.psum(f"pw{i}", [128, 512], F32, (2 + i) * BANK) for i in range(2)]
        load_w_in_block(wblk, 1536, 256)
        load_wout_rows(wo_sb, 512)
        P.op('pool', lambda e: e.memset(wpd.ap(), 0.0), [], [wpd])
        for g in range(4):
            ch, hp = g // 2, (g % 2) * 64
            P.dma('sp', wpd.ap()[hp:hp + 64, ch, hp:hp + 64], wpool_d[l, g], [], [wpd])
        P.dma('sp', psb.ap(), pscale_d[l, :].partition_broadcast(128), [], [psb])
        P.dma('sp', bcol.ap(), bpool_d[l, :].rearrange("(c p) -> p c", p=128), [], [bcol], allow_slow_non_contiguous=True)
        P.dma('sp', scol.ap(), pscale_d[l, :].rearrange("(c p) -> p c", p=128), [], [scol], allow_slow_non_contiguous=True)
        P.op('dve', lambda e: e.tensor_tensor(out=wpdb.ap(), in0=wpd.ap(), in1=psb.ap().rearrange("p (c n) -> p c n", c=2), op=ALU.mult),
             [wpd, psb], [wpdb])
        P.op('dve', lambda e: e.tensor_tensor(out=bcol.ap(), in0=bcol.ap(), in1=scol.ap(), op=ALU.mult), [bcol, scol], [bcol])
        i = 0
        for ch in range(2):
            for tq in range(4):
                pp = pz[i % 2]
                i += 1
                for k in range(8):
                    P.op('pe', lambda e, pp=pp, k=k, ch=ch, tq=tq: e.matmul(pp.ap(), lhsT=wblk.ap()[:, k, ch * 128:(ch + 1) * 128],
                                                                          rhs=hT.ap()[:, k, tq * 512:(tq + 1) * 512],
                                                                          start=(k == 0), stop=(k == 7)), [wblk, hT], [pp])
                dst = ubT.ap()[:, ch, tq * 512:(tq + 1) * 512]
                if i % 2 == 0:
                    P.op('act', lambda e, dst=dst, pp=pp: e.copy(out=dst, in_=pp.ap()), [pp], [ubT])
                else:
                    P.op('dve', lambda e, dst=dst, pp=pp: e.tensor_copy(out=dst, in_=pp.ap()), [pp], [ubT])
        for ch in range(2):
            u = ubT.ap()[:, ch, :]
            seq = [(1, u, pA), (2, pA.ap(), pB)] + ([(4, pB.ap(), pA), (8, pA.ap(), pB)] if ch == 1 else [])
            srcbuf = ubT
            for sh, src, dstb in seq:
                P.op('pool', lambda e, sh=sh, src=src, dstb=dstb: e.tensor_copy(out=dstb.ap()[:, 0:sh], in_=src[:, 0:sh]), [srcbuf], [dstb])
                P.op('pool', lambda e, sh=sh, src=src, dstb=dstb: e.tensor_tensor(out=dstb.ap()[:, sh:S], in0=src[:, sh:S], in1=src[:, 0:S - sh],
                                                                                  op=ALU.add), [srcbuf], [dstb])
                srcbuf = dstb
            for hp, sb_, w in ((0, pA, WINDOWS[2 * ch]), (64, pB, WINDOWS[2 * ch + 1])):
                P.op('dve', lambda e, hp=hp, sb_=sb_, w=w, ch=ch: e.scalar_tensor_tensor(
                    out=dT.ap()[hp:hp + 64, ch, :], in0=sb_.ap()[hp:hp + 64, :], scalar=1.0 / w, in1=ubT.ap()[hp:hp + 64, ch, :],
                    op0=ALU.mult, op1=ALU.subtract), [sb_, ubT], [dT])
                P.op('dve', lambda e, hp=hp, sb_=sb_, w=w: e.tensor_tensor(out=sb_.ap()[hp:hp + 64, 0:w - 1], in0=sb_.ap()[hp:hp + 64, 0:w - 1],
                                                                           in1=invtab.ap()[hp:hp + 64, 0:w - 1], op=ALU.mult), [sb_, invtab], [sb_])
                P.op('dve', lambda e, hp=hp, sb_=sb_, w=w, ch=ch: e.tensor_tensor(out=dT.ap()[hp:hp + 64, ch, 0:w - 1], in0=sb_.ap()[hp:hp + 64, 0:w - 1],
                                                                                  in1=ubT.ap()[hp:hp + 64, ch, 0:w - 1], op=ALU.subtract),
                     [sb_, ubT], [dT])
            for tq in range(4):
                pp = pz[i % 2]
                i += 1
                P.op('pe', lambda e, pp=pp, ch=ch, tq=tq: e.matmul(pp.ap(), lhsT=wpdb.ap()[:, ch, :], rhs=dT.ap()[:, ch, tq * 512:(tq + 1) * 512],
                                                                  start=True, stop=True), [wpdb, dT], [pp])
                P.op('act', lambda e, pp=pp, ch=ch, tq=tq: e.activation(out=ypT.ap()[:, ch, tq * 512:(tq + 1) * 512], in_=pp.ap(), func=AF.Identity,
                                                                       bias=bcol.ap()[:, ch:ch + 1], scale=1.0), [pp, bcol], [ypT])
        for tt in range(NT):
            wout_accumulate(lambda c, tt=tt: ypT.ap()[:, c, tt * 128:(tt + 1) * 128], [ypT], wo_sb, tt, pw)
        P.pop()

        if stop_at == 'pool' and l == stop_layer:
            break

        def attention_group(kind, qcol, kcol, vcol, worow, gqb, gkb):
            P.push()
            nh = 2 if kind == 'A' else 4
            dv = 128 if kind == 'A' else 64
            qT = P.sbuf("qT", [128, 2, 2, S], BF16)
            kT = P.sbuf("kT", [128, 2, S], BF16)
            P.op('pool', lambda e: e.memset(qT.ap(), 0.0), [], [qT])
            vA = P.sbuf("vA", [128, NT, nh, dv + 1], BF16)
            wo_sb = P.sbuf("wo_a", [128, 2, D], BF16)
            load_wout_rows(wo_sb, worow)
            P.op('pool', lambda e: e.memset(vA.ap()[:, :, :, dv:dv + 1], 1.0), [], [vA])

            P.push()
            wq = P.sbuf("wq", [128, 8, 256], BF16)
            wk = P.sbuf("wk", [128, 8, 256], BF16)
            wv = P.sbuf("wv", [128, 8, 256], BF16)
            load_w_in_block(wq, qcol, 256)
            load_w_in_block(wk, kcol, 256)
            load_w_in_block(wv, vcol, 256)
            NB = 4
            sq = [P.sbuf(f"sq{i}", [128, 4, 64], F32) for i in range(NB)]
            ssr = [P.sbuf(f"ssr{i}", [128, 4], F32) for i in range(NB)]
            qg = [P.sbuf(f"qg{i}", [128, 4, 2, 32], F32) for i in range(NB)]
            t1 = [P.sbuf(f"t1{i}", [128, 4, 2, 32], F32) for i in range(NB)]
            t2 = [P.sbuf(f"t2{i}", [128, 4, 2, 32], F32) for i in range(NB)]
            qo = [P.sbuf(f"qo{i}", [128, 4, 64], BF16) for i in range(NB)]
            NPZ = 6
            pz = [P.psum(f"pza{i}", [128, 256], F32, i * BANK) for i in range(NPZ)]
            ptr = [P.psum(f"ptr{i}", [128, 2, 128], BF16, (6 + i) * BANK) for i in range(2)]
            cnt = 0
            pend_tr = []
            n_tr = [0]
            TR_LAG = 2
            for tt in range(NT):
                for which, wsb, gb_, dstT in (('q', wq, gqb, qT), ('k', wk, gkb, kT), ('v', wv, None, None)):
                    pp = pz[cnt % NPZ]
                    for k in range(8):
                        P.op('pe', lambda e, pp=pp, k=k, tt=tt, wsb=wsb: e.matmul(pp.ap(), lhsT=hT.ap()[:, k, tt * 128:(tt + 1) * 128],
                                                                                rhs=wsb.ap()[:, k, :], start=(k == 0), stop=(k == 7)),
                             [hT, wsb], [pp])
                    if which == 'v':
                        P.op('act', lambda e, pp=pp, tt=tt: e.copy(out=vA.ap()[:, tt, :, 0:dv], in_=pp.ap().rearrange("p (h d) -> p h d", h=nh)),
                             [pp], [vA])
                        cnt += 1
                        continue
                    b = cnt % NB
                    s_, r_, g_, a_, b_, o_ = sq[b], ssr[b], qg[b], t1[b], t2[b], qo[b]
                    pv = pp.ap().rearrange("p (h d) -> p h d", h=4)
                    P.op('act', lambda e, s_=s_, pv=pv: e.activation(out=s_.ap(), in_=pv, func=AF.Square), [pp], [s_])
                    P.op('dve', lambda e, s_=s_, r_=r_: e.reduce_sum(out=r_.ap(), in_=s_.ap(), axis=AX.X), [s_], [r_])
                    rstd_from_ss(r_, 64)
                    P.op('dve', lambda e, g_=g_, pv=pv, gb_=gb_: e.tensor_tensor(out=g_.ap().rearrange("p h a d -> p h (a d)"), in0=pv,
                                                                             in1=gb_.ap().unsqueeze(1).to_broadcast([128, 4, 64]), op=ALU.mult),
                         [pp, gb_], [g_])
                    cosb = rope.ap()[:, tt, 0:1, :].unsqueeze(1).to_broadcast([128, 4, 2, 32])
                    P.op('dve', lambda e, a_=a_, g_=g_, cosb=cosb: e.tensor_tensor(out=a_.ap(), in0=g_.ap(), in1=cosb, op=ALU.mult), [g_, rope], [a_])
                    nsin = rope.ap()[:, tt, 1:2, :].to_broadcast([128, 4, 32])
                    psin = rope.ap()[:, tt, 2:3, :].to_broadcast([128, 4, 32])
                    P.op('pool', lambda e, b_=b_, g_=g_, nsin=nsin: e.tensor_tensor(out=b_.ap()[:, :, 0, :], in0=g_.ap()[:, :, 1, :], in1=nsin, op=ALU.mult),
                         [g_, rope], [b_])
                    P.op('pool', lambda e, b_=b_, g_=g_, psin=psin: e.tensor_tensor(out=b_.ap()[:, :, 1, :], in0=g_.ap()[:, :, 0, :], in1=psin, op=ALU.mult),
                         [g_, rope], [b_])
                    P.op('dve', lambda e, a_=a_, b_=b_: e.tensor_tensor(out=a_.ap(), in0=a_.ap(), in1=b_.ap(), op=ALU.add), [a_, b_], [a_])
                    P.op('dve', lambda e, a_=a_, r_=r_, o_=o_: e.tensor_tensor(out=o_.ap(), in0=a_.ap().rearrange("p h a d -> p h (a d)"),
                                                                             in1=r_.ap().unsqueeze(2).to_broadcast([128, 4, 64]), op=ALU.mult),
                         [a_, r_], [o_])
                    def emit_tr(o_=o_, dstT=dstT, tt=tt, ti=len(pend_tr) + n_tr[0]):
                        pt_ = ptr[ti % 2]
                        for pr in range(2):
                            P.op('pe', lambda e, pt_=pt_, pr=pr, o_=o_: e.transpose(pt_.ap()[:, pr, :], o_.ap()[:, 2 * pr:2 * pr + 2, :].rearrange("p h d -> p (h d)"),
                                                                                   identb.ap()), [o_, identb], [pt_])
                        if dstT is qT:
                            for hf in range(2):
                                P.op('act', lambda e, pt_=pt_, tt=tt, hf=hf: e.copy(out=qT.ap()[hf * 64:(hf + 1) * 64, :, hf, tt * 128:(tt + 1) * 128],
                                                                                 in_=pt_.ap()[hf * 64:(hf + 1) * 64, :, :]), [pt_], [qT])
                        else:
                            P.op('act', lambda e, pt_=pt_, dstT=dstT, tt=tt: e.copy(out=dstT.ap()[:, :, tt * 128:(tt + 1) * 128], in_=pt_.ap()), [pt_], [dstT])
                    pend_tr.append(emit_tr)
                    while len(pend_tr) > TR_LAG:
                        pend_tr.pop(0)()
                        n_tr[0] += 1
                    cnt += 1
            while pend_tr:
                pend_tr.pop(0)()
                n_tr[0] += 1
            P.pop()

            P.push()
            NPT = 3 if kind == 'A' else 6
            LAG = 1 if kind == 'A' else 3
            PT = [P.sbuf(f"PT{i}", [128, 512], BF16) for i in range(NPT)]
            pst = [P.psum(f"pst{i}", [128, 512], F32, bk * BANK) for i, bk in enumerate((0, 1) if kind == 'A' else (0, 1, 4, 5))]
            ptr2 = P.psum("ptr2", [128, 2, 128], BF16, 6 * BANK)
            pw = [P.psum("pwa0", [128, 512], F32, 7 * BANK), P.psum("pwa1", [128, 512], F32, 6 * BANK + 1024)] if False else None
            pwo = P.psum("pwo", [128, 512], F32, 7 * BANK)
            if kind == 'A':
                pO = [[P.psum(f"pO{m}{j}", [128, dv + 1], F32, (2 + 2 * m + j // 2) * BANK + (j % 2) * 1024) for j in range(4)] for m in range(2)]
            else:
                pO = [[P.psum(f"pO{m}{j}", [128, dv + 1], F32, (2 + m) * BANK + j * 512) for j in range(4)] for m in range(2)]
            strip = cstrip if kind == 'A' else dstrip
            ycat = P.sbuf("ycat", [128, 4, 256], BF16)
            ycT = P.sbuf("ycT", [128, 2, 128], BF16)
            if kind == 'A':
                Oc = [P.sbuf(f"Oc{m}", [128, 4, dv + 1], F32) for m in range(2)]
                rr_ = P.sbuf("rr_", [128, 2, 4], F32)
                av = P.sbuf("av", [128, 4, 128], F32)
                bv = P.sbuf("bv", [128, 4, 128], F32)
                ssa = P.sbuf("ssa", [128, 4], F32)
            else:
                Oc = [P.sbuf(f"Oc{m}", [128, 4, dv + 1], F32) for m in range(4)]
                rr_ = P.sbuf("rr_", [128, 4, 4], F32)
            state = {'st': 0, 'pt': 0}

            def run_streams(streams, qc):
                nk = 4 * qc + 4
                items = [(si, kt) for kt in range(nk) for si in range(len(streams))]
                staged = []

                def issue_qk(si, kt):
                    qsel, ksel, vsel, Oacc = streams[si]
                    ps = pst[state['st'] % len(pst)]
                    state['st'] += 1
                    masked = (kind == 'C') or (kt >= 4 * qc)
                    if kind == 'A' and masked:
                        c0 = (kt - 4 * qc) * 128
                        P.op('pe', lambda e, ps=ps, kt=kt, c0=c0: e.matmul(ps.ap()[:, c0:512], lhsT=ksel(kt), rhs=qsel(qc)[:, c0:512], start=True, stop=True),
                             [kT, qT], [ps])
                        pt = PT[state['pt'] % NPT]
                        state['pt'] += 1
                        P.op('act', lambda e, pt=pt, ps=ps, c0=c0: e.activation(out=pt.ap()[:, c0:512], in_=ps.ap()[:, c0:512], func=AF.Exp, scale=0.125), [ps], [pt])
                        P.op('pool', lambda e, pt=pt, c0=c0: e.tensor_tensor(out=pt.ap()[:, c0:c0 + 128], in0=pt.ap()[:, c0:c0 + 128], in1=tri01.ap(), op=ALU.mult),
                             [pt, tri01], [pt])
                        return pt
                    P.op('pe', lambda e, ps=ps, kt=kt: e.matmul(ps.ap(), lhsT=ksel(kt), rhs=qsel(qc), start=True, stop=True), [kT, qT], [ps])
                    pt = PT[state['pt'] % NPT]
                    state['pt'] += 1
                    P.op('act', lambda e, pt=pt, ps=ps: e.activation(out=pt.ap(), in_=ps.ap(), func=AF.Exp, scale=0.125), [ps], [pt])
                    if kind == 'C':
                        off = 512 * qc - 128 * kt + 384
                        P.op('pool', lambda e, pt=pt, off=off: e.tensor_tensor(out=pt.ap(), in0=pt.ap(), in1=strip.ap()[:, off:off + 512], op=ALU.mult),
                             [pt, strip], [pt])
                    return pt

                def issue_pv(si, kt, pt):
                    qsel, ksel, vsel, Oacc = streams[si]
                    for j in range(4):
                        qt = 4 * qc + j
                        if kt > qt:
                            continue
                        P.op('pe', lambda e, j=j, kt=kt, pt=pt, Oacc=Oacc, qt=qt: e.matmul(Oacc[j].ap(), lhsT=pt.ap()[:, j * 128:(j + 1) * 128], rhs=vsel(kt),
                                                                                        start=False, stop=(kt == qt), skip_group_check=True),
                             [pt, vA], [Oacc[j]])

                for (qsel, ksel, vsel, Oacc) in streams:
                    banks = sorted(set(o.lo // BANK for o in Oacc))
                    for bk in banks:
                        accs = [o for o in Oacc if o.lo // BANK == bk]
                        bank_ap = P.ps_handle.ap()[:, bk * 512:(bk + 1) * 512]
                        P.op('pe', lambda e, bank_ap=bank_ap: e.matmul(bank_ap, lhsT=zerob.ap(), rhs=dstrip.ap()[:, 0:512], start=True, stop=True),
                             [zerob, dstrip], accs)
                pend = []
                for (si, kt) in items:
                    pt = issue_qk(si, kt)
                    pend.append((si, kt, pt))
                    if len(pend) > LAG:
                        issue_pv(*pend.pop(0))
                while pend:
                    issue_pv(*pend.pop(0))

            pend_tail = []

            def flush_tail():
                while pend_tail:
                    pend_tail.pop(0)()

            for qc in range(4):
                if kind == 'A':
                    for h in range(2):
                        streams = []
                        for m in range(2):
                            lo = m * 64
                            streams.append((lambda qc_, h=h, m=m: qT.ap()[:, h, m, qc_ * 512:(qc_ + 1) * 512],
                                            lambda kt, h=h: kT.ap()[:, h, kt * 128:(kt + 1) * 128],
                                            lambda kt, h=h: vA.ap()[:, kt, h, :],
                                            pO[m]))
                        run_streams(streams, qc)
                        if h == 0:
                            flush_tail()
                        for m in range(2):
                            for jj in range(2):
                                src_bank = pO[m][2 * jj]
                                for j in (2 * jj, 2 * jj + 1):
                                    eng = 'act' if j % 2 == 0 else 'dve'
                                    if eng == 'act':
                                        P.op('act', lambda e, m=m, j=j: e.copy(out=Oc[m].ap()[:, j, :], in_=pO[m][j].ap()), [pO[m][j]], [Oc[m]])
                                    else:
                                        P.op('dve', lambda e, m=m, j=j: e.tensor_copy(out=Oc[m].ap()[:, j, :], in_=pO[m][j].ap()), [pO[m][j]], [Oc[m]])
                        for m in range(2):
                            P.op('dve', lambda e, m=m: e.reciprocal(out=rr_.ap()[:, m, :], in_=Oc[m].ap()[:, :, dv]), [Oc[m]], [rr_])
                        P.op('dve', lambda e: e.tensor_scalar(out=rr_.ap()[:, 1, :], in0=rr_.ap()[:, 1, :], scalar1=neglam.ap(), scalar2=None, op0=ALU.mult),
                             [rr_, neglam], [rr_])
                        P.op('dve', lambda e: e.tensor_tensor(out=av.ap(), in0=Oc[0].ap()[:, :, 0:dv], in1=rr_.ap()[:, 0, :].unsqueeze(2).to_broadcast([128, 4, dv]),
                                                               op=ALU.mult), [Oc[0], rr_], [av])
                        P.op('dve', lambda e: e.tensor_tensor(out=bv.ap(), in0=Oc[1].ap()[:, :, 0:dv], in1=rr_.ap()[:, 1, :].unsqueeze(2).to_broadcast([128, 4, dv]),
                                                              op=ALU.mult), [Oc[1], rr_], [bv])
                        P.op('dve', lambda e: e.tensor_tensor(out=av.ap(), in0=av.ap(), in1=bv.ap(), op=ALU.add), [av, bv], [av])
                        P.op('dve', lambda e: e.tensor_tensor(out=bv.ap(), in0=av.ap(), in1=av.ap(), op=ALU.mult), [av], [bv])
                        P.op('dve', lambda e: e.reduce_sum(out=ssa.ap(), in_=bv.ap(), axis=AX.X), [bv], [ssa])
                        rstd_from_ss(ssa, 128)
                        P.op('dve', lambda e: e.tensor_tensor(out=av.ap(), in0=av.ap(), in1=ssa.ap().unsqueeze(2).to_broadcast([128, 4, dv]), op=ALU.mult),
                             [av, ssa], [av])
                        P.op('dve', lambda e, h=h: e.tensor_tensor(out=ycat.ap()[:, :, h * 128:(h + 1) * 128], in0=av.ap(),
                                                                    in1=gsub.ap().unsqueeze(1).to_broadcast([128, 4, dv]), op=ALU.mult), [av, gsub], [ycat])
                else:
                    for hp in range(2):
                        streams = []
                        for hh in range(2):
                            hd = 2 * hp + hh
                            lo = (hd % 2) * 64
                            pr = hd // 2
                            streams.append((lambda qc_, pr=pr, hd=hd: qT.ap()[:, pr, hd % 2, qc_ * 512:(qc_ + 1) * 512],
                                            lambda kt, pr=pr: kT.ap()[:, pr, kt * 128:(kt + 1) * 128],
                                            lambda kt, hd=hd: vA.ap()[:, kt, hd, :],
                                            pO[hh]))
                        run_streams(streams, qc)
                        if hp == 0:
                            flush_tail()
                        for hh in range(2):
                            hd = 2 * hp + hh
                            for j in range(4):
                                if (hd + j) % 2 == 0:
                                    P.op('act', lambda e, hd=hd, hh=hh, j=j: e.copy(out=Oc[hd].ap()[:, j, :], in_=pO[hh][j].ap()), [pO[hh][j]], [Oc[hd]])
                                else:
                                    P.op('dve', lambda e, hd=hd, hh=hh, j=j: e.tensor_copy(out=Oc[hd].ap()[:, j, :], in_=pO[hh][j].ap()), [pO[hh][j]], [Oc[hd]])
                    for hd in range(4):
                        P.op('dve', lambda e, hd=hd: e.reciprocal(out=rr_.ap()[:, hd, :], in_=Oc[hd].ap()[:, :, dv]), [Oc[hd]], [rr_])
                        eng = 'pool' if hd % 2 == 0 else 'dve'
                        P.op(eng, lambda e, hd=hd: e.tensor_tensor(out=ycat.ap()[:, :, hd * 64:(hd + 1) * 64], in0=Oc[hd].ap()[:, :, 0:dv],
                                                                   in1=rr_.ap()[:, hd, :].unsqueeze(2).to_broadcast([128, 4, dv]), op=ALU.mult),
                             [Oc[hd], rr_], [ycat])
                def tail(qc=qc):
                    for j in range(4):
                      tt = 4 * qc + j
                      for c in range(2):
                          P.op('pe', lambda e, j=j, c=c: e.transpose(ptr2.ap()[:, c, :], ycat.ap()[:, j, c * 128:(c + 1) * 128], identb.ap()),
                               [ycat, identb], [ptr2])
                      P.op('act', lambda e: e.copy(out=ycT.ap(), in_=ptr2.ap()), [ptr2], [ycT])
                      for half in range(2):
                          for c in range(2):
                              P.op('pe', lambda e, c=c, half=half: e.matmul(pwo.ap(), lhsT=ycT.ap()[:, c, :], rhs=wo_sb.ap()[:, c, half * 512:(half + 1) * 512],
                                                                            start=(c == 0), stop=(c == 1)), [ycT, wo_sb], [pwo])
                          xs = Xt[tt].ap()[:, half * 512:(half + 1) * 512]
                          P.op('dve', lambda e, xs=xs: e.tensor_tensor(out=xs, in0=xs, in1=pwo.ap(), op=ALU.add), [Xt[tt], pwo], [Xt[tt]])
                pend_tail.append(tail)
            flush_tail()
            P.pop()
            P.pop()

        attention_group('C', 1792, 2048, 2304, 768, gq['qc'], gq['kc'])
        if stop_at == 'attC' and l == stop_layer:
            break
        attention_group('A', 0, 512, 1024, 0, gq['qa'], gq['ka'])
        attention_group('A', 256, 768, 1280, 256, gq['qa'], gq['ka'])
        P.pop()
        if stop_at == 'mix' and l == stop_layer:
            break

        P.push()
        hT = P.sbuf("h2T", [128, 8, S], BF16)
        P.push()
        ssq = P.sbuf("ssq", [128, NT], F32)
        junk = P.sbuf("junk", [128, D], BF16)
        diag = P.sbuf("diag", [128, NT, 128], F32)
        pn = [P.psum(f"pn{i}", [128, 512], F32, i * BANK) for i in range(2)]
        norm_to_hT(hT, 1, (ssq, junk, diag, pn))
        P.pop()

        cw = P.sbuf("cw", [128, NT, 32], F32)
        P.push()
        wrf = P.sbuf("wrf", [128, 8, 36], F32)
        wrb = P.sbuf("wrb", [128, 8, 36], BF16)
        brb = P.sbuf("brb", [128, 36], F32)
        lg = P.sbuf("lg", [128, NT, 36], F32)
        P.dma('sp', wrf.ap()[:, :, 0:4], w_rg_d[l].rearrange("(c p) n -> p c n", p=128), [], [wrf], allow_slow_non_contiguous=True)
        for g in range(4):
            P.dma('sp', wrf.ap()[:, :, 4 + 8 * g:12 + 8 * g], w_re_d[l, g].rearrange("(c p) n -> p c n", p=128), [], [wrf], allow_slow_non_contiguous=True)
        P.dma('sp', brb.ap()[:, 0:4], b_rg_d[l, :].partition_broadcast(128), [], [brb])
        P.dma('sp', brb.ap()[:, 4:36], b_re_d[l, :].partition_broadcast(128), [], [brb])
        P.op('dve', lambda e: e.tensor_copy(out=wrb.ap(), in_=wrf.ap()), [wrf], [wrb])
        pl = [P.psum(f"pl{i}", [128, 8, 36], F32, (2 + i) * BANK) for i in range(2)]
        for tt in range(NT):
            pp = pl[tt // 8]
            for k in range(8):
                P.op('pe', lambda e, pp=pp, tt=tt, k=k: e.matmul(pp.ap()[:, tt % 8, :], lhsT=hT.ap()[:, k, tt * 128:(tt + 1) * 128], rhs=wrb.ap()[:, k, :],
                                                                start=(k == 0), stop=(k == 7)), [hT, wrb], [pp])
        for i2 in range(2):
            P.op('dve', lambda e, i2=i2: e.tensor_tensor(out=lg.ap()[:, 8 * i2:8 * i2 + 8, :], in0=pl[i2].ap(),
                                                         in1=brb.ap().unsqueeze(1).to_broadcast([128, 8, 36]), op=ALU.add), [pl[i2], brb], [lg])
        mg = P.sbuf("mg", [128, NT], F32)
        ohg = P.sbuf("ohg", [128, NT, 4], F32)
        eg = P.sbuf("eg", [128, NT, 4], F32)
        gw = P.sbuf("gw", [128, NT], F32)
        elm = P.sbuf("elm", [128, NT, 32], F32)
        elm2 = P.sbuf("elm2", [128, NT, 32], F32)
        oh1 = P.sbuf("oh1", [128, NT, 32], F32)
        oh2 = P.sbuf("oh2", [128, NT, 32], F32)
        tp1 = P.sbuf("tp1", [128, NT], F32)
        tp2 = P.sbuf("tp2", [128, NT], F32)
        w1g = P.sbuf("w1g", [128, NT], F32)
        w2g = P.sbuf("w2g", [128, NT], F32)
        gl = lg.ap()[:, :, 0:4]
        el = lg.ap()[:, :, 4:36]
        bc4 = lambda b_: b_.ap().unsqueeze(2).to_broadcast([128, NT, 4])
        bc32 = lambda b_: b_.ap().unsqueeze(2).to_broadcast([128, NT, 32])
        P.op('dve', lambda e: e.reduce_max(out=mg.ap(), in_=gl, axis=AX.X), [lg], [mg])
        P.op('dve', lambda e: e.tensor_tensor(out=ohg.ap(), in0=gl, in1=bc4(mg), op=ALU.is_ge), [lg, mg], [ohg])
        P.op('dve', lambda e: e.tensor_tensor(out=eg.ap(), in0=gl, in1=bc4(mg), op=ALU.subtract), [lg, mg], [eg])
        P.op('act', lambda e: e.activation(out=eg.ap(), in_=eg.ap(), func=AF.Exp), [eg], [eg])
        P.op('dve', lambda e: e.reduce_sum(out=gw.ap(), in_=eg.ap(), axis=AX.X), [eg], [gw])
        P.op('dve', lambda e: e.reciprocal(out=gw.ap(), in_=gw.ap()), [gw], [gw])
        P.op('dve', lambda e: e.tensor_scalar(out=ohg.ap(), in0=ohg.ap(), scalar1=1.0, scalar2=1e30, op0=ALU.subtract, op1=ALU.mult), [ohg], [ohg])
        P.op('dve', lambda e: e.tensor_tensor(out=elm.ap().rearrange("p t (g x) -> p t g x", g=4), in0=el.rearrange("p t (g x) -> p t g x", g=4),
                                              in1=ohg.ap().unsqueeze(3).to_broadcast([128, NT, 4, 8]), op=ALU.add), [lg, ohg], [elm])
        P.op('dve', lambda e: e.reduce_max(out=tp1.ap(), in_=elm.ap(), axis=AX.X), [elm], [tp1])
        P.op('dve', lambda e: e.tensor_tensor(out=oh1.ap(), in0=elm.ap(), in1=bc32(tp1), op=ALU.is_ge), [elm, tp1], [oh1])
        P.op('dve', lambda e: e.scalar_tensor_tensor(out=elm2.ap(), in0=oh1.ap(), scalar=-1e30, in1=elm.ap(), op0=ALU.mult, op1=ALU.add), [oh1, elm], [elm2])
        P.op('dve', lambda e: e.reduce_max(out=tp2.ap(), in_=elm2.ap(), axis=AX.X), [elm2], [tp2])
        P.op('dve', lambda e: e.tensor_tensor(out=oh2.ap(), in0=elm2.ap(), in1=bc32(tp2), op=ALU.is_ge), [elm2, tp2], [oh2])
        P.op('dve', lambda e: e.tensor_tensor(out=tp2.ap(), in0=tp2.ap(), in1=tp1.ap(), op=ALU.subtract), [tp2, tp1], [tp2])
        P.op('act', lambda e: e.activation(out=tp2.ap(), in_=tp2.ap(), func=AF.Exp), [tp2], [tp2])
        P.op('dve', lambda e: e.tensor_scalar(out=tp1.ap(), in0=tp2.ap(), scalar1=1.0, scalar2=None, op0=ALU.add), [tp2], [tp1])
        P.op('dve', lambda e: e.reciprocal(out=tp1.ap(), in_=tp1.ap()), [tp1], [tp1])
        P.op('dve', lambda e: e.tensor_tensor(out=w1g.ap(), in0=tp1.ap(), in1=gw.ap(), op=ALU.mult), [tp1, gw], [w1g])
        P.op('dve', lambda e: e.tensor_tensor(out=w2g.ap(), in0=w1g.ap(), in1=tp2.ap(), op=ALU.mult), [w1g, tp2], [w2g])
        P.op('dve', lambda e: e.tensor_tensor(out=oh1.ap(), in0=oh1.ap(), in1=bc32(w1g), op=ALU.mult), [oh1, w1g], [oh1])
        P.op('dve', lambda e: e.tensor_tensor(out=oh2.ap(), in0=oh2.ap(), in1=bc32(w2g), op=ALU.mult), [oh2, w2g], [oh2])
        P.op('dve', lambda e: e.tensor_tensor(out=cw.ap(), in0=oh1.ap(), in1=oh2.ap(), op=ALU.add), [oh1, oh2], [cw])
        P.pop()

        NW = 2
        W1 = [P.sbuf(f"W1_{i}", [128, 8, 256], BF16) for i in range(NW)]
        W3 = [P.sbuf(f"W3_{i}", [128, 8, 256], BF16) for i in range(NW)]
        W2 = [P.sbuf(f"W2_{i}", [128, 2, D], BF16) for i in range(NW)]
        gT = [[P.sbuf(f"gT_{i}_{q}", [128, 2, 512], BF16) for q in range(4)] for i in range(2)]
        sl = [P.sbuf(f"sl_{i}", [128, 512], BF16) for i in range(2)]
        ph1 = [P.psum(f"ph1_{i}", [128, 512], F32, i * BANK) for i in range(2)]
        ph3 = [P.psum(f"ph3_{i}", [128, 512], F32, (2 + i) * BANK) for i in range(2)]
        py = [P.psum(f"py_{i}", [128, D], F32, (4 + 2 * i) * BANK) for i in range(2)]
        n_exp = 32 if stop_at != 'moe2' else 2
        ci = 0
        yi = [0]

        def emit_y(ex, wi, tq_, tiles):
            g_ = gT[ex % 2][tq_]
            for t4 in tiles:
                tt = tq_ * 4 + t4
                pp = py[yi[0] % 2]
                yi[0] += 1
                for half in range(2):
                    for fc in range(2):
                        P.op('pe', lambda e, pp=pp, half=half, fc=fc, t4=t4, g_=g_, wi=wi: e.matmul(
                            pp.ap()[:, half * 512:(half + 1) * 512], lhsT=g_.ap()[:, fc, t4 * 128:(t4 + 1) * 128],
                            rhs=W2[wi].ap()[:, fc, half * 512:(half + 1) * 512], start=(fc == 0), stop=(fc == 1)), [g_, W2[wi]], [pp])
                P.op('dve', lambda e, pp=pp, tt=tt, ex=ex: e.scalar_tensor_tensor(out=Xt[tt].ap(), in0=pp.ap(), scalar=cw.ap()[:, tt, ex:ex + 1],
                                                                               in1=Xt[tt].ap(), op0=ALU.mult, op1=ALU.add), [pp, cw, Xt[tt]], [Xt[tt]])

        for ex in range(n_exp):
            wi = ex % NW
            P.dma('pool', W1[wi].ap(), w1_d[l, ex].rearrange("(c p) n -> p c n", p=128), [], [W1[wi]])
            P.dma('pool', W3[wi].ap(), w3_d[l, ex].rearrange("(c p) n -> p c n", p=128), [], [W3[wi]])
            P.dma('pool', W2[wi].ap(), w2_d[l, ex].rearrange("(c p) n -> p c n", p=128), [], [W2[wi]])
            P.op('pool', lambda e, wi=wi: e.tensor_tensor(out=W2[wi].ap(), in0=W2[wi].ap(), in1=gab[1].ap().unsqueeze(1).to_broadcast([128, 2, D]),
                                                          op=ALU.mult), [W2[wi], gab[1]], [W2[wi]])
            for tq in range(4):
                g_ = gT[ex % 2][tq]
                for fc in range(2):
                    p1, p3, s_ = ph1[ci % 2], ph3[ci % 2], sl[ci % 2]
                    ci += 1
                    for k in range(8):
                        P.op('pe', lambda e, p1=p1, k=k, fc=fc, tq=tq, wi=wi: e.matmul(p1.ap(), lhsT=W1[wi].ap()[:, k, fc * 128:(fc + 1) * 128],
                                                                                     rhs=hT.ap()[:, k, tq * 512:(tq + 1) * 512], start=(k == 0), stop=(k == 7)),
                             [W1[wi], hT], [p1])
                    for k in range(8):
                        P.op('pe', lambda e, p3=p3, k=k, fc=fc, tq=tq, wi=wi: e.matmul(p3.ap(), lhsT=W3[wi].ap()[:, k, fc * 128:(fc + 1) * 128],
                                                                                     rhs=hT.ap()[:, k, tq * 512:(tq + 1) * 512], start=(k == 0), stop=(k == 7)),
                             [W3[wi], hT], [p3])
                    if tq > 0:
                        emit_y(ex, wi, tq - 1, (2 * fc, 2 * fc + 1))
                    elif ex > 0:
                        emit_y(ex - 1, (ex - 1) % NW, 3, (2 * fc, 2 * fc + 1))
                    P.op('act', lambda e, p1=p1, s_=s_: e.activation(out=s_.ap(), in_=p1.ap(), func=AF.Silu), [p1], [s_])
                    P.op('dve', lambda e, p3=p3, s_=s_, g_=g_, fc=fc: e.tensor_tensor(out=g_.ap()[:, fc, :], in0=s_.ap(), in1=p3.ap(),
                                                                                   op=ALU.mult), [s_, p3], [g_])
            if ex == n_exp - 1:
                emit_y(ex, wi, 3, (0, 1, 2, 3))
        P.pop()
        if stop_at in ('l0', 'moe2') and l == stop_layer:
            break

    ov = out_d.rearrange("(tt p) d -> p tt d", p=128)
    if stop_at == 'n1':
        for c in range(8):
            P.op('dve', lambda e, c=c: e.tensor_copy(out=X.ap()[:, c, :].rearrange("p (a b) -> p a b", a=1)[:, 0, :], in_=hT.ap()[:, c, 0:1024]), [hT], [X])
            P.op('dve', lambda e, c=c: e.tensor_copy(out=X.ap()[:, 8 + c, :], in_=hT.ap()[:, c, 1024:2048]), [hT], [X])
    for t in range(NT):
        P.dma('sp', ov[:, t, :], Xt[t].ap(), [Xt[t]], [out_buf])
    P.finish([out_buf] + extra_final)
    print("max sbuf top", P.max_top, "n_dma", P.n_dma, {e: len(P.ops[e]) for e in ENGINES})
    return nc


_CACHE = {}


def make_in_maps(inputs):
    consts = host_consts()
    f = lambda a: np.ascontiguousarray(np.asarray(a, dtype=np.float32))
    L = DEPTH
    shared = {
        'w_mod': f(inputs['w_mod']), 'b_mod': f(inputs['b_mod']), 'g_norm1': f(inputs['g_norm1']),
        'w_in': f(inputs['w_in']), 'gq_a': f(inputs['gq_a']), 'gk_a': f(inputs['gk_a']),
        'lam_a': f(inputs['lam_a']).reshape(L, 256), 'g_sub_a': f(inputs['g_sub_a']),
        'w_pool': f(inputs['w_pool']), 'b_pool': f(inputs['b_pool']).reshape(L, 256),
        'pool_scale': f(inputs['pool_scale']), 'gq_c': f(inputs['gq_c']), 'gk_c': f(inputs['gk_c']),
        'w_out': f(inputs['w_out']), 'g_norm2': f(inputs['g_norm2']), 'w_rg': f(inputs['w_rg']),
        'b_rg': f(inputs['b_rg']), 'w_re': f(inputs['w_re']), 'b_re': f(inputs['b_re']).reshape(L, 32),
        'w1': f(inputs['w1']).reshape(L, 32, D, 256), 'w3': f(inputs['w3']).reshape(L, 32, D, 256),
        'w2': f(inputs['w2']).reshape(L, 32, 256, D),
    }
    shared.update(consts)
    x = f(inputs['x'])
    c = f(inputs['c'])
    maps = []
    for b in range(8):
        m = dict(shared)
        m['x'] = np.ascontiguousarray(x[b])
        m['c_col'] = np.ascontiguousarray(c[b].reshape(8, 128).T)
        maps.append(m)
    return maps


FUSED = True


def kernel(**inputs):
    in_maps = make_in_maps(inputs)
    if FUSED:
        if 'nc' not in _CACHE:
            _CACHE['nc'] = build_program()
        res = run_bass_kernel_spmd(_CACHE['nc'], in_maps, core_ids=list(range(8)))
        return np.stack([np.asarray(r["out"], dtype=np.float32) for r in res.results], axis=0)
    for l in range(DEPTH):
        if ('nc', l) not in _CACHE:
            _CACHE[('nc', l)] = build_program(layers=(l,))
        res = run_bass_kernel_spmd(_CACHE[('nc', l)], in_maps, core_ids=list(range(8)))
        outs = [np.asarray(r["out"], dtype=np.float32) for r in res.results]
        for b in range(8):
            in_maps[b]['x'] = outs[b]
    return np.stack(outs, axis=0)
```
